# Optimizing a Trainium2 kernel written in Bass

```python
import jax
import jax.numpy as jnp
from jax import lax
import numpy as np

D_MODEL = 1024
BATCH = 8
SEQ = 4096
DEPTH = 4

N_MIXERS = 4
N_RET = (DEPTH + 3) // N_MIXERS
N_GLA = (DEPTH + 2) // N_MIXERS
N_LRU = (DEPTH + 1) // N_MIXERS
N_NSA = DEPTH // N_MIXERS
D_FF = 4 * D_MODEL
NORM_EPS = 1e-6
NEG_INF = -1e30

RET_HEADS = 4
RET_DK = D_MODEL // RET_HEADS
RET_DV = 2 * RET_DK
RET_CHUNK = 128
RET_IN = 2 * RET_HEADS * RET_DK + 2 * RET_HEADS * RET_DV

GLA_HEADS = 4
GLA_DK = D_MODEL // (2 * GLA_HEADS)
GLA_DV = D_MODEL // GLA_HEADS
GLA_RANK = 16
GLA_TAU = 16.0
GLA_CHUNK = 64
GLA_IN = 2 * GLA_HEADS * GLA_DK + 2 * GLA_HEADS * GLA_DV + GLA_RANK

LRU_WIDTH = D_MODEL
LRU_BLOCKS = 8
LRU_BS = LRU_WIDTH // LRU_BLOCKS
CONV_W = 4
LRU_C = 8.0

NSA_HEADS = 16
NSA_GROUPS = 2
NSA_HPG = NSA_HEADS // NSA_GROUPS
NSA_DK = 64
NSA_DV = 64
CMP_L = 32
CMP_STRIDE = 16
CMP_HID = 2 * NSA_DK
SLC_L = 64
SLC_TOP = 16
WIN = 512
NSA_QBLK = 64
FORCE_SCORE = 1e6
NSA_IN = NSA_HEADS * NSA_DK + 3 * NSA_GROUPS * (NSA_DK + NSA_DV) + 3 * NSA_HEADS

kernel_name = 'hybrid_ret_gla_rglru_nsa_trunk'


def rmsnorm(x, g):
    xf = x.astype(jnp.float32)
    y = xf * lax.rsqrt(jnp.mean(xf * xf, axis=-1, keepdims=True) + NORM_EPS)
    return (y * g.astype(jnp.float32)).astype(x.dtype)


def head_norm(o, g, center):
    of = o.astype(jnp.float32)
    if center:
        of = of - jnp.mean(of, axis=-1, keepdims=True)
    y = of * lax.rsqrt(jnp.mean(of * of, axis=-1, keepdims=True) + NORM_EPS)
    return y.reshape(*o.shape[:-2], -1) * g.astype(jnp.float32)


def masked_softmax(s, mask):
    s = jnp.where(mask, s.astype(jnp.float32), NEG_INF)
    m = jnp.max(s, axis=-1, keepdims=True)
    e = jnp.where(mask, jnp.exp(s - m), 0.0)
    return e / jnp.maximum(jnp.sum(e, axis=-1, keepdims=True), 1e-30)


def retention(h, w_in, gn, w_out):
    B, S, _ = h.shape
    H, DK, DV, C = RET_HEADS, RET_DK, RET_DV, RET_CHUNK
    n = S // C
    q, k, v, g = jnp.split(h @ w_in, [H * DK, 2 * H * DK, 2 * H * DK + H * DV], axis=-1)

    def to_chunks(t, d):
        return t.astype(jnp.float32).reshape(B, n, C, H, d).transpose(1, 0, 3, 2, 4)

    q = to_chunks(q, DK)
    k = to_chunks(k, DK) * DK ** -0.5
    v = to_chunks(v, DV)
    log_gamma = jnp.log1p(-jnp.exp2(-5.0 - jnp.arange(H, dtype=jnp.float32)))
    pos = jnp.arange(C, dtype=jnp.float32)
    diff = pos[:, None] - pos[None, :]
    decay_intra = jnp.where(diff >= 0, jnp.exp(log_gamma[:, None, None] * jnp.maximum(diff, 0.0)), 0.0)
    decay_q = jnp.exp(log_gamma[:, None] * (pos + 1.0))[None, :, :, None]
    decay_k = jnp.exp(log_gamma[:, None] * (C - 1.0 - pos))[None, :, :, None]
    decay_chunk = jnp.exp(log_gamma * C)[None, :, None, None]

    def step(state, inp):
        qc, kc, vc = inp
        scores = jnp.einsum('bhid,bhjd->bhij', qc, kc) * decay_intra
        o = jnp.einsum('bhij,bhje->bhie', scores, vc) + jnp.einsum('bhid,bhde->bhie', qc, state) * decay_q
        state = state * decay_chunk + jnp.einsum('bhjd,bhje->bhde', kc * decay_k, vc)
        return state, o

    _, o = lax.scan(step, jnp.zeros((B, H, DK, DV), jnp.float32), (q, k, v))
    o = o.transpose(1, 0, 3, 2, 4).reshape(B, S, H, DV)
    o = head_norm(o, gn, True) * jax.nn.silu(g.astype(jnp.float32))
    return (o @ w_out).astype(h.dtype)


def gla(h, w_in, w_gate_up, b_gate, gn, w_out):
    B, S, _ = h.shape
    H, DK, DV, C = GLA_HEADS, GLA_DK, GLA_DV, GLA_CHUNK
    n = S // C
    cuts = np.cumsum([H * DK, H * DK, H * DV, H * DV]).tolist()
    q, k, v, r, gl = jnp.split(h @ w_in, cuts, axis=-1)
    log_alpha = jax.nn.log_sigmoid((gl @ w_gate_up + b_gate).astype(jnp.float32)) / GLA_TAU

    def to_chunks(t, d):
        return t.astype(jnp.float32).reshape(B, n, C, H, d).transpose(1, 0, 3, 2, 4)

    q = to_chunks(q, DK) * DK ** -0.5
    k = to_chunks(k, DK)
    v = to_chunks(v, DV)
    la = to_chunks(log_alpha, DK)
    causal = jnp.tril(jnp.ones((C, C), dtype=bool))[:, :, None]

    def step(state, inp):
        qc, kc, vc, lac = inp
        b = jnp.cumsum(lac, axis=2)
        rel = jnp.where(causal, b[:, :, :, None, :] - b[:, :, None, :, :], -jnp.inf)
        A = jnp.einsum('bhid,bhjd,bhijd->bhij', qc, kc, jnp.exp(rel))
        b_last = b[:, :, -1:, :]
        o = jnp.einsum('bhij,bhje->bhie', A, vc) + jnp.einsum('bhid,bhde->bhie', qc * jnp.exp(b), state)
        state = state * jnp.exp(b_last)[:, :, 0, :, None] + jnp.einsum('bhjd,bhje->bhde', kc * jnp.exp(b_last - b), vc)
        return state, o

    _, o = lax.scan(step, jnp.zeros((B, H, DK, DV), jnp.float32), (q, k, v, la))
    o = o.transpose(1, 0, 3, 2, 4).reshape(B, S, H, DV)
    o = head_norm(o, gn, False) * jax.nn.silu(r.astype(jnp.float32))
    return (o @ w_out).astype(h.dtype)


def rglru_block(h, w_in, conv_w, conv_b, w_a, b_a, w_x, b_x, lam, w_out):
    B, S, _ = h.shape
    xb, yb = jnp.split(h @ w_in, 2, axis=-1)
    y = jax.nn.gelu(yb.astype(jnp.float32))
    xp = jnp.pad(xb, ((0, 0), (CONV_W - 1, 0), (0, 0)))
    xc = conv_b
    for tap in range(CONV_W):
        xc = xc + xp[:, tap:tap + S] * conv_w[tap]
    xr = xc.reshape(B, S, LRU_BLOCKS, LRU_BS)
    gate_r = jax.nn.sigmoid(jnp.einsum('bsnc,ncd->bsnd', xr, w_a).reshape(B, S, LRU_WIDTH) + b_a).astype(jnp.float32)
    gate_i = jax.nn.sigmoid(jnp.einsum('bsnc,ncd->bsnd', xr, w_x).reshape(B, S, LRU_WIDTH) + b_x).astype(jnp.float32)
    log_a = -LRU_C * gate_r * jax.nn.softplus(-lam.astype(jnp.float32))
    a = jnp.exp(log_a)
    u = jnp.sqrt(-jnp.expm1(2.0 * log_a)) * gate_i * xc.astype(jnp.float32)

    def combine(left, right):
        a1, b1 = left
        a2, b2 = right
        return a1 * a2, a2 * b1 + b2

    _, hs = lax.associative_scan(combine, (a, u), axis=1)
    return ((hs * y) @ w_out).astype(h.dtype)


def nsa(h, w_in, pe_k, w1_k, w2_k, pe_v, w1_v, w2_v, w_out):
    B, S, _ = h.shape
    H, G, HPG, DK, DV, QB = NSA_HEADS, NSA_GROUPS, NSA_HPG, NSA_DK, NSA_DV, NSA_QBLK
    sizes = [H * DK] + [G * DK, G * DV] * 3 + [3 * H]
    cuts = np.cumsum(sizes)[:-1].tolist()
    q, kc, vc, ks, vs, kw, vw, gl = jnp.split(h @ w_in, cuts, axis=-1)
    q = q.reshape(B, S, G, HPG, DK).transpose(0, 2, 3, 1, 4)
    gates = jax.nn.sigmoid(gl.astype(jnp.float32)).reshape(B, S, 3, G, HPG).transpose(0, 3, 4, 1, 2)

    def heads(t, d):
        return t.reshape(B, S, G, d).transpose(0, 2, 1, 3)

    n_cmp = (S - CMP_L) // CMP_STRIDE + 1
    cmp_start = jnp.arange(n_cmp) * CMP_STRIDE
    tok_idx = cmp_start[:, None] + jnp.arange(CMP_L)[None, :]

    def compress(t, pe, w1, w2, d):
        blocks = heads(t, d)[:, :, tok_idx] + pe
        return jax.nn.gelu(blocks.reshape(B, G, n_cmp, CMP_L * d) @ w1) @ w2

    k_cmp = compress(kc, pe_k, w1_k, w2_k, DK)
    v_cmp = compress(vc, pe_v, w1_v, w2_v, DV)
    cmp_end = cmp_start + CMP_L - 1

    n_slc = S // SLC_L
    k_slc = heads(ks, DK).reshape(B, G, n_slc, SLC_L, DK)
    v_slc = heads(vs, DV).reshape(B, G, n_slc, SLC_L, DV)
    slc_start = jnp.arange(n_slc) * SLC_L
    overlap = ((cmp_start[:, None] < slc_start[None, :] + SLC_L)
               & (cmp_start[:, None] + CMP_L > slc_start[None, :])).astype(jnp.float32)
    n_top = min(SLC_TOP, n_slc)
    blk_ids = jnp.arange(n_slc)

    k_win = jnp.pad(heads(kw, DK), ((0, 0), (0, 0), (WIN, 0), (0, 0)))
    v_win = jnp.pad(heads(vw, DV), ((0, 0), (0, 0), (WIN, 0), (0, 0)))

    slopes = jnp.exp2(-8.0 * (jnp.arange(H, dtype=jnp.float32) + 1.0) / H).reshape(1, G, HPG, 1, 1)
    scale = DK ** -0.5
    bi = jnp.arange(B)[:, None, None, None]
    gi = jnp.arange(G)[None, :, None, None]

    def block(i):
        q0 = i * QB
        qb = lax.dynamic_slice_in_dim(q, q0, QB, axis=3)
        t = q0 + jnp.arange(QB)
        dist_c = (t[:, None] - cmp_end[None, :]).astype(jnp.float32)
        s_c = jnp.einsum('bghqd,bgnd->bghqn', qb, k_cmp) * scale - slopes * dist_c
        p_c = masked_softmax(s_c, cmp_end[None, :] <= t[:, None])
        o_c = jnp.einsum('bghqn,bgnd->bghqd', p_c, v_cmp)
        imp = jnp.einsum('bghqn,nj->bgqj', p_c, overlap)
        cur = t // SLC_L
        forced = (blk_ids[None, :] == 0) | (blk_ids[None, :] == cur[:, None]) | (blk_ids[None, :] == cur[:, None] - 1)
        imp = jnp.where(forced, FORCE_SCORE, imp)
        imp = jnp.where(blk_ids[None, :] <= cur[:, None], imp, -1.0)
        _, idx = lax.top_k(imp, n_top)
        kg = k_slc[bi, gi, idx]
        vg = v_slc[bi, gi, idx].reshape(B, G, QB, n_top * SLC_L, DV)
        pos = idx[..., None] * SLC_L + jnp.arange(SLC_L)
        dist_s = (t[:, None, None] - pos).astype(jnp.float32)[:, :, None]
        s_s = jnp.einsum('bghqd,bgqkld->bghqkl', qb, kg) * scale - slopes[..., None] * dist_s
        m_s = (pos <= t[:, None, None]).reshape(B, G, QB, n_top * SLC_L)[:, :, None]
        p_s = masked_softmax(s_s.reshape(B, G, HPG, QB, n_top * SLC_L), m_s)
        o_s = jnp.einsum('bghqm,bgqmd->bghqd', p_s, vg)
        kwb = lax.dynamic_slice_in_dim(k_win, q0, WIN + QB, axis=2)
        vwb = lax.dynamic_slice_in_dim(v_win, q0, WIN + QB, axis=2)
        spos = q0 - WIN + jnp.arange(WIN + QB)
        dist_w = t[:, None] - spos[None, :]
        m_w = (spos[None, :] >= 0) & (dist_w >= 0) & (dist_w < WIN)
        s_w = jnp.einsum('bghqd,bgsd->bghqs', qb, kwb) * scale - slopes * dist_w.astype(jnp.float32)
        p_w = masked_softmax(s_w, m_w)
        o_w = jnp.einsum('bghqs,bgsd->bghqd', p_w, vwb)
        gb = lax.dynamic_slice_in_dim(gates, q0, QB, axis=3)
        return gb[..., 0:1] * o_c + gb[..., 1:2] * o_s + gb[..., 2:3] * o_w

    o = lax.map(block, jnp.arange(S // QB))
    o = o.transpose(1, 0, 4, 2, 3, 5).reshape(B, S, H * DV)
    return (o @ w_out).astype(h.dtype)


def setup_inputs(seed: int = 0) -> dict:
    key = jax.random.key(seed)
    ks = iter(jax.random.split(key, 40))

    def w(shape, fan_in):
        return jax.random.normal(next(ks), shape, jnp.float32) * fan_in ** -0.5

    def gain(shape):
        return 1.0 + 0.02 * jax.random.normal(next(ks), shape, jnp.float32)

    def small(shape, s):
        return s * jax.random.normal(next(ks), shape, jnp.float32)

    x = jax.random.normal(next(ks), (BATCH, SEQ, D_MODEL), jnp.float32)
    u = jax.random.uniform(next(ks), (N_LRU, LRU_WIDTH), jnp.float32, 0.9, 0.999)
    a_base = u ** (1.0 / LRU_C)
    lru_lambda = jnp.log(a_base) - jnp.log1p(-a_base)
    return {
        'x': x,
        'norm_mix_pre': gain((DEPTH, D_MODEL)),
        'norm_mix_post': gain((DEPTH, D_MODEL)),
        'norm_mlp_pre': gain((DEPTH, D_MODEL)),
        'norm_mlp_post': gain((DEPTH, D_MODEL)),
        'mlp_w_up': w((DEPTH, D_MODEL, D_FF), D_MODEL),
        'mlp_w_down': w((DEPTH, D_FF, D_MODEL), D_FF),
        'ret_w_in': w((N_RET, D_MODEL, RET_IN), D_MODEL),
        'ret_gn': gain((N_RET, RET_HEADS * RET_DV)),
        'ret_w_out': w((N_RET, RET_HEADS * RET_DV, D_MODEL), RET_HEADS * RET_DV),
        'gla_w_in': w((N_GLA, D_MODEL, GLA_IN), D_MODEL),
        'gla_w_gate_up': w((N_GLA, GLA_RANK, GLA_HEADS * GLA_DK), GLA_RANK),
        'gla_b_gate': small((N_GLA, GLA_HEADS * GLA_DK), 0.5),
        'gla_gn': gain((N_GLA, GLA_HEADS * GLA_DV)),
        'gla_w_out': w((N_GLA, GLA_HEADS * GLA_DV, D_MODEL), GLA_HEADS * GLA_DV),
        'lru_w_in': w((N_LRU, D_MODEL, 2 * LRU_WIDTH), D_MODEL),
        'lru_conv_w': w((N_LRU, CONV_W, LRU_WIDTH), CONV_W),
        'lru_conv_b': small((N_LRU, LRU_WIDTH), 0.01),
        'lru_w_a': w((N_LRU, LRU_BLOCKS, LRU_BS, LRU_BS), LRU_BS),
        'lru_b_a': small((N_LRU, LRU_WIDTH), 0.01),
        'lru_w_x': w((N_LRU, LRU_BLOCKS, LRU_BS, LRU_BS), LRU_BS),
        'lru_b_x': small((N_LRU, LRU_WIDTH), 0.01),
        'lru_lambda': lru_lambda,
        'lru_w_out': w((N_LRU, LRU_WIDTH, D_MODEL), LRU_WIDTH),
        'nsa_w_in': w((N_NSA, D_MODEL, NSA_IN), D_MODEL),
        'nsa_pe_k': small((N_NSA, CMP_L, NSA_DK), 0.1),
        'nsa_w1_k': w((N_NSA, CMP_L * NSA_DK, CMP_HID), CMP_L * NSA_DK),
        'nsa_w2_k': w((N_NSA, CMP_HID, NSA_DK), CMP_HID),
        'nsa_pe_v': small((N_NSA, CMP_L, NSA_DV), 0.1),
        'nsa_w1_v': w((N_NSA, CMP_L * NSA_DV, CMP_HID), CMP_L * NSA_DV),
        'nsa_w2_v': w((N_NSA, CMP_HID, NSA_DV), CMP_HID),
        'nsa_w_out': w((N_NSA, NSA_HEADS * NSA_DV, D_MODEL), NSA_HEADS * NSA_DV),
    }


def reference(x, norm_mix_pre, norm_mix_post, norm_mlp_pre, norm_mlp_post, mlp_w_up, mlp_w_down,
              ret_w_in, ret_gn, ret_w_out,
              gla_w_in, gla_w_gate_up, gla_b_gate, gla_gn, gla_w_out,
              lru_w_in, lru_conv_w, lru_conv_b, lru_w_a, lru_b_a, lru_w_x, lru_b_x, lru_lambda, lru_w_out,
              nsa_w_in, nsa_pe_k, nsa_w1_k, nsa_w2_k, nsa_pe_v, nsa_w1_v, nsa_w2_v, nsa_w_out):
    for i in range(DEPTH):
        m, j = i % N_MIXERS, i // N_MIXERS
        h = rmsnorm(x, norm_mix_pre[i])
        if m == 0:
            h = retention(h, ret_w_in[j], ret_gn[j], ret_w_out[j])
        elif m == 1:
            h = gla(h, gla_w_in[j], gla_w_gate_up[j], gla_b_gate[j], gla_gn[j], gla_w_out[j])
        elif m == 2:
            h = rglru_block(h, lru_w_in[j], lru_conv_w[j], lru_conv_b[j], lru_w_a[j], lru_b_a[j],
                            lru_w_x[j], lru_b_x[j], lru_lambda[j], lru_w_out[j])
        else:
            h = nsa(h, nsa_w_in[j], nsa_pe_k[j], nsa_w1_k[j], nsa_w2_k[j],
                    nsa_pe_v[j], nsa_w1_v[j], nsa_w2_v[j], nsa_w_out[j])
        x = x + rmsnorm(h, norm_mix_post[i])
        h = rmsnorm(x, norm_mlp_pre[i])
        h = jnp.square(jax.nn.relu(h @ mlp_w_up[i])) @ mlp_w_down[i]
        x = x + rmsnorm(h, norm_mlp_post[i])
    return x
```

```python
import contextlib
import numpy as np
import concourse.bass as bass
import concourse.mybir as mybir
from concourse.bass_utils import run_bass_kernel_spmd

F32 = mybir.dt.float32
BF16 = mybir.dt.bfloat16
AF = mybir.ActivationFunctionType
ALU = mybir.AluOpType
AX = mybir.AxisListType

D = 1024
DFF = 4096
DEPTH = 4
EPS = 1e-6
ENG = ['pe', 'act', 'dve', 'pool', 'sp']
NDSEM = 16


class Buf:
    __slots__ = ('w', 'r', 'x')

    def __init__(self, x=False):
        self.w = None
        self.r = {}
        self.x = x


class Op:
    __slots__ = ('eng', 'fn', 'waits', 'idx', 'inc', 'semval', 'dma', 'dsem', 'dval')


class G:
    def __init__(self, nc, es):
        self.nc = nc
        self.q = {e: [] for e in ENG}
        self.waited = {e: {} for e in ENG}
        self.sem = {e: es.enter_context(nc.semaphore('s_' + e)) for e in ENG}
        self.dq = {}
        for e in ('sp', 'pool', 'act'):
            self.dq[e] = [[es.enter_context(nc.semaphore('d_%s%d' % (e, i))) for i in range(NDSEM)], 0]
        self.uid = 0
        self.fill_regs = {}
        import os
        self.max_ops = int(os.environ.get('KMAX', '100000000'))

    def _rawwait(self, o, sem, val):
        key = id(sem)
        if val <= self.waited[o.eng].get(key, 0):
            return
        self.waited[o.eng][key] = val
        o.waits.append(('raw', sem, val))

    def _wait(self, o, d):
        if d.dma:
            self._rawwait(o, d.dsem, d.dval)
            return
        if d.eng == o.eng and o.eng == 'pe':
            return
        if d.idx <= self.waited[o.eng].get(d.eng, -1):
            return
        self.waited[o.eng][d.eng] = d.idx
        d.inc = True
        o.waits.append(('op', d))

    def op(self, eng, fn, r=(), w=(), dma=False):
        if self.uid >= self.max_ops:
            return None
        o = Op()
        o.eng = eng
        o.fn = fn
        o.waits = []
        o.idx = len(self.q[eng])
        o.inc = False
        o.dma = dma
        o.semval = 0
        if any(b.x for b in r):
            w = list(w) + [b for b in r if b.x and b not in w]
            r = [b for b in r if not b.x]
        for b in r:
            if b.w is not None:
                self._wait(o, b.w)
        for b in w:
            if b.w is not None:
                self._wait(o, b.w)
            for d in b.r.values():
                self._wait(o, d)
        if dma:
            sems, cnt = self.dq[eng]
            k = cnt % len(sems)
            o.dsem = sems[k]
            o.dval = 16 * (cnt // len(sems) + 1)
            self.dq[eng][1] = cnt + 1
            if o.dval > 16:
                self._rawwait(o, o.dsem, o.dval - 16)
        self.q[eng].append(o)
        self.uid += 1
        key = ('d', self.uid) if dma else eng
        for b in r:
            b.r[key] = o
        for b in w:
            b.w = o
            b.r = {}
        return o

    def barrier(self):
        last = {}
        for e in ENG:
            last[e] = None
            for o in reversed(self.q[e]):
                if o.fn is not None:
                    last[e] = o
                    break
        dlast = []
        for e in self.dq:
            sems, cnt = self.dq[e]
            for k in range(len(sems)):
                n = (cnt - k + len(sems) - 1) // len(sems) if cnt > k else 0
                if n > 0:
                    dlast.append((sems[k], 16 * n))
        for e in ENG:
            o = Op()
            o.eng = e
            o.fn = None
            o.waits = []
            o.idx = len(self.q[e])
            o.inc = False
            o.dma = False
            o.semval = 0
            for f in ENG:
                d = last[f]
                if d is None or f == e:
                    continue
                if d.dma:
                    continue
                if d.idx <= self.waited[e].get(f, -1):
                    continue
                self.waited[e][f] = d.idx
                d.inc = True
                o.waits.append(('op', d))
            for sem, val in dlast:
                self._rawwait(o, sem, val)
            self.q[e].append(o)

    def emit(self):
        nc = self.nc
        for e in ENG:
            c = 0
            for o in self.q[e]:
                if o.inc:
                    c += 1
                    o.semval = c
        sem = self.sem

        def run(e, h):
            for o in self.q[e]:
                for wt in o.waits:
                    if wt[0] == 'op':
                        h.wait_ge(sem[wt[1].eng], wt[1].semval)
                    else:
                        h.wait_ge(wt[1], wt[2])
                if o.fn is None:
                    continue
                ins = o.fn(h)
                if o.dma:
                    ins.then_inc(o.dsem, 16)
                elif o.inc:
                    ins.then_inc(sem[e], 1)

        with nc.Block() as block:
            @block.tensor
            def _(h):
                run('pe', h)

            @block.scalar
            def _(h):
                run('act', h)

            @block.vector
            def _(h):
                run('dve', h)

            @block.gpsimd
            def _(h):
                run('pool', h)

            @block.sync
            def _(h):
                run('sp', h)

    def mark(self, name):
        import os
        if os.environ.get('KDBG'):
            print('MARK', name, self.uid, flush=True)

    def mm(self, out, lhsT, rhs, start, stop, r=(), w=(), skip=False):
        if skip:
            return self.op('pe', lambda h: h.matmul(out, lhsT, rhs, start=start, stop=stop, skip_group_check=True), r, w)
        return self.op('pe', lambda h: h.matmul(out, lhsT, rhs, start=start, stop=stop), r, w)

    def tr(self, out, in_, ident, r=(), w=()):
        return self.op('pe', lambda h: h.transpose(out, in_, ident), r, w)

    def act(self, out, in_, func, r=(), w=(), **kw):
        return self.op('act', lambda h: h.activation(out, in_, func, **kw), r, w)

    def tt(self, eng, out, in0, in1, op, r=(), w=()):
        return self.op(eng, lambda h: h.tensor_tensor(out, in0, in1, op), r, w)

    def ts(self, eng, out, in0, s1, s2, op0, op1=None, r=(), w=(), **kw):
        if op1 is None:
            return self.op(eng, lambda h: h.tensor_scalar(out, in0, s1, None, op0, **kw), r, w)
        return self.op(eng, lambda h: h.tensor_scalar(out, in0, s1, s2, op0, op1, **kw), r, w)

    def stt(self, eng, out, in0, sc, in1, op0, op1, r=(), w=()):
        return self.op(eng, lambda h: h.scalar_tensor_tensor(out, in0, sc, in1, op0, op1), r, w)

    def cp(self, eng, out, in_, r=(), w=()):
        if eng == 'act':
            return self.op('act', lambda h: h.copy(out, in_), r, w)
        return self.op(eng, lambda h: h.tensor_copy(out, in_), r, w)

    def affsel(self, out, in_, pattern, op, fill, base, cm, r=(), w=()):
        return self.op('pool', lambda h: h.affine_select(out, in_, pattern, op, float(fill), base=base,
                                                         channel_multiplier=cm), r, w)

    def memset(self, eng, ap, val, r=(), w=()):
        return self.op(eng, lambda h: h.memset(ap, val), r, w)

    def dma(self, eng, out, in_, r=(), w=(), **kw):
        return self.op(eng, lambda h: h.dma_start(out, in_, **kw), r, w, dma=True)


class K:
    def __init__(self, nc, g, es, S):
        self.nc = nc
        self.g = g
        self.es = es
        self.S = S
        self.NT = S // 128
        self.n = 0

    def sb(self, es, shape, dt, name=None):
        self.n += 1
        return es.enter_context(self.nc.sbuf_tensor('%s_%d' % (name or 't', self.n), list(shape), dt))

    def ps(self, es, shape, dt=F32, name=None):
        self.n += 1
        return es.enter_context(self.nc.psum_tensor('%s_%d' % (name or 'p', self.n), list(shape), dt))


def setup_consts(k):
    g, es = k.g, k.es
    k.ident_f = k.sb(es, [128, 128], F32, 'identf')
    k.ident_b = k.sb(es, [128, 128], BF16, 'identb')
    k.b_ident = Buf()
    ones = k.sb(es, [128, 128], F32, 'ones')
    bo = Buf()
    g.memset('pool', ones[:], 1.0, w=[bo])
    g.affsel(k.ident_f[:], ones[:], [[-1, 128]], ALU.is_equal, 0.0, 0, 1, r=[bo], w=[k.b_ident])
    g.cp('pool', k.ident_b[:], k.ident_f[:], r=[k.b_ident], w=[k.b_ident])
    k.ones_f = ones
    k.b_ones = bo
    k.stage = k.sb(es, [16, D], F32, 'stage')
    k.b_stage = Buf()


def load_cols(k, es, rows_ap, R, ps_bank, b_ps, name):
    g = k.g
    stage = k.stage[0:R, :]
    cols = k.sb(es, [128, 8, R], F32, name)
    bs, bc = k.b_stage, Buf()
    if isinstance(rows_ap, list):
        r0 = 0
        for ra in rows_ap:
            n = ra.shape[0]
            g.dma('sp', k.stage[r0:r0 + n, :], ra, w=[bs])
            r0 += n
        assert r0 == R
    else:
        g.dma('sp', stage, rows_ap, w=[bs])
    for c in range(8):
        g.tr(ps_bank[:, c * R:(c + 1) * R], stage[:, c * 128:(c + 1) * 128], k.ident_f[0:R, 0:R],
             r=[bs, k.b_ident], w=[b_ps])
    g.cp('dve', cols[:].rearrange('p c r -> p (c r)'), ps_bank[:, 0:8 * R], r=[b_ps], w=[bc])
    return cols, bc


def emit_front(k, xin_tile_ap, b_xd, xt, b_xt, gcol_ap, b_gcol, hT_ap, b_hT, scr, ps_tr, b_ps_tr, st, b_st,
               hb, b_hb):
    g = k.g
    g.dma('sp', xt[:], xin_tile_ap, r=[b_xd], w=[b_xt])
    g.act(scr[:], xt[:], AF.Square, r=[b_xt], w=[b_st, k.b_scr], accum_out=st[:, 0:1])
    g.ts('dve', st[:, 1:2], st[:, 0:1], 1.0 / D, EPS, ALU.mult, ALU.add, r=[b_st], w=[b_st])
    g.act(st[:, 3:4], st[:, 1:2], AF.Sqrt, r=[b_st], w=[b_st])
    g.op('dve', lambda h, o=st[:, 2:3], i=st[:, 3:4]: h.reciprocal(o, i), r=[b_st], w=[b_st])
    g.ts('dve', hb[:], xt[:], st[:, 2:3], None, ALU.mult, r=[b_xt, b_st], w=[b_hb])
    for c in range(8):
        g.tr(ps_tr[:, c * 128:(c + 1) * 128], hb[:, c * 128:(c + 1) * 128], k.ident_b[:],
             r=[b_hb, k.b_ident], w=[b_ps_tr])
    g.tt('dve', hT_ap, ps_tr[:].rearrange('p (c t) -> p c t', c=8),
         gcol_ap.unsqueeze(2).to_broadcast([128, 8, 128]), ALU.mult, r=[b_ps_tr, b_gcol], w=[b_hT])


def emit_post(k, ps_ap, b_ps, xres, b_xres, gpost, b_gpost, xout_tile_ap, b_xd, scr, st, b_st,
              tmp, b_tmp):
    g = k.g
    g.act(scr[:], ps_ap, AF.Square, r=[b_ps], w=[b_st, k.b_scr], accum_out=st[:, 0:1])
    g.ts('dve', st[:, 1:2], st[:, 0:1], 1.0 / D, EPS, ALU.mult, ALU.add, r=[b_st], w=[b_st])
    g.act(st[:, 3:4], st[:, 1:2], AF.Sqrt, r=[b_st], w=[b_st])
    g.op('dve', lambda h, o=st[:, 2:3], i=st[:, 3:4]: h.reciprocal(o, i), r=[b_st], w=[b_st])
    g.stt('dve', tmp[:], ps_ap, st[:, 2:3], gpost[:], ALU.mult, ALU.mult, r=[b_ps, b_st, b_gpost], w=[b_tmp])
    g.tt('pool', xres[:], tmp[:], xres[:], ALU.add, r=[b_tmp], w=[b_xres])
    g.dma('sp', xout_tile_ap, xres[:], r=[b_xres], w=[b_xd])


def phase_mlp(k, li, xd, b_xd, W):
    g, nc = k.g, k.nc
    NT = k.NT
    GT = 4 if NT % 4 == 0 else 1
    NG = NT // GT
    TG = GT * 128
    with contextlib.ExitStack() as es:
        wup = k.sb(es, [128, 8, DFF], BF16, 'wup')
        wdn = k.sb(es, [128, 32, D], BF16, 'wdn')
        b_wup = [Buf() for _ in range(8)]
        b_wdn = [Buf() for _ in range(8)]
        wup_d = W['mlp_w_up'][li].rearrange('(c p) f -> p c f', p=128)
        wdn_d = W['mlp_w_down'][li].rearrange('(c p) d -> p c d', p=128)
        for j in range(8):
            g.dma('pool', wup[:, :, j * 512:(j + 1) * 512], wup_d[:, :, j * 512:(j + 1) * 512], w=[b_wup[j]])
        for j in range(8):
            g.dma('pool', wdn[:, j * 4:(j + 1) * 4, :], wdn_d[:, j * 4:(j + 1) * 4, :], w=[b_wdn[j]])
        ps_tr = k.ps(es, [128, 1024], BF16, 'pstr')
        b_ps_tr = Buf(True)
        ps_up = [k.ps(es, [128, 512], F32, 'psup') for _ in range(3)]
        b_ps_up = [Buf(True) for _ in range(3)]
        ps_dn = [k.ps(es, [128, 1024], F32, 'psdn') for _ in range(2)]
        b_ps_dn = [Buf(True) for _ in range(2)]
        gcol, b_gcol = load_cols(k, es, W['norm_mlp_pre'][li:li + 1, :], 1, ps_up[0], b_ps_up[0], 'gcol')
        gpost = k.sb(es, [128, D], F32, 'gpost')
        b_gpost = Buf()
        g.dma('sp', gpost[:], W['norm_mlp_post'][li:li + 1, :].partition_broadcast(128), w=[b_gpost])
        NX = 3
        xt = [k.sb(es, [128, D], F32, 'xt') for _ in range(NX)]
        b_xt = [Buf() for _ in range(NX)]
        hb = [k.sb(es, [128, D], BF16, 'hb') for _ in range(2)]
        b_hb = [Buf() for _ in range(2)]
        scr = k.sb(es, [128, D], BF16, 'scr')
        k.b_scr = Buf()
        st = [k.sb(es, [128, 4], F32, 'st') for _ in range(4)]
        b_st = [Buf() for _ in range(4)]
        hT = k.sb(es, [128, 8, TG], BF16, 'hT')
        b_hT = [Buf() for _ in range(GT)]
        aT = k.sb(es, [128, 32, TG], BF16, 'aT')
        b_aT = [Buf() for _ in range(32)]
        rr = [k.sb(es, [128, TG], F32, 'rr') for _ in range(2)]
        b_rr = [Buf() for _ in range(2)]
        tmp = [k.sb(es, [128, D], F32, 'tmp') for _ in range(2)]
        b_tmp = [Buf() for _ in range(2)]
        cnt = 0
        for gi in range(NG):
            for tl in range(GT):
                t = gi * GT + tl
                i = cnt % NX
                emit_front(k, xd[t * 128:(t + 1) * 128, :], b_xd[t], xt[i], b_xt[i], gcol[:, :, 0], b_gcol,
                           hT[:, :, tl * 128:(tl + 1) * 128], b_hT[tl], scr, ps_tr, b_ps_tr, st[cnt % 2],
                           b_st[cnt % 2], hb[cnt % 2], b_hb[cnt % 2])
                cnt += 1
            for f in range(32):
                pi = f % 3
                for c in range(8):
                    g.mm(ps_up[pi][:, 0:TG], wup[:, c, f * 128:(f + 1) * 128], hT[:, c, :], c == 0, c == 7,
                         r=[b_wup[f // 4]] + b_hT, w=[b_ps_up[pi]])
                ri = f % 2
                g.act(rr[ri][:], ps_up[pi][:, 0:TG], AF.Relu, r=[b_ps_up[pi]], w=[b_rr[ri]])
                g.tt('pool' if f % 2 else 'dve', aT[:, f, :], rr[ri][:], rr[ri][:], ALU.mult, r=[b_rr[ri]],
                     w=[b_aT[f]])
            for tl in range(GT):
                t = gi * GT + tl
                pd = t % 2
                for hf in range(2):
                    for f in range(32):
                        g.mm(ps_dn[pd][:, hf * 512:(hf + 1) * 512], aT[:, f, tl * 128:(tl + 1) * 128],
                             wdn[:, f, hf * 512:(hf + 1) * 512], f == 0, f == 31,
                             r=[b_aT[f], b_wdn[f // 4]], w=[b_ps_dn[pd]])
                i = cnt % NX
                cnt += 1
                g.dma('sp', xt[i][:], xd[t * 128:(t + 1) * 128, :], r=[b_xd[t]], w=[b_xt[i]])
                emit_post(k, ps_dn[pd][:], b_ps_dn[pd], xt[i], b_xt[i], gpost, b_gpost,
                          xd[t * 128:(t + 1) * 128, :], b_xd[t], scr, st[2 + pd], b_st[2 + pd], tmp[pd], b_tmp[pd])
        g.barrier()


class PsPool:
    def __init__(self, k, es, n):
        self.t = [k.ps(es, [128, 512], F32, 'pp') for _ in range(n)]
        self.b = [Buf(True) for _ in range(n)]
        self.i = 0

    def next(self):
        j = self.i % len(self.t)
        self.i += 1
        return self.t[j], self.b[j]


def load_w(k, wt, w_dram_ap, ncols, piece, nchunk=None):
    g = k.g
    src = w_dram_ap.rearrange('(c p) f -> p c f', p=128)
    bufs = []
    for j in range(0, ncols, piece):
        b = Buf()
        e = min(ncols, j + piece)
        g.dma('pool', wt[:, :, j:e], src[:, :, j:e], w=[b])
        bufs.append(b)
    return bufs


def mixer_common(k, es, li, W, nxt=2, npool=6):
    g = k.g
    c = {}
    c['pp'] = PsPool(k, es, npool)
    c['ps_out'] = k.ps(es, [128, 1024], F32, 'psout')
    c['b_ps_out'] = Buf(True)
    pt, pb = c['pp'].next()
    c['gcol'], c['b_gcol'] = load_cols(k, es, W['norm_mix_pre'][li:li + 1, :], 1, pt, pb, 'gcol')
    c['gpost'] = k.sb(es, [128, D], F32, 'gpost')
    c['b_gpost'] = Buf()
    g.dma('sp', c['gpost'][:], W['norm_mix_post'][li:li + 1, :].partition_broadcast(128), w=[c['b_gpost']])
    c['xt'] = [k.sb(es, [128, D], F32, 'xt') for _ in range(nxt)]
    c['b_xt'] = [Buf() for _ in range(nxt)]
    c['hb'] = k.sb(es, [128, D], BF16, 'hb')
    c['b_hb'] = Buf()
    c['scr'] = c['hb']
    k.b_scr = c['b_hb']
    c['st'] = [k.sb(es, [128, 4], F32, 'st') for _ in range(4)]
    c['b_st'] = [Buf() for _ in range(4)]
    c['tmp'] = k.sb(es, [128, D], F32, 'tmp')
    c['b_tmp'] = Buf()
    return c


def mixer_front(k, c, t, xd, b_xd, hT_ap, b_hT):
    pt, pb = c['pp'].next()
    i = t % 2
    ix = t % len(c['xt'])
    emit_front(k, xd[t * 128:(t + 1) * 128, :], b_xd[t], c['xt'][ix], c['b_xt'][ix], c['gcol'][:, :, 0], c['b_gcol'],
               hT_ap, b_hT, c['scr'], pt[:].bitcast(BF16), pb, c['st'][i], c['b_st'][i], c['hb'], c['b_hb'])


def mixer_back(k, c, t, xd, b_xd):
    i = t % 2
    ix = t % len(c['xt'])
    emit_post(k, c['ps_out'][:], c['b_ps_out'], c['xt'][ix], c['b_xt'][ix], c['gpost'], c['b_gpost'],
              xd[t * 128:(t + 1) * 128, :], b_xd[t], c['scr'], c['st'][2 + i], c['b_st'][2 + i], c['tmp'], c['b_tmp'])


def head_norm_stats(k, src_ap, mv_ap, b_src, b_mv, scratch6, b_s6):
    g = k.g
    g.op('dve', lambda h: h.bn_stats(scratch6, src_ap), r=[b_src], w=[b_s6])
    g.op('dve', lambda h: h.bn_aggr(mv_ap, scratch6), r=[b_s6], w=[b_mv])


def phase_ret(k, li, xd, b_xd, W):
    import math
    g, nc = k.g, k.nc
    NT = k.NT
    j = li // 4
    H, DK, DV = 4, 256, 512
    lg = [math.log1p(-2.0 ** (-5.0 - h)) for h in range(H)]
    with contextlib.ExitStack() as es:
        win = k.sb(es, [128, 8, 6144], BF16, 'win')
        wout = k.sb(es, [128, 16, D], BF16, 'wout')
        b_win = load_w(k, win, W['ret_w_in'][j], 6144, 512)
        b_wout = load_w(k, wout, W['ret_w_out'][j], D, 512)
        c = mixer_common(k, es, li, W)
        pp = c['pp']
        pt, pb = pp.next()
        gncol, b_gncol = load_cols(k, es, W['ret_gn'][j:j + 1, :].rearrange('o (r f) -> (o r) f', r=2), 2, pt, pb,
                                   'gncol')
        decq = k.sb(es, [128, 8, 128], BF16, 'decq')
        dintra = k.sb(es, [128, H, 128], F32, 'dintra')
        dk = k.sb(es, [128, H], F32, 'dk')
        iot = k.sb(es, [128, 128], F32, 'iot')
        iot2 = k.sb(es, [128, 128], F32, 'iot2')
        iot3 = k.sb(es, [128, 1], F32, 'iot3')
        b_c = Buf()
        g.op('pool', lambda h: h.iota(iot[:], [[1, 128]], base=1, channel_multiplier=0,
                                      allow_small_or_imprecise_dtypes=True), w=[b_c])
        g.op('pool', lambda h: h.iota(iot2[:], [[1, 128]], base=0, channel_multiplier=-1,
                                      allow_small_or_imprecise_dtypes=True), w=[b_c])
        g.op('pool', lambda h: h.iota(iot3[:], [[0, 1]], base=127, channel_multiplier=-1,
                                      allow_small_or_imprecise_dtypes=True), w=[b_c])
        g.ts('pool', iot2[:], iot2[:], 0.0, None, ALU.max, r=[b_c], w=[b_c])
        for h in range(H):
            g.act(decq[:, 2 * h, :], iot[:], AF.Exp, r=[b_c], w=[b_c], scale=lg[h])
            g.act(decq[:, 2 * h + 1, :], iot[:], AF.Exp, r=[b_c], w=[b_c], scale=lg[h])
            g.act(dintra[:, h, :], iot2[:], AF.Exp, r=[b_c], w=[b_c], scale=lg[h])
            g.act(dk[:, h:h + 1], iot3[:], AF.Exp, r=[b_c], w=[b_c], scale=lg[h])
            g.affsel(dintra[:, h, :], dintra[:, h, :], [[1, 128]], ALU.is_ge, 0.0, 0, -1, r=[b_c], w=[b_c])
        g.ts('dve', dk[:], dk[:], 1.0 / 16.0, None, ALU.mult, r=[b_c], w=[b_c])
        hT = [k.sb(es, [128, 8, 128], BF16, 'hT')] * 2
        b_hT = [Buf()] * 2
        qT = k.sb(es, [128, 8, 128], BF16, 'qT')
        qdT = k.sb(es, [128, 8, 128], BF16, 'qdT')
        kT = k.sb(es, [128, 8, 128], BF16, 'kT')
        b_qT, b_qdT, b_kT = Buf(), Buf(), Buf()
        kdec = k.sb(es, [128, 1024], BF16, 'kdec')
        b_kdec = Buf()
        v = k.sb(es, [128, 2048], BF16, 'v')
        b_v = Buf()
        sg = k.sb(es, [128, 2048], BF16, 'sg')
        b_sg = Buf()
        stf = k.sb(es, [128, 8, 512], F32, 'stf')
        stb = k.sb(es, [128, 8, 512], BF16, 'stb')
        b_stf = [Buf() for _ in range(8)]
        b_stb = [Buf() for _ in range(8)]
        for i in range(8):
            g.memset('pool', stf[:, i, :], 0.0, w=[b_stf[i]])
            g.memset('pool', stb[:, i, :], 0.0, w=[b_stb[i]])
        sT = k.sb(es, [128, H, 128], BF16, 'sT')
        b_sT = Buf()
        onorm = k.sb(es, [128, 2048], BF16, 'onorm')
        b_on = Buf()
        oT = k.sb(es, [128, 16, 128], BF16, 'oT')
        b_oT = Buf()
        s6 = k.sb(es, [128, 6], F32, 's6')
        b_s6 = Buf()
        mv = k.sb(es, [128, H, 2], F32, 'mv')
        b_mv = Buf()
        hs = k.sb(es, [128, 3, H], F32, 'hs')
        b_hs = Buf()
        ontmp = c['tmp'][:, 0:256].bitcast(BF16)
        b_ontmp = c['b_tmp']

        g.mark('ret setup done')
        mixer_front(k, c, 0, xd, b_xd, hT[0][:], b_hT[0])
        for t in range(NT):
            hTt, bh = hT[t % 2], b_hT[t % 2]
            g.mark('ret tile %d' % t)
            for which in range(2):
                for half in range(2):
                    pt, pb = pp.next()
                    for cc in range(4):
                        col = which * 1024 + (half * 4 + cc) * 128
                        for ch in range(8):
                            g.mm(pt[:, cc * 128:(cc + 1) * 128], win[:, ch, col:col + 128], hTt[:, ch, :], ch == 0,
                                 ch == 7, r=[b_win[col // 512], bh], w=[pb])
                    dst = slice(half * 4, half * 4 + 4)
                    if which == 0:
                        g.cp('act', qT[:, dst, :].rearrange('p a b -> p (a b)'), pt[:], r=[pb], w=[b_qT])
                        g.tt('dve', qdT[:, dst, :].rearrange('p a b -> p (a b)'),
                             qT[:, dst, :].rearrange('p a b -> p (a b)'),
                             decq[:, dst, :].rearrange('p a b -> p (a b)'), ALU.mult,
                             r=[b_qT, b_c], w=[b_qdT])
                    else:
                        g.act(kT[:, dst, :].rearrange('p a b -> p (a b)'), pt[:], AF.Identity, r=[pb], w=[b_kT],
                              scale=1.0 / 16.0)
            g.mark('ret tokmajor')
            for blk in range(10):
                col = 1024 + blk * 512
                pt, pb = pp.next()
                for ch in range(8):
                    g.mm(pt[:], hTt[:, ch, :], win[:, ch, col:col + 512], ch == 0, ch == 7,
                         r=[b_win[col // 512], bh], w=[pb])
                if blk < 2:
                    for hh in range(2):
                        h = blk * 2 + hh
                        g.ts('dve', kdec[:, h * 256:(h + 1) * 256], pt[:, hh * 256:(hh + 1) * 256], dk[:, h:h + 1],
                             None, ALU.mult, r=[pb, b_c], w=[b_kdec])
                elif blk < 6:
                    o0 = (blk - 2) * 512
                    if blk % 2:
                        g.cp('act', v[:, o0:o0 + 512], pt[:], r=[pb], w=[b_v])
                    else:
                        g.cp('dve', v[:, o0:o0 + 512], pt[:], r=[pb], w=[b_v])
                else:
                    o0 = (blk - 6) * 512
                    g.act(sg[:, o0:o0 + 512], pt[:], AF.Silu, r=[pb], w=[b_sg])
            if t + 1 < NT:
                mixer_front(k, c, t + 1, xd, b_xd, hT[(t + 1) % 2][:], b_hT[(t + 1) % 2])
            g.mark('ret scores')
            pt, pb = pp.next()
            for h in range(H):
                for dc in range(2):
                    g.mm(pt[:, h * 128:(h + 1) * 128], kT[:, 2 * h + dc, :], qT[:, 2 * h + dc, :], dc == 0, dc == 1,
                         r=[b_kT, b_qT], w=[pb])
            g.tt('dve', sT[:].rearrange('p h t -> p (h t)'), pt[:], dintra[:].rearrange('p h t -> p (h t)'), ALU.mult,
                 r=[pb, b_c], w=[b_sT])
            g.mark('ret heads')
            o_ps = []
            for h in range(H):
                pt, pb = pp.next()
                g.mm(pt[:], sT[:, h, :], v[:, h * 512:(h + 1) * 512], True, False, r=[b_sT, b_v], w=[pb])
                for dc in range(2):
                    g.mm(pt[:], qdT[:, 2 * h + dc, :], stb[:, 2 * h + dc, :], False, dc == 1,
                         r=[b_qdT, b_stb[2 * h + dc]], w=[pb])
                g.op('dve', lambda hh, pt=pt: hh.bn_stats(s6[:], pt[:]), r=[pb], w=[b_s6])
                g.op('dve', lambda hh, h=h: hh.bn_aggr(mv[:, h, :], s6[:]), r=[b_s6], w=[b_mv])
                g.ts('dve', hs[:, 0, h:h + 1], mv[:, h, 1:2], EPS, None, ALU.add, r=[b_mv], w=[b_hs])
                g.act(hs[:, 1, h:h + 1], hs[:, 0, h:h + 1], AF.Sqrt, r=[b_hs], w=[b_hs])
                g.op('dve', lambda hh, h=h: hh.reciprocal(hs[:, 0, h:h + 1], hs[:, 1, h:h + 1]), r=[b_hs], w=[b_hs])
                g.stt('dve', hs[:, 2, h:h + 1], mv[:, h, 0:1], -1.0, hs[:, 0, h:h + 1], ALU.mult, ALU.mult,
                      r=[b_mv, b_hs], w=[b_hs])
                g.act(ontmp, pt[:], AF.Identity, r=[pb, b_hs], w=[b_ontmp], scale=hs[:, 0, h:h + 1],
                      bias=hs[:, 2, h:h + 1])
                g.tt('pool', onorm[:, h * 512:(h + 1) * 512], ontmp, sg[:, h * 512:(h + 1) * 512], ALU.mult,
                     r=[b_ontmp, b_sg], w=[b_on])
                for dc in range(2):
                    i = 2 * h + dc
                    pt2, pb2 = pp.next()
                    g.mm(pt2[:], kdec[:, h * 256 + dc * 128:h * 256 + (dc + 1) * 128], v[:, h * 512:(h + 1) * 512],
                         True, True, r=[b_kdec, b_v], w=[pb2])
                    g.stt('dve', stf[:, i, :], stf[:, i, :], math.exp(lg[h] * 128.0), pt2[:], ALU.mult, ALU.add,
                          r=[pb2], w=[b_stf[i]])
                    g.cp('act', stb[:, i, :], stf[:, i, :], r=[b_stf[i]], w=[b_stb[i]])
            g.mark('ret oT')
            for half in range(2):
                pt, pb = pp.next()
                ptb = pt[:].bitcast(BF16)
                for cc in range(8):
                    ec = half * 8 + cc
                    g.tr(ptb[:, cc * 128:(cc + 1) * 128], onorm[:, ec * 128:(ec + 1) * 128], k.ident_b[:],
                         r=[b_on, k.b_ident], w=[pb])
                g.tt('dve', oT[:, half * 8:(half + 1) * 8, :], ptb.rearrange('p (c t) -> p c t', c=8),
                     gncol[:, :, half].unsqueeze(2).to_broadcast([128, 8, 128]), ALU.mult, r=[pb, b_gncol], w=[b_oT])
            g.mark('ret outproj')
            for hf in range(2):
                for ec in range(16):
                    g.mm(c['ps_out'][:, hf * 512:(hf + 1) * 512], oT[:, ec, :], wout[:, ec, hf * 512:(hf + 1) * 512],
                         ec == 0, ec == 15, r=[b_oT, b_wout[hf]], w=[c['b_ps_out']])
            mixer_back(k, c, t, xd, b_xd)
        g.barrier()


def phase_lru(k, li, xd, b_xd, W):
    g, nc = k.g, k.nc
    NT = k.NT
    j = li // 4
    GT = 4 if NT % 4 == 0 else 1
    NG = NT // GT
    TG = GT * 128
    with contextlib.ExitStack() as es:
        win = k.sb(es, [128, 8, 2048], BF16, 'win')
        wout = k.sb(es, [128, 8, D], BF16, 'wout')
        b_win = load_w(k, win, W['lru_w_in'][j], 2048, 512)
        b_wout = load_w(k, wout, W['lru_w_out'][j], D, 512)
        wa = k.sb(es, [128, 8, 128], BF16, 'wa')
        wx = k.sb(es, [128, 8, 128], BF16, 'wx')
        b_wa, b_wx = Buf(), Buf()
        g.dma('pool', wa[:], W['lru_w_a'][j].rearrange('n c d -> c n d'), w=[b_wa])
        g.dma('pool', wx[:], W['lru_w_x'][j].rearrange('n c d -> c n d'), w=[b_wx])
        c = mixer_common(k, es, li, W, nxt=2 * GT)
        pp = c['pp']
        pt, pb = pp.next()
        cols, b_cols = load_cols(k, es, [W['lru_conv_w'][j], W['lru_conv_b'][j:j + 1, :], W['lru_b_a'][j:j + 1, :],
                                         W['lru_b_x'][j:j + 1, :], W['lru_lambda'][j:j + 1, :]], 8, pt, pb, 'lcols')
        c8 = k.sb(es, [128, 8], F32, 'c8')
        c8t = k.sb(es, [128, 8], F32, 'c8t')
        b_c8 = Buf()
        g.act(c8t[:], cols[:, :, 7], AF.Exp, r=[b_cols], w=[b_c8], scale=-1.0)
        g.ts('dve', c8t[:], c8t[:], 1.0, None, ALU.add, r=[b_c8], w=[b_c8])
        g.act(c8[:], c8t[:], AF.Ln, r=[b_c8], w=[b_c8])
        g.ts('dve', c8[:], c8[:], -8.0, None, ALU.mult, r=[b_c8], w=[b_c8])
        hT = k.sb(es, [128, 8, TG], BF16, 'hT')
        b_hT = [Buf() for _ in range(GT)]
        xbuf = k.sb(es, [128, 8, TG + 3], F32, 'xbuf')
        b_xbuf = [Buf() for _ in range(8)]
        hlast = k.sb(es, [128, 8], F32, 'hlast')
        b_hl = [Buf() for _ in range(8)]
        for n in range(8):
            g.memset('pool', xbuf[:, n, 0:3], 0.0, w=[b_xbuf[n]])
            g.memset('pool', hlast[:, n:n + 1], 0.0, w=[b_hl[n]])
        hyT = k.sb(es, [128, 8, TG], BF16, 'hyT')
        b_hy = [Buf() for _ in range(8)]

        def T(name, dt=F32):
            return [k.sb(es, [128, TG], dt, name) for _ in range(2)], [Buf() for _ in range(2)]
        xc, b_xc = T('xc')
        xcb, b_xcb = T('xcb', BF16)
        ysb, b_ysb = T('ysb')
        u, b_u = T('u')
        sgm, b_sgm = T('sgm')
        y, b_y = T('y')
        gr, b_gr = T('gr')
        gi, b_gi = T('gi')
        a, b_a = T('a')
        a2, b_a2 = T('a2')
        uu, b_uu = T('uu')
        hs, b_hs = T('hs')
        for gi_ in range(NG):
            for tl in range(GT):
                t = gi_ * GT + tl
                mixer_front(k, c, t, xd, b_xd, hT[:, :, tl * 128:(tl + 1) * 128], b_hT[tl])
            for n in range(8):
                s2 = n % 2
                pt, pb = pp.next()
                for ch in range(8):
                    g.mm(pt[:, 0:TG], win[:, ch, n * 128:(n + 1) * 128], hT[:, ch, :], ch == 0, ch == 7,
                         r=[b_win[(n * 128) // 512]] + b_hT, w=[pb])
                g.cp('act', xbuf[:, n, 3:3 + TG], pt[:, 0:TG], r=[pb], w=[b_xbuf[n]])
                g.ts('dve', xc[s2][:], xbuf[:, n, 0:TG], cols[:, n, 0:1], cols[:, n, 4:5], ALU.mult, ALU.add,
                     r=[b_xbuf[n], b_cols], w=[b_xc[s2]])
                for tap in range(1, 4):
                    g.stt('dve', xc[s2][:], xbuf[:, n, tap:tap + TG], cols[:, n, tap:tap + 1],
                          xc[s2][:], ALU.mult, ALU.add, r=[b_xbuf[n], b_cols], w=[b_xc[s2]])
                g.cp('pool', xbuf[:, n, 0:3], xbuf[:, n, TG:TG + 3], r=[], w=[b_xbuf[n]])
                g.cp('act', xcb[s2][:], xc[s2][:], r=[b_xc[s2]], w=[b_xcb[s2]])
                pt2, pb2 = pp.next()
                for ch in range(8):
                    g.mm(pt2[:, 0:TG], win[:, ch, 1024 + n * 128:1024 + (n + 1) * 128], hT[:, ch, :], ch == 0, ch == 7,
                         r=[b_win[(1024 + n * 128) // 512]] + b_hT, w=[pb2])
                g.cp('act', ysb[s2][:], pt2[:, 0:TG], r=[pb2], w=[b_ysb[s2]])
                g.act(u[s2][:], pt2[:, 0:TG], AF.Square, r=[pb2], w=[b_u[s2]])
                g.ts('dve', u[s2][:], u[s2][:], 0.044715, 1.0, ALU.mult, ALU.add, r=[], w=[b_u[s2]])
                g.tt('dve', u[s2][:], u[s2][:], ysb[s2][:], ALU.mult, r=[b_ysb[s2]], w=[b_u[s2]])
                g.act(sgm[s2][:], u[s2][:], AF.Sigmoid, r=[b_u[s2]], w=[b_sgm[s2]], scale=1.5957691216057308)
                g.tt('pool', y[s2][:], sgm[s2][:], ysb[s2][:], ALU.mult, r=[b_sgm[s2], b_ysb[s2]], w=[b_y[s2]])
                pt3, pb3 = pp.next()
                g.mm(pt3[:, 0:TG], wa[:, n, :], xcb[s2][:], True, True, r=[b_wa, b_xcb[s2]], w=[pb3])
                g.act(gr[s2][:], pt3[:, 0:TG], AF.Sigmoid, r=[pb3, b_cols], w=[b_gr[s2]], bias=cols[:, n, 5:6])
                pt4, pb4 = pp.next()
                g.mm(pt4[:, 0:TG], wx[:, n, :], xcb[s2][:], True, True, r=[b_wx, b_xcb[s2]], w=[pb4])
                g.act(gi[s2][:], pt4[:, 0:TG], AF.Sigmoid, r=[pb4, b_cols], w=[b_gi[s2]], bias=cols[:, n, 6:7])
                g.act(a[s2][:], gr[s2][:], AF.Exp, r=[b_gr[s2], b_c8], w=[b_a[s2]], scale=c8[:, n:n + 1])
                g.tt('pool', a2[s2][:], a[s2][:], a[s2][:], ALU.mult, r=[b_a[s2]], w=[b_a2[s2]])
                g.ts('dve', a2[s2][:], a2[s2][:], -1.0, 1.0, ALU.mult, ALU.add, r=[], w=[b_a2[s2]])
                g.act(a2[s2][:], a2[s2][:], AF.Sqrt, r=[], w=[b_a2[s2]])
                g.tt('dve', uu[s2][:], a2[s2][:], gi[s2][:], ALU.mult, r=[b_a2[s2], b_gi[s2]], w=[b_uu[s2]])
                g.tt('pool', uu[s2][:], uu[s2][:], xc[s2][:], ALU.mult, r=[b_xc[s2]], w=[b_uu[s2]])
                g.op('dve', lambda h, o=hs[s2][:], d0=a[s2][:], d1=uu[s2][:], ini=hlast[:, n:n + 1]:
                     h.tensor_tensor_scan(o, d0, d1, ini, ALU.mult, ALU.add),
                     r=[b_a[s2], b_uu[s2], b_hl[n]], w=[b_hs[s2]])
                g.cp('pool', hlast[:, n:n + 1], hs[s2][:, TG - 1:TG], r=[b_hs[s2]], w=[b_hl[n]])
                g.tt('dve' if n % 2 else 'pool', hyT[:, n, :], hs[s2][:], y[s2][:], ALU.mult,
                     r=[b_hs[s2], b_y[s2]], w=[b_hy[n]])
            for tl in range(GT):
                t = gi_ * GT + tl
                for hf in range(2):
                    for n in range(8):
                        g.mm(c['ps_out'][:, hf * 512:(hf + 1) * 512], hyT[:, n, tl * 128:(tl + 1) * 128],
                             wout[:, n, hf * 512:(hf + 1) * 512], n == 0, n == 7, r=[b_hy[n], b_wout[hf]],
                             w=[c['b_ps_out']])
                mixer_back(k, c, t, xd, b_xd)
        g.barrier()


def phase_gla(k, li, xd, b_xd, W):
    g, nc = k.g, k.nc
    NT = k.NT
    j = li // 4
    H, DK, DV = 4, 128, 256
    with contextlib.ExitStack() as es:
        win = k.sb(es, [128, 8, 3088], BF16, 'win')
        wout = k.sb(es, [128, 8, D], BF16, 'wout')
        b_win = load_w(k, win, W['gla_w_in'][j], 3088, 512)
        b_wout = load_w(k, wout, W['gla_w_out'][j], D, 512)
        wup = k.sb(es, [16, 512], F32, 'wup')
        bgate = k.sb(es, [1, 512], F32, 'bgate')
        b_wup = Buf()
        g.dma('sp', wup[:], W['gla_w_gate_up'][j], w=[b_wup])
        g.dma('sp', bgate[:], W['gla_b_gate'][j:j + 1, :], w=[b_wup])
        c = mixer_common(k, es, li, W)
        pp = c['pp']
        pt, pb = pp.next()
        gncol, b_gncol = load_cols(k, es, W['gla_gn'][j:j + 1, :], 1, pt, pb, 'gncol')
        UT = k.sb(es, [128, 128], F32, 'UT')
        VT = k.sb(es, [128, 128], F32, 'VT')
        mk = k.sb(es, [128, H, 128], F32, 'mk')
        b_c = Buf()
        g.memset('pool', UT[:], -1.0 / 16.0, w=[b_c])
        g.memset('pool', VT[:], -1.0 / 16.0, w=[b_c])
        g.memset('pool', mk[:], 1.0, w=[b_c])
        g.affsel(UT[:], UT[:], [[1, 128]], ALU.is_ge, 0.0, 0, -1, r=[b_c], w=[b_c])
        g.affsel(VT[:], VT[:], [[-1, 128]], ALU.is_gt, 0.0, 0, 1, r=[b_c], w=[b_c])
        for h in range(H):
            g.affsel(mk[:, h, :], mk[:, h, :], [[1, 128]], ALU.is_ge, 0.0, 0, -1, r=[b_c], w=[b_c])
        hT = k.sb(es, [128, 8, 128], BF16, 'hT')
        b_hT = Buf()
        glT = k.sb(es, [16, 128], F32, 'glT')
        b_glT = Buf()
        ez = k.sb(es, [128, 512], F32, 'ez')
        lsp = k.sb(es, [128, 512], F32, 'lsp')
        b_ez, b_lsp = Buf(), Buf()
        E1 = k.sb(es, [128, 512], F32, 'E1')
        E2 = k.sb(es, [128, 512], F32, 'E2')
        E3 = k.sb(es, [128, 512], F32, 'E3')
        b_E1, b_E2, b_E3 = Buf(), Buf(), Buf()
        qtT = k.sb(es, [128, H, 128], BF16, 'qtT')
        ktT = k.sb(es, [128, H, 128], BF16, 'ktT')
        khat = k.sb(es, [128, 512], BF16, 'khat')
        b_qtT, b_ktT, b_khat = Buf(), Buf(), Buf()
        v = k.sb(es, [128, 1024], BF16, 'v')
        sr = k.sb(es, [128, 1024], BF16, 'sr')
        b_v, b_sr = Buf(), Buf()
        AT = k.sb(es, [128, H, 128], BF16, 'AT')
        b_AT = Buf()
        stf = k.sb(es, [128, H, DV], F32, 'stf')
        stb = k.sb(es, [128, H, DV], BF16, 'stb')
        b_stf = [Buf() for _ in range(H)]
        b_stb = [Buf() for _ in range(H)]
        for h in range(H):
            g.memset('pool', stf[:, h, :], 0.0, w=[b_stf[h]])
            g.memset('pool', stb[:, h, :], 0.0, w=[b_stb[h]])
        ss = k.sb(es, [128, 3, H], F32, 'ss')
        b_ss = Buf()
        ontmp = c['tmp'][:, 0:128].bitcast(BF16)
        b_ontmp = c['b_tmp']
        onorm = k.sb(es, [128, 1024], BF16, 'onorm')
        b_on = Buf()
        oT = k.sb(es, [128, 8, 128], BF16, 'oT')
        b_oT = Buf()
        sq_scr = k.sb(es, [128, 256], BF16, 'sqscr')

        mixer_front(k, c, 0, xd, b_xd, hT[:], b_hT)
        for t in range(NT):
            pt_gl, pb_gl = pp.next()
            for ch in range(8):
                g.mm(pt_gl[0:16, 0:128], win[:, ch, 3072:3088], hT[:, ch, :], ch == 0, ch == 7, r=[b_win[6], b_hT],
                     w=[pb_gl])
            g.cp('act', glT[:], pt_gl[0:16, 0:128], r=[pb_gl], w=[b_glT])
            pt_z, pb_z = pp.next()
            g.mm(pt_z[:], glT[:], wup[:], True, False, r=[b_glT, b_wup], w=[pb_z])
            g.mm(pt_z[:], k.ones_f[0:1, :], bgate[:], False, True, r=[k.b_ones, b_wup], w=[pb_z])
            g.act(ez[:], pt_z[:], AF.Exp, r=[pb_z], w=[b_ez], scale=-1.0)
            g.act(lsp[:], ez[:], AF.Ln, r=[b_ez], w=[b_lsp], bias=1.0)
            pt_c, pb_c = pp.next()
            g.mm(pt_c[:], VT[:], lsp[:], True, True, r=[b_c, b_lsp], w=[pb_c])
            g.act(E3[:], pt_c[:], AF.Exp, r=[pb_c], w=[b_E3])
            pt_b, pb_b = pp.next()
            for h in range(H):
                g.mm(pt_b[:, h * 128:(h + 1) * 128], lsp[:, h * 128:(h + 1) * 128], UT[:], True, True,
                     r=[b_c, b_lsp], w=[pb_b])
            g.act(E1[:], pt_b[:], AF.Exp, r=[pb_b], w=[b_E1])
            g.act(E2[:], pt_b[:], AF.Exp, r=[pb_b], w=[b_E2], scale=-1.0)
            for which in range(2):
                pt, pb = pp.next()
                for cc in range(4):
                    col = which * 512 + cc * 128
                    for ch in range(8):
                        g.mm(pt[:, cc * 128:(cc + 1) * 128], win[:, ch, col:col + 128], hT[:, ch, :], ch == 0, ch == 7,
                             r=[b_win[col // 512], b_hT], w=[pb])
                if which == 0:
                    g.stt('dve', qtT[:].rearrange('p h t -> p (h t)'), pt[:], DK ** -0.5, E1[:], ALU.mult, ALU.mult,
                          r=[pb, b_E1], w=[b_qtT])
                else:
                    g.tt('dve', ktT[:].rearrange('p h t -> p (h t)'), pt[:], E2[:], ALU.mult, r=[pb, b_E2],
                         w=[b_ktT])
            pt, pb = pp.next()
            for ch in range(8):
                g.mm(pt[:], hT[:, ch, :], win[:, ch, 512:1024], ch == 0, ch == 7, r=[b_win[1], b_hT], w=[pb])
            g.tt('dve', khat[:], pt[:], E3[:], ALU.mult, r=[pb, b_E3], w=[b_khat])
            for blk in range(4):
                col = 1024 + blk * 512
                pt, pb = pp.next()
                for ch in range(8):
                    g.mm(pt[:], hT[:, ch, :], win[:, ch, col:col + 512], ch == 0, ch == 7,
                         r=[b_win[col // 512], b_hT], w=[pb])
                if blk < 2:
                    g.cp('act' if blk else 'dve', v[:, blk * 512:(blk + 1) * 512], pt[:], r=[pb], w=[b_v])
                else:
                    g.act(sr[:, (blk - 2) * 512:(blk - 1) * 512], pt[:], AF.Silu, r=[pb], w=[b_sr])
            if t + 1 < NT:
                mixer_front(k, c, t + 1, xd, b_xd, hT[:], b_hT)
            pt, pb = pp.next()
            for h in range(H):
                g.mm(pt[:, h * 128:(h + 1) * 128], ktT[:, h, :], qtT[:, h, :], True, True, r=[b_ktT, b_qtT], w=[pb])
            g.tt('dve', AT[:].rearrange('p h t -> p (h t)'), pt[:], mk[:].rearrange('p h t -> p (h t)'), ALU.mult,
                 r=[pb, b_c], w=[b_AT])
            o_banks = [pp.next(), pp.next()]
            for h in range(H):
                pt, pb = o_banks[h // 2]
                osl = pt[:, (h % 2) * 256:(h % 2 + 1) * 256]
                g.mm(osl, AT[:, h, :], v[:, h * 256:(h + 1) * 256], True, False, r=[b_AT, b_v], w=[pb])
                g.mm(osl, qtT[:, h, :], stb[:, h, :], False, True, r=[b_qtT, b_stb[h]], w=[pb])
                g.act(sq_scr[:], osl, AF.Square, r=[pb], w=[b_ss], accum_out=ss[:, 0, h:h + 1])
            g.ts('dve', ss[:, 1, :], ss[:, 0, :], 1.0 / DV, EPS, ALU.mult, ALU.add, r=[b_ss], w=[b_ss])
            g.act(ss[:, 2, :], ss[:, 1, :], AF.Sqrt, r=[b_ss], w=[b_ss])
            g.op('dve', lambda hh: hh.reciprocal(ss[:, 1, :], ss[:, 2, :]), r=[b_ss], w=[b_ss])
            for h in range(H):
                pt, pb = o_banks[h // 2]
                osl = pt[:, (h % 2) * 256:(h % 2 + 1) * 256]
                g.act(ontmp, osl, AF.Identity, r=[pb, b_ss], w=[b_ontmp], scale=ss[:, 1, h:h + 1])
                g.tt('pool', onorm[:, h * 256:(h + 1) * 256], ontmp, sr[:, h * 256:(h + 1) * 256], ALU.mult,
                     r=[b_ontmp, b_sr], w=[b_on])
                pt2, pb2 = pp.next()
                g.mm(pt2[:, 0:256], khat[:, h * 128:(h + 1) * 128], v[:, h * 256:(h + 1) * 256], True, True,
                     r=[b_khat, b_v], w=[pb2])
                g.stt('dve', stf[:, h, :], stf[:, h, :], E1[:, h * 128 + 127:h * 128 + 128], pt2[:, 0:256], ALU.mult,
                      ALU.add, r=[pb2, b_E1], w=[b_stf[h]])
                g.cp('act', stb[:, h, :], stf[:, h, :], r=[b_stf[h]], w=[b_stb[h]])
            pt, pb = pp.next()
            ptb = pt[:].bitcast(BF16)
            for cc in range(8):
                g.tr(ptb[:, cc * 128:(cc + 1) * 128], onorm[:, cc * 128:(cc + 1) * 128], k.ident_b[:],
                     r=[b_on, k.b_ident], w=[pb])
            g.tt('dve', oT[:], ptb.rearrange('p (c t) -> p c t', c=8),
                 gncol[:, :, 0].unsqueeze(2).to_broadcast([128, 8, 128]), ALU.mult, r=[pb, b_gncol], w=[b_oT])
            for hf in range(2):
                for ec in range(8):
                    g.mm(c['ps_out'][:, hf * 512:(hf + 1) * 512], oT[:, ec, :], wout[:, ec, hf * 512:(hf + 1) * 512],
                         ec == 0, ec == 7, r=[b_oT, b_wout[hf]], w=[c['b_ps_out']])
            mixer_back(k, c, t, xd, b_xd)
        g.barrier()


def bf16_round(x):
    u = np.ascontiguousarray(x, dtype=np.float32).view(np.uint32).astype(np.uint64)
    r = ((u + 0x7FFF + ((u >> 16) & 1)) & 0xFFFF0000).astype(np.uint32)
    return r.view(np.float32)


def hilo(x):
    hi = bf16_round(x)
    lo = bf16_round(np.asarray(x, dtype=np.float32) - hi)
    return hi, lo


def nsa_tables(NT):
    H = 16
    slopes = np.exp2(-8.0 * (np.arange(H, dtype=np.float64) + 1.0) / H).astype(np.float32)
    ip = np.arange(128, dtype=np.float32)
    qA = np.zeros((6, H, 128), np.float32)
    for h in range(H):
        a, b = hilo(np.full(128, -8.0 * 128.0 * slopes[h], np.float32))
        qA[0, h], qA[1, h] = a, b
        a, b = hilo(-8.0 * slopes[h] * ip)
        qA[2, h], qA[3, h] = a, b
        a, b = hilo(np.full(128, 8.0 * slopes[h], np.float32))
        qA[4, h], qA[5, h] = a, b
    kA = np.zeros((6, 33, 128), np.float32)
    for d in range(33):
        kA[0, d] = kA[1, d] = d
        kA[2, d] = kA[3, d] = 1.0
        kA[4, d] = kA[5, d] = ip
    s_ = np.arange(256)
    ce = 16 * s_ + 15
    b_, a_ = ce // 128, ce % 128
    kAc = np.zeros((6, NT, 256), np.float32)
    for T in range(NT):
        kAc[0, T] = kAc[1, T] = np.where(b_ > T, 64, T - b_)
        kAc[2, T] = kAc[3, T] = 1.0
        kAc[4, T] = kAc[5, T] = a_
    n_ = s_ - 1
    j_ = np.arange(64)
    ovl = ((16 * n_[:, None] < 64 * j_[None, :] + 64) & (16 * n_[:, None] + 32 > 64 * j_[None, :]) &
           (n_[:, None] >= 0)).astype(np.float32)
    return {'c_qA': qA, 'c_kA': kA, 'c_kAc': kAc, 'c_ovl': ovl}


def phase_nsa(k, li, xd, b_xd, W):
    g, nc = k.g, k.nc
    NT = k.NT
    S = k.S
    j = li // 4
    G, HPG, DK = 2, 8, 64
    with contextlib.ExitStack() as es:
        win = k.sb(es, [128, 8, 1840], BF16, 'win')
        wout = k.sb(es, [128, 8, D], BF16, 'wout')
        b_win = load_w(k, win, W['nsa_w_in'][j], 1840, 460)
        b_wout = load_w(k, wout, W['nsa_w_out'][j], D, 512)
        bw_all = b_win
        w1 = [k.sb(es, [64, 32, 128], BF16, 'w1') for _ in range(2)]
        w2 = [k.sb(es, [128, 64], BF16, 'w2') for _ in range(2)]
        pe = [k.sb(es, [32, 64], F32, 'pe') for _ in range(2)]
        b_w1 = Buf()
        for i, nm in enumerate(['k', 'v']):
            g.dma('pool', w1[i][:], W['nsa_w1_' + nm][j].rearrange('(l d) f -> d l f', d=64), w=[b_w1])
            g.dma('pool', w2[i][:], W['nsa_w2_' + nm][j], w=[b_w1])
            g.dma('sp', pe[i][:], W['nsa_pe_' + nm][j], w=[b_w1])
        qA = k.sb(es, [6, 16 * 128], BF16, 'qA')
        kA = k.sb(es, [6, 33 * 128], BF16, 'kA')
        kAc = k.sb(es, [6, NT * 256], BF16, 'kAc')
        ovl = k.sb(es, [128, 2, 64], BF16, 'ovl')
        b_tab = Buf()
        g.dma('pool', qA[:], W['c_qA'].rearrange('r h i -> r (h i)'), w=[b_tab])
        g.dma('pool', kA[:], W['c_kA'].rearrange('r d i -> r (d i)'), w=[b_tab])
        g.dma('pool', kAc[:], W['c_kAc'].rearrange('r t s -> r (t s)'), w=[b_tab])
        g.dma('pool', ovl[:], W['c_ovl'].rearrange('(n p) j -> p n j', p=128), w=[b_tab])
        c = mixer_common(k, es, li, W, npool=4)
        pp = c['pp']
        tri4 = k.sb(es, [128, 512], F32, 'tri4')
        triu4 = k.sb(es, [128, 512], F32, 'triu4')
        Ex = k.sb(es, [64, NT, 128], BF16, 'Ex')
        zrow = k.sb(es, [1, 512], BF16, 'zrow')
        b_c = Buf()
        g.memset('pool', tri4[:], 1.0, w=[b_c])
        g.memset('pool', triu4[:], 1.0, w=[b_c])
        g.memset('pool', Ex[:], 1.0, w=[b_c])
        g.memset('pool', zrow[:], 0.0, w=[b_c])
        g.affsel(tri4[:], tri4[:], [[0, 4], [1, 128]], ALU.is_ge, 0.0, 0, -1, r=[b_c], w=[b_c])
        g.affsel(triu4[:], triu4[:], [[0, 4], [-1, 128]], ALU.is_gt, 0.0, 0, 1, r=[b_c], w=[b_c])
        for hf in range(2):
            g.affsel(Ex[:, :, hf * 64:(hf + 1) * 64], Ex[:, :, hf * 64:(hf + 1) * 64],
                                                            [[-2, NT], [0, 64]], ALU.is_equal, 0.0, -hf, 1, r=[b_c], w=[b_c])
        Mw = k.sb(es, [128, 2304], BF16, 'Mw')
        Vw = k.sb(es, [128, 128], F32, 'Vw')
        Aw = k.sb(es, [128, 128], F32, 'Aw')
        g.memset('pool', Mw[:], 1.0, w=[b_c])
        g.memset('pool', Vw[:], 1.0, w=[b_c])
        g.memset('pool', Aw[:], 0.0, w=[b_c])
        g.affsel(Mw[:], Mw[:], [[1, 2304]], ALU.is_ge, 0.0, -15, -16, r=[b_c], w=[b_c])
        for hf in range(2):
            rows = slice(hf * 64, (hf + 1) * 64)
            g.affsel(Vw[rows, :], Vw[rows, :], [[-1, 128]], ALU.is_ge, 0.0, 64 + hf, 0, r=[b_c], w=[b_c])
            g.affsel(Aw[rows, :], Aw[rows, :], [[-1, 128]], ALU.is_ge, -1.0, 64 + hf, 0, r=[b_c], w=[b_c])
            g.affsel(Aw[rows, :], Aw[rows, :], [[1, 128]], ALU.not_equal, 1e6, -(64 + hf), 0, r=[b_c], w=[b_c])
            g.affsel(Aw[rows, :], Aw[rows, :], [[1, 128]], ALU.not_equal, 1e6, -(63 + hf), 0, r=[b_c], w=[b_c])
        kcT = [[k.sb(es, [64, 160], BF16, 'kcT') for _ in range(G)] for _ in range(2)]
        b_kc = Buf()
        for i in range(2):
            for gg in range(G):
                g.memset('pool', kcT[i][gg][:], 0.0, w=[b_kc])
        ksT = [k.sb(es, [64, S], BF16, 'ksT') for _ in range(G)]
        kwT = [k.sb(es, [64, 5 * 128], BF16, 'kwT') for _ in range(G)]
        b_kw = [Buf() for _ in range(5)]
        b_ks = [Buf() for _ in range(NT)]
        vs = k.sb(es, [128, NT, G, 65], BF16, 'vs')
        vw = k.sb(es, [128, 5, G, 65], BF16, 'vw')
        b_vs = [Buf() for _ in range(NT)]
        b_v1 = Buf()
        g.memset('pool', vs[:], 1.0, w=[b_v1])
        g.memset('pool', vw[:], 1.0, w=[b_v1])
        hidT = [[k.sb(es, [128, 256], BF16, 'hidT') for _ in range(G)] for _ in range(2)]
        b_hid = Buf()
        for i in range(2):
            for gg in range(G):
                g.memset('pool', hidT[i][gg][:], 0.0, w=[b_hid])
        kcmpT = [k.sb(es, [64, 256], BF16, 'kcmpT') for _ in range(G)]
        b_kcmp = Buf()
        for gg in range(G):
            g.memset('pool', kcmpT[gg][:], 0.0, w=[b_kcmp])
        vcmp = k.sb(es, [128, 2, G, 65], BF16, 'vcmp')
        b_vcmp = Buf()
        g.memset('pool', vcmp[:], 1.0, w=[b_vcmp])
        c1 = k.sb(es, [128, 2], F32, 'c1')
        peT = k.sb(es, [64, 2, 32], BF16, 'peT')
        b_c1 = Buf()
        for i in range(2):
            pt, pb = pp.next()
            g.tr(pt[0:64, 0:32], pe[i][:], k.ident_f[0:32, 0:32], r=[b_w1, k.b_ident], w=[pb])
            g.cp('dve', peT[:, i, :], pt[0:64, 0:32], r=[pb], w=[b_c1])
        for i in range(2):
            pt, pb = pp.next()
            for l in range(32):
                g.mm(pt[:, 0:1], w1[i][:, l, :], peT[:, i, l:l + 1], l == 0, l == 31, r=[b_w1, b_c1], w=[pb])
            g.cp('dve', c1[:, i:i + 1], pt[:, 0:1], r=[pb], w=[b_c1])
        hT = k.sb(es, [128, 8, 128], BF16, 'hT')
        b_hT = Buf()
        qT = [k.sb(es, [64, 8, 128], BF16, 'qT') for _ in range(G)]
        b_qT = Buf()
        gts = k.sb(es, [128, 48], F32, 'gts')
        b_gts = Buf()
        hsm = [k.sb(es, [128, 8], F32, 'hsm') for _ in range(5)]
        b_hsm = Buf()
        mask4 = [k.sb(es, [128, 512], BF16, 'mask4') for _ in range(2)]
        b_m4 = Buf()
        e16 = [k.sb(es, [128, 512], BF16, 'e16') for _ in range(2)]
        b_e16 = [Buf() for _ in range(2)]
        e32 = [k.sb(es, [128, 512], F32, 'e32') for _ in range(2)]
        b_e32 = [Buf() for _ in range(2)]
        pc = [k.sb(es, [128, 8, 128], BF16, 'pc') for _ in range(2)]
        b_pc = Buf()
        pT = [k.sb(es, [128, 512], BF16, 'pT') for _ in range(3)]
        b_pT = [Buf() for _ in range(3)]
        imp = k.sb(es, [128, 64], F32, 'imp')
        imp2 = k.sb(es, [128, 64], F32, 'imp2')
        m8 = k.sb(es, [128, 16], F32, 'm8')
        selm = k.sb(es, [128, 64], F32, 'selm')
        b_imp = Buf()
        selT4 = k.sb(es, [64, 4, 128], BF16, 'selT4')
        b_selT = Buf()
        rec = k.sb(es, [128, 3, 8], F32, 'rec')
        b_rec = Buf()
        og = k.sb(es, [128, 1024], F32, 'og')
        b_og = Buf()
        ogb = k.sb(es, [128, 1024], BF16, 'ogb')
        b_ogb = Buf()
        oT = k.sb(es, [128, 8, 128], BF16, 'oT')
        b_oT = Buf()
        acc = [k.ps(es, [128, 512], F32, 'acc') for _ in range(2)]
        b_acc = [Buf(True) for _ in range(2)]
        npt = [0]

        def zero_bank(t_, b_):
            g.mm(t_[:], zrow[0:1, 0:128], zrow[0:1, :], True, False, r=[b_c], w=[b_], skip=True)

        def gelu_small(src_ps, bias_ap, dst, n, rlist, wbuf):
            xs, us, ss_ = hsm[0], hsm[1], hsm[2]
            g.act(xs[:, 0:n], src_ps, AF.Identity, r=rlist + [b_c1], w=[b_hsm], bias=bias_ap)
            g.tt('dve', us[:, 0:n], xs[:, 0:n], xs[:, 0:n], ALU.mult, r=[b_hsm], w=[b_hsm])
            g.ts('dve', us[:, 0:n], us[:, 0:n], 0.044715, 1.0, ALU.mult, ALU.add, r=[b_hsm], w=[b_hsm])
            g.tt('dve', us[:, 0:n], us[:, 0:n], xs[:, 0:n], ALU.mult, r=[b_hsm], w=[b_hsm])
            g.act(ss_[:, 0:n], us[:, 0:n], AF.Sigmoid, r=[b_hsm], w=[b_hsm], scale=1.5957691216057308)
            g.tt('dve', dst, ss_[:, 0:n], xs[:, 0:n], ALU.mult, r=[b_hsm], w=[wbuf])

        def qk_exp(T, kt, gg, half, k_ap, b_kcache):
            pt, pb = pp.next()
            hs0 = gg * 8 + half * 4
            g.mm(pt[:], k_ap, qT[gg][:, half * 4:(half + 1) * 4, :].rearrange('p a b -> p (a b)'),
                 True, False, r=[b_kcache, b_qT], w=[pb])
            d = T - kt
            g.mm(pt[:], kA[:, d * 128:(d + 1) * 128], qA[:, hs0 * 128:(hs0 + 4) * 128], False, True, r=[b_tab], w=[pb])
            return pt, pb

        mixer_front(k, c, 0, xd, b_xd, hT[:], b_hT)
        for T in range(NT):
            g.mark('nsa tile %d' % T)
            for gg in range(G):
                for half in range(2):
                    pt, pb = pp.next()
                    for hh in range(4):
                        h = gg * 8 + half * 4 + hh
                        for ch in range(8):
                            g.mm(pt[0:64, hh * 128:(hh + 1) * 128], win[:, ch, h * 64:(h + 1) * 64], hT[:, ch, :], ch == 0,
                                 ch == 7, r=bw_all + [b_hT], w=[pb])
                    g.cp('act' if half else 'dve', qT[gg][:, half * 4:(half + 1) * 4, :].rearrange('p a b -> p (a b)'),
                         pt[0:64, :], r=[pb], w=[b_qT])
            for idx, (col0, dst, off) in enumerate([(1024, kcT[0], 16), (1152, kcT[1], 16), (1280, ksT, 0), (1536, kwT, 0)]):
                pt, pb = pp.next()
                for gg in range(G):
                    for ch in range(8):
                        g.mm(pt[0:64, gg * 128:(gg + 1) * 128], win[:, ch, col0 + gg * 64:col0 + (gg + 1) * 64], hT[:, ch, :],
                             ch == 0, ch == 7, r=bw_all + [b_hT], w=[pb])
                for gg in range(G):
                    if idx < 2:
                        wb, c0 = b_kc, 16
                    elif idx == 2:
                        wb, c0 = b_ks[T], T * 128
                    else:
                        wb, c0 = b_kw[T % 5], (T % 5) * 128
                    g.cp('act' if gg else 'dve', dst[gg][:, c0:c0 + 128],
                         pt[0:64, gg * 128:(gg + 1) * 128], r=[pb], w=[wb])
            pt, pb = pp.next()
            for ch in range(8):
                g.mm(pt[:, 0:128], hT[:, ch, :], win[:, ch, 1408:1536], ch == 0, ch == 7, r=bw_all + [b_hT], w=[pb])
            for ch in range(8):
                g.mm(pt[:, 128:256], hT[:, ch, :], win[:, ch, 1664:1792], ch == 0, ch == 7, r=bw_all + [b_hT], w=[pb])
            for ch in range(8):
                g.mm(pt[:, 256:304], hT[:, ch, :], win[:, ch, 1792:1840], ch == 0, ch == 7, r=bw_all + [b_hT], w=[pb])
            for gg in range(G):
                g.cp('dve', vs[:, T, gg, 0:64], pt[:, gg * 64:(gg + 1) * 64], r=[pb, b_v1], w=[b_vs[T]])
                g.cp('act', vw[:, T % 5, gg, 0:64], pt[:, 128 + gg * 64:128 + (gg + 1) * 64], r=[pb, b_v1], w=[b_kw[T % 5]])
            g.act(gts[:], pt[:, 256:304], AF.Sigmoid, r=[pb], w=[b_gts])
            if T + 1 < NT:
                mixer_front(k, c, T + 1, xd, b_xd, hT[:], b_hT)
            for i in range(2):
                for gg in range(G):
                    pt, pb = pp.next()
                    for l in range(32):
                        rhs = kcT[i][gg][:, l:l + 128].rearrange('p (m s) -> p m s', s=16)[:, :, 0]
                        g.mm(pt[:, 0:8], w1[i][:, l, :], rhs, l == 0, l == 31, r=[b_w1, b_kc], w=[pb])
                    gelu_small(pt[:, 0:8], c1[:, i:i + 1], hidT[i][gg][:, 8 * T:8 * T + 8], 8, [pb], b_hid)
            for i in range(2):
                for gg in range(G):
                    g.cp('dve', kcT[i][gg][:, 0:16], kcT[i][gg][:, 128:144], r=[], w=[b_kc])
            for gg in range(G):
                pt, pb = pp.next()
                g.mm(pt[0:64, 0:8], w2[0][:], hidT[0][gg][:, 8 * T:8 * T + 8], True, True, r=[b_w1, b_hid], w=[pb])
                g.cp('dve', kcmpT[gg][:, 8 * T:8 * T + 8], pt[0:64, 0:8], r=[pb], w=[b_kcmp])
            ntl = [0] if 8 * T + 7 < 128 else [0, 1]
            pt, pb = pp.next()
            for nt_ in ntl:
                for gg in range(G):
                    sl = pt[:, (nt_ * 2 + gg) * 64:(nt_ * 2 + gg + 1) * 64]
                    g.mm(sl, hidT[1][gg][:, nt_ * 128:(nt_ + 1) * 128], w2[1][:], True, True, r=[b_w1, b_hid], w=[pb])
            for nt_ in ntl:
                for gg in range(G):
                    sl = pt[:, (nt_ * 2 + gg) * 64:(nt_ * 2 + gg + 1) * 64]
                    g.cp('dve', vcmp[:, nt_, gg, 0:64], sl, r=[pb], w=[b_vcmp])
            for gg in range(G):
                g.mark('nsa T%d g%d' % (T, gg))
                for nt_ in ntl:
                    off = min(128 * T - 2048 * nt_, 2176)
                    for rr_ in range(4):
                        g.cp('pool' if rr_ % 2 else 'act', mask4[nt_][:, rr_ * 128:(rr_ + 1) * 128], Mw[:, off:off + 128],
                             r=[b_c], w=[b_m4])
                    if nt_ == 0:
                        g.memset('pool', mask4[0][0:1, :], 0.0, w=[b_m4])
                    for half in range(2):
                        pt, pb = pp.next()
                        hs0 = gg * 8 + half * 4
                        g.mm(pt[:], kcmpT[gg][:, nt_ * 128:(nt_ + 1) * 128],
                             qT[gg][:, half * 4:(half + 1) * 4, :].rearrange('p a b -> p (a b)'), True, False,
                             r=[b_kcmp, b_qT], w=[pb])
                        g.mm(pt[:], kAc[:, T * 256 + nt_ * 128:T * 256 + (nt_ + 1) * 128],
                             qA[:, hs0 * 128:(hs0 + 4) * 128], False, True, r=[b_tab], w=[pb])
                        ei = half
                        g.act(e16[ei][:], pt[:], AF.Exp, r=[pb], w=[b_e16[ei]], scale=0.125, bias=-30.0)
                        g.tt('dve', pc[nt_][:, half * 4:(half + 1) * 4, :].rearrange('p a b -> p (a b)'), e16[ei][:],
                             mask4[nt_][:], ALU.mult, r=[b_e16[ei], b_m4], w=[b_pc])
                pu, pbu = pp.next()
                for hf in range(2):
                    zero_bank(acc[hf], b_acc[hf])
                for hp in range(8):
                    for ii, nt_ in enumerate(ntl):
                        last = ii == len(ntl) - 1
                        g.mm(acc[hp // 4][:, (hp % 4) * 65:(hp % 4) * 65 + 65], pc[nt_][:, hp, :], vcmp[:, nt_, gg, :],
                             False, False, r=[b_pc, b_vcmp], w=[b_acc[hp // 4]], skip=True)
                    for ii, nt_ in enumerate(ntl):
                        g.mm(pu[:, hp * 64:(hp + 1) * 64], pc[nt_][:, hp, :], ovl[:, nt_, :], ii == 0,
                             ii == len(ntl) - 1, r=[b_pc, b_tab], w=[pbu])
                for hf in range(2):
                    den = acc[hf][:, 0:260].rearrange('p (h c) -> p h c', c=65)[:, :, 64]
                    g.ts('dve', rec[:, 0, hf * 4:(hf + 1) * 4], den, 1e-30, None, ALU.max, r=[b_acc[hf]], w=[b_rec])
                g.op('dve', lambda hh: hh.reciprocal(rec[:, 0, :], rec[:, 0, :]), r=[b_rec], w=[b_rec])
                for hp in range(8):
                    if hp == 0:
                        g.ts('dve', imp[:], pu[:, 0:64], rec[:, 0, 0:1], None, ALU.mult, r=[pbu, b_rec], w=[b_imp])
                    else:
                        g.stt('dve', imp[:], pu[:, hp * 64:(hp + 1) * 64], rec[:, 0, hp:hp + 1], imp[:], ALU.mult, ALU.add,
                              r=[pbu, b_rec], w=[b_imp])
                for hp in range(8):
                    h = gg * 8 + hp
                    g.tt('dve', hsm[3][:, hp:hp + 1], rec[:, 0, hp:hp + 1], gts[:, h:h + 1], ALU.mult,
                         r=[b_rec, b_gts], w=[b_hsm])
                    g.ts('dve', og[:, h * 64:(h + 1) * 64],
                         acc[hp // 4][:, (hp % 4) * 65:(hp % 4) * 65 + 64], hsm[3][:, hp:hp + 1], None, ALU.mult,
                         r=[b_acc[hp // 4], b_hsm], w=[b_og])
                g.tt('dve', imp[:], imp[:], Vw[:, 64 - 2 * T:128 - 2 * T], ALU.mult, r=[b_c], w=[b_imp])
                g.tt('dve', imp[:], imp[:], Aw[:, 64 - 2 * T:128 - 2 * T], ALU.add, r=[b_c], w=[b_imp])
                g.memset('pool', imp[:, 0:1], 1e6, w=[b_imp])
                g.op('dve', lambda hh: hh.max(m8[:, 0:8], imp[:]), r=[], w=[b_imp])
                g.op('dve', lambda hh: hh.match_replace(imp2[:], m8[:, 0:8], imp[:], -3.0e38), r=[], w=[b_imp])
                g.op('dve', lambda hh: hh.max(m8[:, 8:16], imp2[:]), r=[], w=[b_imp])
                g.ts('dve', selm[:], imp[:], m8[:, 15:16], None, ALU.is_ge, r=[], w=[b_imp])
                pt, pb = pp.next()
                g.tr(pt[0:64, 0:128], selm[:], k.ident_f[:], r=[b_imp, k.b_ident], w=[pb])
                for rr_ in range(4):
                    g.cp('dve' if rr_ % 2 else 'act', selT4[:, rr_, :], pt[0:64, 0:128], r=[pb], w=[b_selT])
                for br, kts in enumerate([list(range(0, T + 1)), list(range(max(0, T - 4), T + 1))]):
                    g.mark('nsa T%d g%d br%d' % (T, gg, br))
                    for hf in range(2):
                        zero_bank(acc[hf], b_acc[hf])
                    for ki, kt in enumerate(kts):
                        d = T - kt
                        lastk = ki == len(kts) - 1
                        if br == 0 and d >= 1:
                            pm, pbm = pp.next()
                            g.mm(pm[:], Ex[:, kt, :], selT4[:].rearrange('p a b -> p (a b)'), True, True,
                                 r=[b_c, b_selT], w=[pbm])
                        for half in range(2):
                            if br == 0:
                                k_ap, bk, v_ap, bv = ksT[gg][:, kt * 128:(kt + 1) * 128], b_ks[kt], vs[:, kt, gg, :], b_vs[kt]
                            else:
                                sl5 = kt % 5
                                k_ap, bk, v_ap, bv = kwT[gg][:, sl5 * 128:(sl5 + 1) * 128], b_kw[sl5], vw[:, sl5, gg, :], b_kw[sl5]
                            pt, pb = qk_exp(T, kt, gg, half, k_ap, bk)
                            pi = npt[0] % 3
                            npt[0] += 1
                            if br == 1 and 1 <= d <= 3:
                                g.act(pT[pi][:], pt[:], AF.Exp, r=[pb], w=[b_pT[pi]], scale=0.125, bias=-30.0)
                            else:
                                ei = half
                                g.act(e32[ei][:], pt[:], AF.Exp, r=[pb], w=[b_e32[ei]], scale=0.125, bias=-30.0)
                                if d == 0:
                                    g.tt('dve', pT[pi][:], e32[ei][:], tri4[:], ALU.mult, r=[b_e32[ei], b_c], w=[b_pT[pi]])
                                elif br == 1:
                                    g.tt('dve', pT[pi][:], e32[ei][:], triu4[:], ALU.mult, r=[b_e32[ei], b_c],
                                         w=[b_pT[pi]])
                                else:
                                    g.tt('dve', pT[pi][:], e32[ei][:], pm[:], ALU.mult, r=[b_e32[ei], pbm], w=[b_pT[pi]])
                            for hh in range(4):
                                g.mm(acc[half][:, hh * 65:hh * 65 + 65], pT[pi][:, hh * 128:(hh + 1) * 128],
                                     v_ap, False, False, r=[b_pT[pi], bv], w=[b_acc[half]], skip=True)
                    bi = br + 1
                    for hf in range(2):
                        den = acc[hf][:, 0:260].rearrange('p (h c) -> p h c', c=65)[:, :, 64]
                        g.ts('dve', rec[:, bi, hf * 4:(hf + 1) * 4], den, 1e-30, None, ALU.max, r=[b_acc[hf]], w=[b_rec])
                    g.op('dve', lambda hh, bi=bi: hh.reciprocal(rec[:, bi, :], rec[:, bi, :]), r=[b_rec], w=[b_rec])
                    for hp in range(8):
                        h = gg * 8 + hp
                        g.tt('dve', hsm[4][:, hp:hp + 1], rec[:, bi, hp:hp + 1], gts[:, bi * 16 + h:bi * 16 + h + 1],
                             ALU.mult, r=[b_rec, b_gts], w=[b_hsm])
                        g.stt('dve', og[:, h * 64:(h + 1) * 64], acc[hp // 4][:, (hp % 4) * 65:(hp % 4) * 65 + 64],
                              hsm[4][:, hp:hp + 1], og[:, h * 64:(h + 1) * 64], ALU.mult, ALU.add,
                              r=[b_acc[hp // 4], b_hsm], w=[b_og])
            g.cp('act', ogb[:], og[:], r=[b_og], w=[b_ogb])
            pt, pb = pp.next()
            ptb = pt[:].bitcast(BF16)
            for cc in range(8):
                g.tr(ptb[:, cc * 128:(cc + 1) * 128], ogb[:, cc * 128:(cc + 1) * 128], k.ident_b[:],
                     r=[b_ogb, k.b_ident], w=[pb])
            g.cp('dve', oT[:].rearrange('p a b -> p (a b)'), ptb, r=[pb], w=[b_oT])
            for hf in range(2):
                for ec in range(8):
                    g.mm(c['ps_out'][:, hf * 512:(hf + 1) * 512], oT[:, ec, :], wout[:, ec, hf * 512:(hf + 1) * 512],
                         ec == 0, ec == 7, r=[b_oT, b_wout[hf]], w=[c['b_ps_out']])
            mixer_back(k, c, T, xd, b_xd)
        g.barrier()


PARAM_SHAPES = {
    'norm_mix_pre': (DEPTH, D), 'norm_mix_post': (DEPTH, D), 'norm_mlp_pre': (DEPTH, D), 'norm_mlp_post': (DEPTH, D),
    'mlp_w_up': (DEPTH, D, DFF), 'mlp_w_down': (DEPTH, DFF, D),
    'ret_w_in': (1, D, 6144), 'ret_gn': (1, 2048), 'ret_w_out': (1, 2048, D),
    'gla_w_in': (1, D, 3088), 'gla_w_gate_up': (1, 16, 512), 'gla_b_gate': (1, 512), 'gla_gn': (1, 1024),
    'gla_w_out': (1, 1024, D),
    'lru_w_in': (1, D, 2048), 'lru_conv_w': (1, 4, 1024), 'lru_conv_b': (1, 1024), 'lru_w_a': (1, 8, 128, 128),
    'lru_b_a': (1, 1024), 'lru_w_x': (1, 8, 128, 128), 'lru_b_x': (1, 1024), 'lru_lambda': (1, 1024),
    'lru_w_out': (1, 1024, D),
    'nsa_w_in': (1, D, 1840), 'nsa_pe_k': (1, 32, 64), 'nsa_w1_k': (1, 2048, 128), 'nsa_w2_k': (1, 128, 64),
    'nsa_pe_v': (1, 32, 64), 'nsa_w1_v': (1, 2048, 128), 'nsa_w2_v': (1, 128, 64), 'nsa_w_out': (1, 1024, D),
}


def build(S, phases):
    nc = bass.Bass('TRN2', target_bir_lowering=False)
    x_in = nc.dram_tensor('x', [S, D], F32, kind='ExternalInput').ap()
    W = {}
    for name, shp in PARAM_SHAPES.items():
        W[name] = nc.dram_tensor(name, list(shp), F32, kind='ExternalInput').ap()
    if any(kind == 'mix' and li % 4 == 3 for kind, li in phases):
        for name, arr in nsa_tables(S // 128).items():
            W[name] = nc.dram_tensor(name, list(arr.shape), F32, kind='ExternalInput').ap()
    y = nc.dram_tensor('y', [S, D], F32, kind='ExternalOutput').ap()
    with contextlib.ExitStack() as es:
        g = G(nc, es)
        k = K(nc, g, es, S)
        setup_consts(k)
        NT = S // 128
        b_xd = [Buf() for _ in range(NT)]
        for t in range(NT):
            g.dma('sp', y[t * 128:(t + 1) * 128, :], x_in[t * 128:(t + 1) * 128, :], w=[b_xd[t]])
        for kind, li in phases:
            if kind == 'mlp':
                phase_mlp(k, li, y, b_xd, W)
            else:
                MIXERS[li % 4](k, li, y, b_xd, W)
        g.barrier()
        g.emit()
    return nc


MIXERS = {0: phase_ret, 1: phase_gla, 2: phase_lru, 3: phase_nsa}

ALL_PHASES = [(kind, li) for li in range(DEPTH) for kind in ('mix', 'mlp')]


def run(inputs, S, phases):
    nc = build(S, phases)
    B = inputs['x'].shape[0]
    in_maps = []
    tabs = nsa_tables(S // 128) if any(kind == 'mix' and li % 4 == 3 for kind, li in phases) else {}
    for b in range(B):
        m = {'x': np.ascontiguousarray(inputs['x'][b], dtype=np.float32)}
        for name in PARAM_SHAPES:
            m[name] = np.ascontiguousarray(inputs[name], dtype=np.float32)
        m.update(tabs)
        in_maps.append(m)
    res = run_bass_kernel_spmd(nc, in_maps, core_ids=list(range(B)))
    return np.stack([np.asarray(r['y']) for r in res.results], axis=0)


def kernel(**inputs):
    out = run(inputs, 4096, ALL_PHASES)
    return out.astype(np.float32)
```

```python
import contextlib
import numpy as np
import concourse.bass as bass
import concourse.mybir as mybir
from concourse.bass_utils import run_bass_kernel_spmd

F32 = mybir.dt.float32
BF16 = mybir.dt.bfloat16
AF = mybir.ActivationFunctionType
ALU = mybir.AluOpType
AX = mybir.AxisListType

D = 1024
DFF = 4096
DEPTH = 4
EPS = 1e-6
ENG = ['pe', 'act', 'dve', 'pool', 'sp']
NDSEM = 16


class Buf:
    __slots__ = ('w', 'r', 'x')

    def __init__(self, x=False):
        self.w = None
        self.r = {}
        self.x = x


class Op:
    __slots__ = ('eng', 'fn', 'waits', 'idx', 'inc', 'semval', 'dma', 'dsem', 'dval')


class G:
    def __init__(self, nc, es):
        self.nc = nc
        self.q = {e: [] for e in ENG}
        self.waited = {e: {} for e in ENG}
        self.sem = {e: es.enter_context(nc.semaphore('s_' + e)) for e in ENG}
        self.dq = {}
        for e in ('sp', 'pool', 'act'):
            self.dq[e] = [[es.enter_context(nc.semaphore('d_%s%d' % (e, i))) for i in range(NDSEM)], 0]
        self.uid = 0
        self.fill_regs = {}
        import os
        self.max_ops = int(os.environ.get('KMAX', '100000000'))

    def _rawwait(self, o, sem, val):
        key = id(sem)
        if val <= self.waited[o.eng].get(key, 0):
            return
        self.waited[o.eng][key] = val
        o.waits.append(('raw', sem, val))

    def _wait(self, o, d):
        if d.dma:
            self._rawwait(o, d.dsem, d.dval)
            return
        if d.eng == o.eng and o.eng == 'pe':
            return
        if d.idx <= self.waited[o.eng].get(d.eng, -1):
            return
        self.waited[o.eng][d.eng] = d.idx
        d.inc = True
        o.waits.append(('op', d))

    def op(self, eng, fn, r=(), w=(), dma=False):
        if self.uid >= self.max_ops:
            return None
        o = Op()
        o.eng = eng
        o.fn = fn
        o.waits = []
        o.idx = len(self.q[eng])
        o.inc = False
        o.dma = dma
        o.semval = 0
        if any(b.x for b in r):
            w = list(w) + [b for b in r if b.x and b not in w]
            r = [b for b in r if not b.x]
        for b in r:
            if b.w is not None:
                self._wait(o, b.w)
        for b in w:
            if b.w is not None:
                self._wait(o, b.w)
            for d in b.r.values():
                self._wait(o, d)
        if dma:
            sems, cnt = self.dq[eng]
            k = cnt % len(sems)
            o.dsem = sems[k]
            o.dval = 16 * (cnt // len(sems) + 1)
            self.dq[eng][1] = cnt + 1
            if o.dval > 16:
                self._rawwait(o, o.dsem, o.dval - 16)
        self.q[eng].append(o)
        self.uid += 1
        key = ('d', self.uid) if dma else eng
        for b in r:
            b.r[key] = o
        for b in w:
            b.w = o
            b.r = {}
        return o

    def barrier(self):
        last = {}
        for e in ENG:
            last[e] = None
            for o in reversed(self.q[e]):
                if o.fn is not None:
                    last[e] = o
                    break
        dlast = []
        for e in self.dq:
            sems, cnt = self.dq[e]
            for k in range(len(sems)):
                n = (cnt - k + len(sems) - 1) // len(sems) if cnt > k else 0
                if n > 0:
                    dlast.append((sems[k], 16 * n))
        for e in ENG:
            o = Op()
            o.eng = e
            o.fn = None
            o.waits = []
            o.idx = len(self.q[e])
            o.inc = False
            o.dma = False
            o.semval = 0
            for f in ENG:
                d = last[f]
                if d is None or f == e:
                    continue
                if d.dma:
                    continue
                if d.idx <= self.waited[e].get(f, -1):
                    continue
                self.waited[e][f] = d.idx
                d.inc = True
                o.waits.append(('op', d))
            for sem, val in dlast:
                self._rawwait(o, sem, val)
            self.q[e].append(o)

    def emit(self):
        nc = self.nc
        for e in ENG:
            c = 0
            for o in self.q[e]:
                if o.inc:
                    c += 1
                    o.semval = c
        sem = self.sem

        def run(e, h):
            for o in self.q[e]:
                for wt in o.waits:
                    if wt[0] == 'op':
                        h.wait_ge(sem[wt[1].eng], wt[1].semval)
                    else:
                        h.wait_ge(wt[1], wt[2])
                if o.fn is None:
                    continue
                ins = o.fn(h)
                if o.dma:
                    ins.then_inc(o.dsem, 16)
                elif o.inc:
                    ins.then_inc(sem[e], 1)

        with nc.Block() as block:
            @block.tensor
            def _(h):
                run('pe', h)

            @block.scalar
            def _(h):
                run('act', h)

            @block.vector
            def _(h):
                run('dve', h)

            @block.gpsimd
            def _(h):
                run('pool', h)

            @block.sync
            def _(h):
                run('sp', h)

    def mark(self, name):
        import os
        if os.environ.get('KDBG'):
            print('MARK', name, self.uid, flush=True)

    def mm(self, out, lhsT, rhs, start, stop, r=(), w=(), skip=False):
        if skip:
            return self.op('pe', lambda h: h.matmul(out, lhsT, rhs, start=start, stop=stop, skip_group_check=True), r, w)
        return self.op('pe', lambda h: h.matmul(out, lhsT, rhs, start=start, stop=stop), r, w)

    def tr(self, out, in_, ident, r=(), w=()):
        return self.op('pe', lambda h: h.transpose(out, in_, ident), r, w)

    def act(self, out, in_, func, r=(), w=(), **kw):
        return self.op('act', lambda h: h.activation(out, in_, func, **kw), r, w)

    def tt(self, eng, out, in0, in1, op, r=(), w=()):
        return self.op(eng, lambda h: h.tensor_tensor(out, in0, in1, op), r, w)

    def ts(self, eng, out, in0, s1, s2, op0, op1=None, r=(), w=(), **kw):
        if op1 is None:
            return self.op(eng, lambda h: h.tensor_scalar(out, in0, s1, None, op0, **kw), r, w)
        return self.op(eng, lambda h: h.tensor_scalar(out, in0, s1, s2, op0, op1, **kw), r, w)

    def stt(self, eng, out, in0, sc, in1, op0, op1, r=(), w=()):
        return self.op(eng, lambda h: h.scalar_tensor_tensor(out, in0, sc, in1, op0, op1), r, w)

    def cp(self, eng, out, in_, r=(), w=()):
        if eng == 'act':
            return self.op('act', lambda h: h.copy(out, in_), r, w)
        return self.op(eng, lambda h: h.tensor_copy(out, in_), r, w)

    def affsel(self, out, in_, pattern, op, fill, base, cm, r=(), w=()):
        return self.op('pool', lambda h: h.affine_select(out, in_, pattern, op, float(fill), base=base,
                                                         channel_multiplier=cm), r, w)

    def memset(self, eng, ap, val, r=(), w=()):
        return self.op(eng, lambda h: h.memset(ap, val), r, w)

    def dma(self, eng, out, in_, r=(), w=(), **kw):
        return self.op(eng, lambda h: h.dma_start(out, in_, **kw), r, w, dma=True)


class K:
    def __init__(self, nc, g, es, S):
        self.nc = nc
        self.g = g
        self.es = es
        self.S = S
        self.NT = S // 128
        self.n = 0

    def sb(self, es, shape, dt, name=None):
        self.n += 1
        return es.enter_context(self.nc.sbuf_tensor('%s_%d' % (name or 't', self.n), list(shape), dt))

    def ps(self, es, shape, dt=F32, name=None):
        self.n += 1
        return es.enter_context(self.nc.psum_tensor('%s_%d' % (name or 'p', self.n), list(shape), dt))


def setup_consts(k):
    g, es = k.g, k.es
    k.ident_f = k.sb(es, [128, 128], F32, 'identf')
    k.ident_b = k.sb(es, [128, 128], BF16, 'identb')
    k.b_ident = Buf()
    ones = k.sb(es, [128, 128], F32, 'ones')
    bo = Buf()
    g.memset('pool', ones[:], 1.0, w=[bo])
    g.affsel(k.ident_f[:], ones[:], [[-1, 128]], ALU.is_equal, 0.0, 0, 1, r=[bo], w=[k.b_ident])
    g.cp('pool', k.ident_b[:], k.ident_f[:], r=[k.b_ident], w=[k.b_ident])
    k.ones_f = ones
    k.b_ones = bo
    k.stage = k.sb(es, [16, D], F32, 'stage')
    k.b_stage = Buf()


def load_cols(k, es, rows_ap, R, ps_bank, b_ps, name):
    g = k.g
    stage = k.stage[0:R, :]
    cols = k.sb(es, [128, 8, R], F32, name)
    bs, bc = k.b_stage, Buf()
    if isinstance(rows_ap, list):
        r0 = 0
        for ra in rows_ap:
            n = ra.shape[0]
            g.dma('sp', k.stage[r0:r0 + n, :], ra, w=[bs])
            r0 += n
        assert r0 == R
    else:
        g.dma('sp', stage, rows_ap, w=[bs])
    for c in range(8):
        g.tr(ps_bank[:, c * R:(c + 1) * R], stage[:, c * 128:(c + 1) * 128], k.ident_f[0:R, 0:R],
             r=[bs, k.b_ident], w=[b_ps])
    g.cp('dve', cols[:].rearrange('p c r -> p (c r)'), ps_bank[:, 0:8 * R], r=[b_ps], w=[bc])
    return cols, bc


def emit_front(k, xin_tile_ap, b_xd, xt, b_xt, gcol_ap, b_gcol, hT_ap, b_hT, scr, ps_tr, b_ps_tr, st, b_st,
               hb, b_hb):
    g = k.g
    g.dma('sp', xt[:], xin_tile_ap, r=[b_xd], w=[b_xt])
    g.act(scr[:], xt[:], AF.Square, r=[b_xt], w=[b_st, k.b_scr], accum_out=st[:, 0:1])
    g.ts('dve', st[:, 1:2], st[:, 0:1], 1.0 / D, EPS, ALU.mult, ALU.add, r=[b_st], w=[b_st])
    g.act(st[:, 3:4], st[:, 1:2], AF.Sqrt, r=[b_st], w=[b_st])
    g.op('dve', lambda h, o=st[:, 2:3], i=st[:, 3:4]: h.reciprocal(o, i), r=[b_st], w=[b_st])
    g.ts('dve', hb[:], xt[:], st[:, 2:3], None, ALU.mult, r=[b_xt, b_st], w=[b_hb])
    for c in range(8):
        g.tr(ps_tr[:, c * 128:(c + 1) * 128], hb[:, c * 128:(c + 1) * 128], k.ident_b[:],
             r=[b_hb, k.b_ident], w=[b_ps_tr])
    g.tt('dve', hT_ap, ps_tr[:].rearrange('p (c t) -> p c t', c=8),
         gcol_ap.unsqueeze(2).to_broadcast([128, 8, 128]), ALU.mult, r=[b_ps_tr, b_gcol], w=[b_hT])


def emit_post(k, ps_ap, b_ps, xres, b_xres, gpost, b_gpost, xout_tile_ap, b_xd, scr, st, b_st,
              tmp, b_tmp):
    g = k.g
    g.act(scr[:], ps_ap, AF.Square, r=[b_ps], w=[b_st, k.b_scr], accum_out=st[:, 0:1])
    g.ts('dve', st[:, 1:2], st[:, 0:1], 1.0 / D, EPS, ALU.mult, ALU.add, r=[b_st], w=[b_st])
    g.act(st[:, 3:4], st[:, 1:2], AF.Sqrt, r=[b_st], w=[b_st])
    g.op('dve', lambda h, o=st[:, 2:3], i=st[:, 3:4]: h.reciprocal(o, i), r=[b_st], w=[b_st])
    g.stt('dve', tmp[:], ps_ap, st[:, 2:3], gpost[:], ALU.mult, ALU.mult, r=[b_ps, b_st, b_gpost], w=[b_tmp])
    g.tt('pool', xres[:], tmp[:], xres[:], ALU.add, r=[b_tmp], w=[b_xres])
    g.dma('sp', xout_tile_ap, xres[:], r=[b_xres], w=[b_xd])


def phase_mlp(k, li, xd, b_xd, W):
    g, nc = k.g, k.nc
    NT = k.NT
    GT = 4 if NT % 4 == 0 else 1
    NG = NT // GT
    TG = GT * 128
    with contextlib.ExitStack() as es:
        wup = k.sb(es, [128, 8, DFF], BF16, 'wup')
        wdn = k.sb(es, [128, 32, D], BF16, 'wdn')
        b_wup = [Buf() for _ in range(8)]
        b_wdn = [Buf() for _ in range(8)]
        wup_d = W['mlp_w_up'][li].rearrange('(c p) f -> p c f', p=128)
        wdn_d = W['mlp_w_down'][li].rearrange('(c p) d -> p c d', p=128)
        for j in range(8):
            g.dma('pool', wup[:, :, j * 512:(j + 1) * 512], wup_d[:, :, j * 512:(j + 1) * 512], w=[b_wup[j]])
        for j in range(8):
            g.dma('pool', wdn[:, j * 4:(j + 1) * 4, :], wdn_d[:, j * 4:(j + 1) * 4, :], w=[b_wdn[j]])
        ps_tr = k.ps(es, [128, 1024], BF16, 'pstr')
        b_ps_tr = Buf(True)
        ps_up = [k.ps(es, [128, 512], F32, 'psup') for _ in range(3)]
        b_ps_up = [Buf(True) for _ in range(3)]
        ps_dn = [k.ps(es, [128, 1024], F32, 'psdn') for _ in range(2)]
        b_ps_dn = [Buf(True) for _ in range(2)]
        gcol, b_gcol = load_cols(k, es, W['norm_mlp_pre'][li:li + 1, :], 1, ps_up[0], b_ps_up[0], 'gcol')
        gpost = k.sb(es, [128, D], F32, 'gpost')
        b_gpost = Buf()
        g.dma('sp', gpost[:], W['norm_mlp_post'][li:li + 1, :].partition_broadcast(128), w=[b_gpost])
        NX = 3
        xt = [k.sb(es, [128, D], F32, 'xt') for _ in range(NX)]
        b_xt = [Buf() for _ in range(NX)]
        hb = [k.sb(es, [128, D], BF16, 'hb') for _ in range(2)]
        b_hb = [Buf() for _ in range(2)]
        scr = k.sb(es, [128, D], BF16, 'scr')
        k.b_scr = Buf()
        st = [k.sb(es, [128, 4], F32, 'st') for _ in range(4)]
        b_st = [Buf() for _ in range(4)]
        hT = k.sb(es, [128, 8, TG], BF16, 'hT')
        b_hT = [Buf() for _ in range(GT)]
        aT = k.sb(es, [128, 32, TG], BF16, 'aT')
        b_aT = [Buf() for _ in range(32)]
        rr = [k.sb(es, [128, TG], F32, 'rr') for _ in range(2)]
        b_rr = [Buf() for _ in range(2)]
        tmp = [k.sb(es, [128, D], F32, 'tmp') for _ in range(2)]
        b_tmp = [Buf() for _ in range(2)]
        cnt = 0

        def fronts(gi):
            nonlocal cnt
            for tl in range(GT):
                t = gi * GT + tl
                i = cnt % NX
                emit_front(k, xd[t * 128:(t + 1) * 128, :], b_xd[t], xt[i], b_xt[i], gcol[:, :, 0], b_gcol,
                           hT[:, :, tl * 128:(tl + 1) * 128], b_hT[tl], scr, ps_tr, b_ps_tr, st[cnt % 2],
                           b_st[cnt % 2], hb[cnt % 2], b_hb[cnt % 2])
                cnt += 1

        fronts(0)
        for gi in range(NG):
            for f in range(32):
                pi = f % 3
                for c in range(8):
                    g.mm(ps_up[pi][:, 0:TG], wup[:, c, f * 128:(f + 1) * 128], hT[:, c, :], c == 0, c == 7,
                         r=[b_wup[f // 4]] + b_hT, w=[b_ps_up[pi]])
                ri = f % 2
                g.act(rr[ri][:], ps_up[pi][:, 0:TG], AF.Relu, r=[b_ps_up[pi]], w=[b_rr[ri]])
                g.tt('pool' if f % 2 else 'dve', aT[:, f, :], rr[ri][:], rr[ri][:], ALU.mult, r=[b_rr[ri]],
                     w=[b_aT[f]])
            if gi + 1 < NG:
                fronts(gi + 1)
            for tl in range(GT):
                t = gi * GT + tl
                pd = t % 2
                for hf in range(2):
                    for f in range(32):
                        g.mm(ps_dn[pd][:, hf * 512:(hf + 1) * 512], aT[:, f, tl * 128:(tl + 1) * 128],
                             wdn[:, f, hf * 512:(hf + 1) * 512], f == 0, f == 31,
                             r=[b_aT[f], b_wdn[f // 4]], w=[b_ps_dn[pd]])
                i = cnt % NX
                cnt += 1
                g.dma('sp', xt[i][:], xd[t * 128:(t + 1) * 128, :], r=[b_xd[t]], w=[b_xt[i]])
                emit_post(k, ps_dn[pd][:], b_ps_dn[pd], xt[i], b_xt[i], gpost, b_gpost,
                          xd[t * 128:(t + 1) * 128, :], b_xd[t], scr, st[2 + pd], b_st[2 + pd], tmp[pd], b_tmp[pd])
        g.barrier()


class PsPool:
    def __init__(self, k, es, n):
        self.t = [k.ps(es, [128, 512], F32, 'pp') for _ in range(n)]
        self.b = [Buf(True) for _ in range(n)]
        self.i = 0

    def next(self):
        j = self.i % len(self.t)
        self.i += 1
        return self.t[j], self.b[j]


def load_w(k, wt, w_dram_ap, ncols, piece, nchunk=None):
    g = k.g
    src = w_dram_ap.rearrange('(c p) f -> p c f', p=128)
    bufs = []
    for j in range(0, ncols, piece):
        b = Buf()
        e = min(ncols, j + piece)
        g.dma('pool', wt[:, :, j:e], src[:, :, j:e], w=[b])
        bufs.append(b)
    return bufs


def mixer_common(k, es, li, W, nxt=2, npool=6):
    g = k.g
    c = {}
    c['pp'] = PsPool(k, es, npool)
    c['ps_out'] = k.ps(es, [128, 1024], F32, 'psout')
    c['b_ps_out'] = Buf(True)
    pt, pb = c['pp'].next()
    c['gcol'], c['b_gcol'] = load_cols(k, es, W['norm_mix_pre'][li:li + 1, :], 1, pt, pb, 'gcol')
    c['gpost'] = k.sb(es, [128, D], F32, 'gpost')
    c['b_gpost'] = Buf()
    g.dma('sp', c['gpost'][:], W['norm_mix_post'][li:li + 1, :].partition_broadcast(128), w=[c['b_gpost']])
    c['xt'] = [k.sb(es, [128, D], F32, 'xt') for _ in range(nxt)]
    c['b_xt'] = [Buf() for _ in range(nxt)]
    c['hb'] = k.sb(es, [128, D], BF16, 'hb')
    c['b_hb'] = Buf()
    c['scr'] = c['hb']
    k.b_scr = c['b_hb']
    c['st'] = [k.sb(es, [128, 4], F32, 'st') for _ in range(4)]
    c['b_st'] = [Buf() for _ in range(4)]
    c['tmp'] = k.sb(es, [128, D], F32, 'tmp')
    c['b_tmp'] = Buf()
    return c


def mixer_front(k, c, t, xd, b_xd, hT_ap, b_hT):
    pt, pb = c['pp'].next()
    i = t % 2
    ix = t % len(c['xt'])
    emit_front(k, xd[t * 128:(t + 1) * 128, :], b_xd[t], c['xt'][ix], c['b_xt'][ix], c['gcol'][:, :, 0], c['b_gcol'],
               hT_ap, b_hT, c['scr'], pt[:].bitcast(BF16), pb, c['st'][i], c['b_st'][i], c['hb'], c['b_hb'])


def mixer_back(k, c, t, xd, b_xd):
    i = t % 2
    ix = t % len(c['xt'])
    emit_post(k, c['ps_out'][:], c['b_ps_out'], c['xt'][ix], c['b_xt'][ix], c['gpost'], c['b_gpost'],
              xd[t * 128:(t + 1) * 128, :], b_xd[t], c['scr'], c['st'][2 + i], c['b_st'][2 + i], c['tmp'], c['b_tmp'])


def head_norm_stats(k, src_ap, mv_ap, b_src, b_mv, scratch6, b_s6):
    g = k.g
    g.op('dve', lambda h: h.bn_stats(scratch6, src_ap), r=[b_src], w=[b_s6])
    g.op('dve', lambda h: h.bn_aggr(mv_ap, scratch6), r=[b_s6], w=[b_mv])


def phase_ret(k, li, xd, b_xd, W):
    import math
    g, nc = k.g, k.nc
    NT = k.NT
    j = li // 4
    H, DK, DV = 4, 256, 512
    lg = [math.log1p(-2.0 ** (-5.0 - h)) for h in range(H)]
    with contextlib.ExitStack() as es:
        win = k.sb(es, [128, 8, 6144], BF16, 'win')
        wout = k.sb(es, [128, 16, D], BF16, 'wout')
        b_win = load_w(k, win, W['ret_w_in'][j], 6144, 512)
        b_wout = load_w(k, wout, W['ret_w_out'][j], D, 512)
        c = mixer_common(k, es, li, W)
        pp = c['pp']
        pt, pb = pp.next()
        gncol, b_gncol = load_cols(k, es, W['ret_gn'][j:j + 1, :].rearrange('o (r f) -> (o r) f', r=2), 2, pt, pb,
                                   'gncol')
        decq = k.sb(es, [128, 8, 128], BF16, 'decq')
        dintra = k.sb(es, [128, H, 128], F32, 'dintra')
        dk = k.sb(es, [128, H], F32, 'dk')
        iot = k.sb(es, [128, 128], F32, 'iot')
        iot2 = k.sb(es, [128, 128], F32, 'iot2')
        iot3 = k.sb(es, [128, 1], F32, 'iot3')
        b_c = Buf()
        g.op('pool', lambda h: h.iota(iot[:], [[1, 128]], base=1, channel_multiplier=0,
                                      allow_small_or_imprecise_dtypes=True), w=[b_c])
        g.op('pool', lambda h: h.iota(iot2[:], [[1, 128]], base=0, channel_multiplier=-1,
                                      allow_small_or_imprecise_dtypes=True), w=[b_c])
        g.op('pool', lambda h: h.iota(iot3[:], [[0, 1]], base=127, channel_multiplier=-1,
                                      allow_small_or_imprecise_dtypes=True), w=[b_c])
        g.ts('pool', iot2[:], iot2[:], 0.0, None, ALU.max, r=[b_c], w=[b_c])
        for h in range(H):
            g.act(decq[:, 2 * h, :], iot[:], AF.Exp, r=[b_c], w=[b_c], scale=lg[h])
            g.act(decq[:, 2 * h + 1, :], iot[:], AF.Exp, r=[b_c], w=[b_c], scale=lg[h])
            g.act(dintra[:, h, :], iot2[:], AF.Exp, r=[b_c], w=[b_c], scale=lg[h])
            g.act(dk[:, h:h + 1], iot3[:], AF.Exp, r=[b_c], w=[b_c], scale=lg[h])
            g.affsel(dintra[:, h, :], dintra[:, h, :], [[1, 128]], ALU.is_ge, 0.0, 0, -1, r=[b_c], w=[b_c])
        g.ts('dve', dk[:], dk[:], 1.0 / 16.0, None, ALU.mult, r=[b_c], w=[b_c])
        hT = [k.sb(es, [128, 8, 128], BF16, 'hT')] * 2
        b_hT = [Buf()] * 2
        qT = k.sb(es, [128, 8, 128], BF16, 'qT')
        qdT = k.sb(es, [128, 8, 128], BF16, 'qdT')
        kT = k.sb(es, [128, 8, 128], BF16, 'kT')
        b_qT, b_qdT, b_kT = Buf(), Buf(), Buf()
        kdec = k.sb(es, [128, 1024], BF16, 'kdec')
        b_kdec = Buf()
        v = k.sb(es, [128, 2048], BF16, 'v')
        b_v = Buf()
        sg = k.sb(es, [128, 2048], BF16, 'sg')
        b_sg = Buf()
        stf = k.sb(es, [128, 8, 512], F32, 'stf')
        stb = k.sb(es, [128, 8, 512], BF16, 'stb')
        b_stf = [Buf() for _ in range(8)]
        b_stb = [Buf() for _ in range(8)]
        for i in range(8):
            g.memset('pool', stf[:, i, :], 0.0, w=[b_stf[i]])
            g.memset('pool', stb[:, i, :], 0.0, w=[b_stb[i]])
        sT = k.sb(es, [128, H, 128], BF16, 'sT')
        b_sT = Buf()
        onorm = k.sb(es, [128, 2048], BF16, 'onorm')
        b_on = Buf()
        oT = k.sb(es, [128, 16, 128], BF16, 'oT')
        b_oT = Buf()
        s6 = k.sb(es, [128, 6], F32, 's6')
        b_s6 = Buf()
        mv = k.sb(es, [128, H, 2], F32, 'mv')
        b_mv = Buf()
        hs = k.sb(es, [128, 3, H], F32, 'hs')
        b_hs = Buf()
        ontmp = c['tmp'][:, 0:256].bitcast(BF16)
        b_ontmp = c['b_tmp']

        g.mark('ret setup done')
        mixer_front(k, c, 0, xd, b_xd, hT[0][:], b_hT[0])
        for t in range(NT):
            hTt, bh = hT[t % 2], b_hT[t % 2]
            g.mark('ret tile %d' % t)
            for which in range(2):
                for half in range(2):
                    pt, pb = pp.next()
                    for cc in range(4):
                        col = which * 1024 + (half * 4 + cc) * 128
                        for ch in range(8):
                            g.mm(pt[:, cc * 128:(cc + 1) * 128], win[:, ch, col:col + 128], hTt[:, ch, :], ch == 0,
                                 ch == 7, r=[b_win[col // 512], bh], w=[pb])
                    dst = slice(half * 4, half * 4 + 4)
                    if which == 0:
                        g.cp('act', qT[:, dst, :].rearrange('p a b -> p (a b)'), pt[:], r=[pb], w=[b_qT])
                        g.tt('dve', qdT[:, dst, :].rearrange('p a b -> p (a b)'),
                             qT[:, dst, :].rearrange('p a b -> p (a b)'),
                             decq[:, dst, :].rearrange('p a b -> p (a b)'), ALU.mult,
                             r=[b_qT, b_c], w=[b_qdT])
                    else:
                        g.act(kT[:, dst, :].rearrange('p a b -> p (a b)'), pt[:], AF.Identity, r=[pb], w=[b_kT],
                              scale=1.0 / 16.0)
            g.mark('ret tokmajor')
            for blk in range(10):
                col = 1024 + blk * 512
                pt, pb = pp.next()
                for ch in range(8):
                    g.mm(pt[:], hTt[:, ch, :], win[:, ch, col:col + 512], ch == 0, ch == 7,
                         r=[b_win[col // 512], bh], w=[pb])
                if blk < 2:
                    for hh in range(2):
                        h = blk * 2 + hh
                        g.ts('dve', kdec[:, h * 256:(h + 1) * 256], pt[:, hh * 256:(hh + 1) * 256], dk[:, h:h + 1],
                             None, ALU.mult, r=[pb, b_c], w=[b_kdec])
                elif blk < 6:
                    o0 = (blk - 2) * 512
                    if blk % 2:
                        g.cp('act', v[:, o0:o0 + 512], pt[:], r=[pb], w=[b_v])
                    else:
                        g.cp('dve', v[:, o0:o0 + 512], pt[:], r=[pb], w=[b_v])
                else:
                    o0 = (blk - 6) * 512
                    g.act(sg[:, o0:o0 + 512], pt[:], AF.Silu, r=[pb], w=[b_sg])
            if t + 1 < NT:
                mixer_front(k, c, t + 1, xd, b_xd, hT[(t + 1) % 2][:], b_hT[(t + 1) % 2])
            g.mark('ret scores')
            pt, pb = pp.next()
            for h in range(H):
                for dc in range(2):
                    g.mm(pt[:, h * 128:(h + 1) * 128], kT[:, 2 * h + dc, :], qT[:, 2 * h + dc, :], dc == 0, dc == 1,
                         r=[b_kT, b_qT], w=[pb])
            g.tt('dve', sT[:].rearrange('p h t -> p (h t)'), pt[:], dintra[:].rearrange('p h t -> p (h t)'), ALU.mult,
                 r=[pb, b_c], w=[b_sT])
            g.mark('ret heads')
            o_ps = []
            for h in range(H):
                pt, pb = pp.next()
                g.mm(pt[:], sT[:, h, :], v[:, h * 512:(h + 1) * 512], True, False, r=[b_sT, b_v], w=[pb])
                for dc in range(2):
                    g.mm(pt[:], qdT[:, 2 * h + dc, :], stb[:, 2 * h + dc, :], False, dc == 1,
                         r=[b_qdT, b_stb[2 * h + dc]], w=[pb])
                g.op('dve', lambda hh, pt=pt: hh.bn_stats(s6[:], pt[:]), r=[pb], w=[b_s6])
                g.op('dve', lambda hh, h=h: hh.bn_aggr(mv[:, h, :], s6[:]), r=[b_s6], w=[b_mv])
                g.ts('dve', hs[:, 0, h:h + 1], mv[:, h, 1:2], EPS, None, ALU.add, r=[b_mv], w=[b_hs])
                g.act(hs[:, 1, h:h + 1], hs[:, 0, h:h + 1], AF.Sqrt, r=[b_hs], w=[b_hs])
                g.op('dve', lambda hh, h=h: hh.reciprocal(hs[:, 0, h:h + 1], hs[:, 1, h:h + 1]), r=[b_hs], w=[b_hs])
                g.stt('dve', hs[:, 2, h:h + 1], mv[:, h, 0:1], -1.0, hs[:, 0, h:h + 1], ALU.mult, ALU.mult,
                      r=[b_mv, b_hs], w=[b_hs])
                g.act(ontmp, pt[:], AF.Identity, r=[pb, b_hs], w=[b_ontmp], scale=hs[:, 0, h:h + 1],
                      bias=hs[:, 2, h:h + 1])
                g.tt('pool', onorm[:, h * 512:(h + 1) * 512], ontmp, sg[:, h * 512:(h + 1) * 512], ALU.mult,
                     r=[b_ontmp, b_sg], w=[b_on])
                for dc in range(2):
                    i = 2 * h + dc
                    pt2, pb2 = pp.next()
                    g.mm(pt2[:], kdec[:, h * 256 + dc * 128:h * 256 + (dc + 1) * 128], v[:, h * 512:(h + 1) * 512],
                         True, True, r=[b_kdec, b_v], w=[pb2])
                    g.stt('dve', stf[:, i, :], stf[:, i, :], math.exp(lg[h] * 128.0), pt2[:], ALU.mult, ALU.add,
                          r=[pb2], w=[b_stf[i]])
                    g.cp('act', stb[:, i, :], stf[:, i, :], r=[b_stf[i]], w=[b_stb[i]])
            g.mark('ret oT')
            for half in range(2):
                pt, pb = pp.next()
                ptb = pt[:].bitcast(BF16)
                for cc in range(8):
                    ec = half * 8 + cc
                    g.tr(ptb[:, cc * 128:(cc + 1) * 128], onorm[:, ec * 128:(ec + 1) * 128], k.ident_b[:],
                         r=[b_on, k.b_ident], w=[pb])
                g.tt('dve', oT[:, half * 8:(half + 1) * 8, :], ptb.rearrange('p (c t) -> p c t', c=8),
                     gncol[:, :, half].unsqueeze(2).to_broadcast([128, 8, 128]), ALU.mult, r=[pb, b_gncol], w=[b_oT])
            g.mark('ret outproj')
            for hf in range(2):
                for ec in range(16):
                    g.mm(c['ps_out'][:, hf * 512:(hf + 1) * 512], oT[:, ec, :], wout[:, ec, hf * 512:(hf + 1) * 512],
                         ec == 0, ec == 15, r=[b_oT, b_wout[hf]], w=[c['b_ps_out']])
            mixer_back(k, c, t, xd, b_xd)
        g.barrier()


def phase_lru(k, li, xd, b_xd, W):
    g, nc = k.g, k.nc
    NT = k.NT
    j = li // 4
    GT = 4 if NT % 4 == 0 else 1
    NG = NT // GT
    TG = GT * 128
    with contextlib.ExitStack() as es:
        win = k.sb(es, [128, 8, 2048], BF16, 'win')
        wout = k.sb(es, [128, 8, D], BF16, 'wout')
        b_win = load_w(k, win, W['lru_w_in'][j], 2048, 512)
        b_wout = load_w(k, wout, W['lru_w_out'][j], D, 512)
        wa = k.sb(es, [128, 8, 128], BF16, 'wa')
        wx = k.sb(es, [128, 8, 128], BF16, 'wx')
        b_wa, b_wx = Buf(), Buf()
        g.dma('pool', wa[:], W['lru_w_a'][j].rearrange('n c d -> c n d'), w=[b_wa])
        g.dma('pool', wx[:], W['lru_w_x'][j].rearrange('n c d -> c n d'), w=[b_wx])
        c = mixer_common(k, es, li, W, nxt=2 * GT)
        pp = c['pp']
        pt, pb = pp.next()
        cols, b_cols = load_cols(k, es, [W['lru_conv_w'][j], W['lru_conv_b'][j:j + 1, :], W['lru_b_a'][j:j + 1, :],
                                         W['lru_b_x'][j:j + 1, :], W['lru_lambda'][j:j + 1, :]], 8, pt, pb, 'lcols')
        c8 = k.sb(es, [128, 8], F32, 'c8')
        c8t = k.sb(es, [128, 8], F32, 'c8t')
        b_c8 = Buf()
        g.act(c8t[:], cols[:, :, 7], AF.Exp, r=[b_cols], w=[b_c8], scale=-1.0)
        g.ts('dve', c8t[:], c8t[:], 1.0, None, ALU.add, r=[b_c8], w=[b_c8])
        g.act(c8[:], c8t[:], AF.Ln, r=[b_c8], w=[b_c8])
        g.ts('dve', c8[:], c8[:], -8.0, None, ALU.mult, r=[b_c8], w=[b_c8])
        hT = k.sb(es, [128, 8, TG], BF16, 'hT')
        b_hT = [Buf() for _ in range(GT)]
        xbuf = k.sb(es, [128, 8, TG + 3], F32, 'xbuf')
        b_xbuf = [Buf() for _ in range(8)]
        hlast = k.sb(es, [128, 8], F32, 'hlast')
        b_hl = [Buf() for _ in range(8)]
        for n in range(8):
            g.memset('pool', xbuf[:, n, 0:3], 0.0, w=[b_xbuf[n]])
            g.memset('pool', hlast[:, n:n + 1], 0.0, w=[b_hl[n]])
        hyT = k.sb(es, [128, 8, TG], BF16, 'hyT')
        b_hy = [Buf() for _ in range(8)]

        def T(name, dt=F32):
            return [k.sb(es, [128, TG], dt, name) for _ in range(2)], [Buf() for _ in range(2)]
        xc, b_xc = T('xc')
        xcb, b_xcb = T('xcb', BF16)
        ysb, b_ysb = T('ysb')
        u, b_u = T('u')
        sgm, b_sgm = T('sgm')
        y, b_y = T('y')
        gr, b_gr = T('gr')
        gi, b_gi = T('gi')
        a, b_a = T('a')
        a2, b_a2 = T('a2')
        uu, b_uu = T('uu')
        hs, b_hs = T('hs')
        for gi_ in range(NG):
            for tl in range(GT):
                t = gi_ * GT + tl
                mixer_front(k, c, t, xd, b_xd, hT[:, :, tl * 128:(tl + 1) * 128], b_hT[tl])
            for n in range(8):
                s2 = n % 2
                pt, pb = pp.next()
                for ch in range(8):
                    g.mm(pt[:, 0:TG], win[:, ch, n * 128:(n + 1) * 128], hT[:, ch, :], ch == 0, ch == 7,
                         r=[b_win[(n * 128) // 512]] + b_hT, w=[pb])
                g.cp('act', xbuf[:, n, 3:3 + TG], pt[:, 0:TG], r=[pb], w=[b_xbuf[n]])
                g.ts('dve', xc[s2][:], xbuf[:, n, 0:TG], cols[:, n, 0:1], cols[:, n, 4:5], ALU.mult, ALU.add,
                     r=[b_xbuf[n], b_cols], w=[b_xc[s2]])
                for tap in range(1, 4):
                    g.stt('dve', xc[s2][:], xbuf[:, n, tap:tap + TG], cols[:, n, tap:tap + 1],
                          xc[s2][:], ALU.mult, ALU.add, r=[b_xbuf[n], b_cols], w=[b_xc[s2]])
                g.cp('pool', xbuf[:, n, 0:3], xbuf[:, n, TG:TG + 3], r=[], w=[b_xbuf[n]])
                g.cp('act', xcb[s2][:], xc[s2][:], r=[b_xc[s2]], w=[b_xcb[s2]])
                pt2, pb2 = pp.next()
                for ch in range(8):
                    g.mm(pt2[:, 0:TG], win[:, ch, 1024 + n * 128:1024 + (n + 1) * 128], hT[:, ch, :], ch == 0, ch == 7,
                         r=[b_win[(1024 + n * 128) // 512]] + b_hT, w=[pb2])
                g.cp('act', ysb[s2][:], pt2[:, 0:TG], r=[pb2], w=[b_ysb[s2]])
                g.act(u[s2][:], pt2[:, 0:TG], AF.Square, r=[pb2], w=[b_u[s2]])
                g.ts('dve', u[s2][:], u[s2][:], 0.044715, 1.0, ALU.mult, ALU.add, r=[], w=[b_u[s2]])
                g.tt('dve', u[s2][:], u[s2][:], ysb[s2][:], ALU.mult, r=[b_ysb[s2]], w=[b_u[s2]])
                g.act(sgm[s2][:], u[s2][:], AF.Sigmoid, r=[b_u[s2]], w=[b_sgm[s2]], scale=1.5957691216057308)
                g.tt('pool', y[s2][:], sgm[s2][:], ysb[s2][:], ALU.mult, r=[b_sgm[s2], b_ysb[s2]], w=[b_y[s2]])
                pt3, pb3 = pp.next()
                g.mm(pt3[:, 0:TG], wa[:, n, :], xcb[s2][:], True, True, r=[b_wa, b_xcb[s2]], w=[pb3])
                g.act(gr[s2][:], pt3[:, 0:TG], AF.Sigmoid, r=[pb3, b_cols], w=[b_gr[s2]], bias=cols[:, n, 5:6])
                pt4, pb4 = pp.next()
                g.mm(pt4[:, 0:TG], wx[:, n, :], xcb[s2][:], True, True, r=[b_wx, b_xcb[s2]], w=[pb4])
                g.act(gi[s2][:], pt4[:, 0:TG], AF.Sigmoid, r=[pb4, b_cols], w=[b_gi[s2]], bias=cols[:, n, 6:7])
                g.act(a[s2][:], gr[s2][:], AF.Exp, r=[b_gr[s2], b_c8], w=[b_a[s2]], scale=c8[:, n:n + 1])
                g.tt('pool', a2[s2][:], a[s2][:], a[s2][:], ALU.mult, r=[b_a[s2]], w=[b_a2[s2]])
                g.ts('dve', a2[s2][:], a2[s2][:], -1.0, 1.0, ALU.mult, ALU.add, r=[], w=[b_a2[s2]])
                g.act(a2[s2][:], a2[s2][:], AF.Sqrt, r=[], w=[b_a2[s2]])
                g.tt('dve', uu[s2][:], a2[s2][:], gi[s2][:], ALU.mult, r=[b_a2[s2], b_gi[s2]], w=[b_uu[s2]])
                g.tt('pool', uu[s2][:], uu[s2][:], xc[s2][:], ALU.mult, r=[b_xc[s2]], w=[b_uu[s2]])
                g.op('dve', lambda h, o=hs[s2][:], d0=a[s2][:], d1=uu[s2][:], ini=hlast[:, n:n + 1]:
                     h.tensor_tensor_scan(o, d0, d1, ini, ALU.mult, ALU.add),
                     r=[b_a[s2], b_uu[s2], b_hl[n]], w=[b_hs[s2]])
                g.cp('pool', hlast[:, n:n + 1], hs[s2][:, TG - 1:TG], r=[b_hs[s2]], w=[b_hl[n]])
                g.tt('dve' if n % 2 else 'pool', hyT[:, n, :], hs[s2][:], y[s2][:], ALU.mult,
                     r=[b_hs[s2], b_y[s2]], w=[b_hy[n]])
            for tl in range(GT):
                t = gi_ * GT + tl
                for hf in range(2):
                    for n in range(8):
                        g.mm(c['ps_out'][:, hf * 512:(hf + 1) * 512], hyT[:, n, tl * 128:(tl + 1) * 128],
                             wout[:, n, hf * 512:(hf + 1) * 512], n == 0, n == 7, r=[b_hy[n], b_wout[hf]],
                             w=[c['b_ps_out']])
                mixer_back(k, c, t, xd, b_xd)
        g.barrier()


def phase_gla(k, li, xd, b_xd, W):
    g, nc = k.g, k.nc
    NT = k.NT
    j = li // 4
    H, DK, DV = 4, 128, 256
    with contextlib.ExitStack() as es:
        win = k.sb(es, [128, 8, 3088], BF16, 'win')
        wout = k.sb(es, [128, 8, D], BF16, 'wout')
        b_win = load_w(k, win, W['gla_w_in'][j], 3088, 512)
        b_wout = load_w(k, wout, W['gla_w_out'][j], D, 512)
        wup = k.sb(es, [16, 512], F32, 'wup')
        bgate = k.sb(es, [1, 512], F32, 'bgate')
        b_wup = Buf()
        g.dma('sp', wup[:], W['gla_w_gate_up'][j], w=[b_wup])
        g.dma('sp', bgate[:], W['gla_b_gate'][j:j + 1, :], w=[b_wup])
        c = mixer_common(k, es, li, W)
        pp = c['pp']
        pt, pb = pp.next()
        gncol, b_gncol = load_cols(k, es, W['gla_gn'][j:j + 1, :], 1, pt, pb, 'gncol')
        UT = k.sb(es, [128, 128], F32, 'UT')
        VT = k.sb(es, [128, 128], F32, 'VT')
        mk = k.sb(es, [128, H, 128], F32, 'mk')
        b_c = Buf()
        g.memset('pool', UT[:], -1.0 / 16.0, w=[b_c])
        g.memset('pool', VT[:], -1.0 / 16.0, w=[b_c])
        g.memset('pool', mk[:], 1.0, w=[b_c])
        g.affsel(UT[:], UT[:], [[1, 128]], ALU.is_ge, 0.0, 0, -1, r=[b_c], w=[b_c])
        g.affsel(VT[:], VT[:], [[-1, 128]], ALU.is_gt, 0.0, 0, 1, r=[b_c], w=[b_c])
        for h in range(H):
            g.affsel(mk[:, h, :], mk[:, h, :], [[1, 128]], ALU.is_ge, 0.0, 0, -1, r=[b_c], w=[b_c])
        hT = k.sb(es, [128, 8, 128], BF16, 'hT')
        b_hT = Buf()
        glT = k.sb(es, [16, 128], F32, 'glT')
        b_glT = Buf()
        ez = k.sb(es, [128, 512], F32, 'ez')
        lsp = k.sb(es, [128, 512], F32, 'lsp')
        b_ez, b_lsp = Buf(), Buf()
        E1 = k.sb(es, [128, 512], F32, 'E1')
        E2 = k.sb(es, [128, 512], F32, 'E2')
        E3 = k.sb(es, [128, 512], F32, 'E3')
        b_E1, b_E2, b_E3 = Buf(), Buf(), Buf()
        qtT = k.sb(es, [128, H, 128], BF16, 'qtT')
        ktT = k.sb(es, [128, H, 128], BF16, 'ktT')
        khat = k.sb(es, [128, 512], BF16, 'khat')
        b_qtT, b_ktT, b_khat = Buf(), Buf(), Buf()
        v = k.sb(es, [128, 1024], BF16, 'v')
        sr = k.sb(es, [128, 1024], BF16, 'sr')
        b_v, b_sr = Buf(), Buf()
        AT = k.sb(es, [128, H, 128], BF16, 'AT')
        b_AT = Buf()
        stf = k.sb(es, [128, H, DV], F32, 'stf')
        stb = k.sb(es, [128, H, DV], BF16, 'stb')
        b_stf = [Buf() for _ in range(H)]
        b_stb = [Buf() for _ in range(H)]
        for h in range(H):
            g.memset('pool', stf[:, h, :], 0.0, w=[b_stf[h]])
            g.memset('pool', stb[:, h, :], 0.0, w=[b_stb[h]])
        ss = k.sb(es, [128, 3, H], F32, 'ss')
        b_ss = Buf()
        ontmp = c['tmp'][:, 0:128].bitcast(BF16)
        b_ontmp = c['b_tmp']
        onorm = k.sb(es, [128, 1024], BF16, 'onorm')
        b_on = Buf()
        oT = k.sb(es, [128, 8, 128], BF16, 'oT')
        b_oT = Buf()
        sq_scr = k.sb(es, [128, 256], BF16, 'sqscr')

        mixer_front(k, c, 0, xd, b_xd, hT[:], b_hT)
        for t in range(NT):
            pt_gl, pb_gl = pp.next()
            for ch in range(8):
                g.mm(pt_gl[0:16, 0:128], win[:, ch, 3072:3088], hT[:, ch, :], ch == 0, ch == 7, r=[b_win[6], b_hT],
                     w=[pb_gl])
            g.cp('act', glT[:], pt_gl[0:16, 0:128], r=[pb_gl], w=[b_glT])
            pt_z, pb_z = pp.next()
            g.mm(pt_z[:], glT[:], wup[:], True, False, r=[b_glT, b_wup], w=[pb_z])
            g.mm(pt_z[:], k.ones_f[0:1, :], bgate[:], False, True, r=[k.b_ones, b_wup], w=[pb_z])
            g.act(ez[:], pt_z[:], AF.Exp, r=[pb_z], w=[b_ez], scale=-1.0)
            g.act(lsp[:], ez[:], AF.Ln, r=[b_ez], w=[b_lsp], bias=1.0)
            pt_c, pb_c = pp.next()
            g.mm(pt_c[:], VT[:], lsp[:], True, True, r=[b_c, b_lsp], w=[pb_c])
            g.act(E3[:], pt_c[:], AF.Exp, r=[pb_c], w=[b_E3])
            pt_b, pb_b = pp.next()
            for h in range(H):
                g.mm(pt_b[:, h * 128:(h + 1) * 128], lsp[:, h * 128:(h + 1) * 128], UT[:], True, True,
                     r=[b_c, b_lsp], w=[pb_b])
            g.act(E1[:], pt_b[:], AF.Exp, r=[pb_b], w=[b_E1])
            g.act(E2[:], pt_b[:], AF.Exp, r=[pb_b], w=[b_E2], scale=-1.0)
            for which in range(2):
                pt, pb = pp.next()
                for cc in range(4):
                    col = which * 512 + cc * 128
                    for ch in range(8):
                        g.mm(pt[:, cc * 128:(cc + 1) * 128], win[:, ch, col:col + 128], hT[:, ch, :], ch == 0, ch == 7,
                             r=[b_win[col // 512], b_hT], w=[pb])
                if which == 0:
                    g.stt('dve', qtT[:].rearrange('p h t -> p (h t)'), pt[:], DK ** -0.5, E1[:], ALU.mult, ALU.mult,
                          r=[pb, b_E1], w=[b_qtT])
                else:
                    g.tt('dve', ktT[:].rearrange('p h t -> p (h t)'), pt[:], E2[:], ALU.mult, r=[pb, b_E2],
                         w=[b_ktT])
            pt, pb = pp.next()
            for ch in range(8):
                g.mm(pt[:], hT[:, ch, :], win[:, ch, 512:1024], ch == 0, ch == 7, r=[b_win[1], b_hT], w=[pb])
            g.tt('dve', khat[:], pt[:], E3[:], ALU.mult, r=[pb, b_E3], w=[b_khat])
            for blk in range(4):
                col = 1024 + blk * 512
                pt, pb = pp.next()
                for ch in range(8):
                    g.mm(pt[:], hT[:, ch, :], win[:, ch, col:col + 512], ch == 0, ch == 7,
                         r=[b_win[col // 512], b_hT], w=[pb])
                if blk < 2:
                    g.cp('act' if blk else 'dve', v[:, blk * 512:(blk + 1) * 512], pt[:], r=[pb], w=[b_v])
                else:
                    g.act(sr[:, (blk - 2) * 512:(blk - 1) * 512], pt[:], AF.Silu, r=[pb], w=[b_sr])
            if t + 1 < NT:
                mixer_front(k, c, t + 1, xd, b_xd, hT[:], b_hT)
            pt, pb = pp.next()
            for h in range(H):
                g.mm(pt[:, h * 128:(h + 1) * 128], ktT[:, h, :], qtT[:, h, :], True, True, r=[b_ktT, b_qtT], w=[pb])
            g.tt('dve', AT[:].rearrange('p h t -> p (h t)'), pt[:], mk[:].rearrange('p h t -> p (h t)'), ALU.mult,
                 r=[pb, b_c], w=[b_AT])
            o_banks = [pp.next(), pp.next()]
            for h in range(H):
                pt, pb = o_banks[h // 2]
                osl = pt[:, (h % 2) * 256:(h % 2 + 1) * 256]
                g.mm(osl, AT[:, h, :], v[:, h * 256:(h + 1) * 256], True, False, r=[b_AT, b_v], w=[pb])
                g.mm(osl, qtT[:, h, :], stb[:, h, :], False, True, r=[b_qtT, b_stb[h]], w=[pb])
                g.act(sq_scr[:], osl, AF.Square, r=[pb], w=[b_ss], accum_out=ss[:, 0, h:h + 1])
            g.ts('dve', ss[:, 1, :], ss[:, 0, :], 1.0 / DV, EPS, ALU.mult, ALU.add, r=[b_ss], w=[b_ss])
            g.act(ss[:, 2, :], ss[:, 1, :], AF.Sqrt, r=[b_ss], w=[b_ss])
            g.op('dve', lambda hh: hh.reciprocal(ss[:, 1, :], ss[:, 2, :]), r=[b_ss], w=[b_ss])
            for h in range(H):
                pt, pb = o_banks[h // 2]
                osl = pt[:, (h % 2) * 256:(h % 2 + 1) * 256]
                g.act(ontmp, osl, AF.Identity, r=[pb, b_ss], w=[b_ontmp], scale=ss[:, 1, h:h + 1])
                g.tt('pool', onorm[:, h * 256:(h + 1) * 256], ontmp, sr[:, h * 256:(h + 1) * 256], ALU.mult,
                     r=[b_ontmp, b_sr], w=[b_on])
                pt2, pb2 = pp.next()
                g.mm(pt2[:, 0:256], khat[:, h * 128:(h + 1) * 128], v[:, h * 256:(h + 1) * 256], True, True,
                     r=[b_khat, b_v], w=[pb2])
                g.stt('dve', stf[:, h, :], stf[:, h, :], E1[:, h * 128 + 127:h * 128 + 128], pt2[:, 0:256], ALU.mult,
                      ALU.add, r=[pb2, b_E1], w=[b_stf[h]])
                g.cp('act', stb[:, h, :], stf[:, h, :], r=[b_stf[h]], w=[b_stb[h]])
            pt, pb = pp.next()
            ptb = pt[:].bitcast(BF16)
            for cc in range(8):
                g.tr(ptb[:, cc * 128:(cc + 1) * 128], onorm[:, cc * 128:(cc + 1) * 128], k.ident_b[:],
                     r=[b_on, k.b_ident], w=[pb])
            g.tt('dve', oT[:], ptb.rearrange('p (c t) -> p c t', c=8),
                 gncol[:, :, 0].unsqueeze(2).to_broadcast([128, 8, 128]), ALU.mult, r=[pb, b_gncol], w=[b_oT])
            for hf in range(2):
                for ec in range(8):
                    g.mm(c['ps_out'][:, hf * 512:(hf + 1) * 512], oT[:, ec, :], wout[:, ec, hf * 512:(hf + 1) * 512],
                         ec == 0, ec == 7, r=[b_oT, b_wout[hf]], w=[c['b_ps_out']])
            mixer_back(k, c, t, xd, b_xd)
        g.barrier()


def bf16_round(x):
    u = np.ascontiguousarray(x, dtype=np.float32).view(np.uint32).astype(np.uint64)
    r = ((u + 0x7FFF + ((u >> 16) & 1)) & 0xFFFF0000).astype(np.uint32)
    return r.view(np.float32)


def hilo(x):
    hi = bf16_round(x)
    lo = bf16_round(np.asarray(x, dtype=np.float32) - hi)
    return hi, lo


def nsa_tables(NT):
    H = 16
    slopes = np.exp2(-8.0 * (np.arange(H, dtype=np.float64) + 1.0) / H).astype(np.float32)
    ip = np.arange(128, dtype=np.float32)
    qA = np.zeros((6, H, 128), np.float32)
    for h in range(H):
        a, b = hilo(np.full(128, -8.0 * 128.0 * slopes[h], np.float32))
        qA[0, h], qA[1, h] = a, b
        a, b = hilo(-8.0 * slopes[h] * ip)
        qA[2, h], qA[3, h] = a, b
        a, b = hilo(np.full(128, 8.0 * slopes[h], np.float32))
        qA[4, h], qA[5, h] = a, b
    kA = np.zeros((6, 33, 128), np.float32)
    for d in range(33):
        kA[0, d] = kA[1, d] = d
        kA[2, d] = kA[3, d] = 1.0
        kA[4, d] = kA[5, d] = ip
    s_ = np.arange(256)
    ce = 16 * s_ + 15
    b_, a_ = ce // 128, ce % 128
    kAc = np.zeros((6, NT, 256), np.float32)
    for T in range(NT):
        kAc[0, T] = kAc[1, T] = np.where(b_ > T, 64, T - b_)
        kAc[2, T] = kAc[3, T] = 1.0
        kAc[4, T] = kAc[5, T] = a_
    n_ = s_ - 1
    j_ = np.arange(64)
    ovl = ((16 * n_[:, None] < 64 * j_[None, :] + 64) & (16 * n_[:, None] + 32 > 64 * j_[None, :]) &
           (n_[:, None] >= 0)).astype(np.float32)
    return {'c_qA': qA, 'c_kA': kA, 'c_kAc': kAc, 'c_ovl': ovl}


def phase_nsa(k, li, xd, b_xd, W):
    g, nc = k.g, k.nc
    NT = k.NT
    S = k.S
    j = li // 4
    G, HPG, DK = 2, 8, 64
    with contextlib.ExitStack() as es:
        win = k.sb(es, [128, 8, 1840], BF16, 'win')
        wout = k.sb(es, [128, 8, D], BF16, 'wout')
        b_win = load_w(k, win, W['nsa_w_in'][j], 1840, 460)
        b_wout = load_w(k, wout, W['nsa_w_out'][j], D, 512)
        bw_all = b_win
        w1 = [k.sb(es, [64, 32, 128], BF16, 'w1') for _ in range(2)]
        w2 = [k.sb(es, [128, 64], BF16, 'w2') for _ in range(2)]
        pe = [k.sb(es, [32, 64], F32, 'pe') for _ in range(2)]
        b_w1 = Buf()
        for i, nm in enumerate(['k', 'v']):
            g.dma('pool', w1[i][:], W['nsa_w1_' + nm][j].rearrange('(l d) f -> d l f', d=64), w=[b_w1])
            g.dma('pool', w2[i][:], W['nsa_w2_' + nm][j], w=[b_w1])
            g.dma('sp', pe[i][:], W['nsa_pe_' + nm][j], w=[b_w1])
        qA = k.sb(es, [6, 16 * 128], BF16, 'qA')
        kA = k.sb(es, [6, 33 * 128], BF16, 'kA')
        kAc = k.sb(es, [6, NT * 256], BF16, 'kAc')
        ovl = k.sb(es, [128, 2, 64], BF16, 'ovl')
        b_tab = Buf()
        g.dma('pool', qA[:], W['c_qA'].rearrange('r h i -> r (h i)'), w=[b_tab])
        g.dma('pool', kA[:], W['c_kA'].rearrange('r d i -> r (d i)'), w=[b_tab])
        g.dma('pool', kAc[:], W['c_kAc'].rearrange('r t s -> r (t s)'), w=[b_tab])
        g.dma('pool', ovl[:], W['c_ovl'].rearrange('(n p) j -> p n j', p=128), w=[b_tab])
        c = mixer_common(k, es, li, W, npool=4)
        pp = c['pp']
        tri4 = k.sb(es, [128, 512], F32, 'tri4')
        triu4 = k.sb(es, [128, 512], F32, 'triu4')
        Ex = k.sb(es, [64, NT, 128], BF16, 'Ex')
        zrow = k.sb(es, [1, 512], BF16, 'zrow')
        b_c = Buf()
        g.memset('pool', tri4[:], 1.0, w=[b_c])
        g.memset('pool', triu4[:], 1.0, w=[b_c])
        g.memset('pool', Ex[:], 1.0, w=[b_c])
        g.memset('pool', zrow[:], 0.0, w=[b_c])
        g.affsel(tri4[:], tri4[:], [[0, 4], [1, 128]], ALU.is_ge, 0.0, 0, -1, r=[b_c], w=[b_c])
        g.affsel(triu4[:], triu4[:], [[0, 4], [-1, 128]], ALU.is_gt, 0.0, 0, 1, r=[b_c], w=[b_c])
        for hf in range(2):
            g.affsel(Ex[:, :, hf * 64:(hf + 1) * 64], Ex[:, :, hf * 64:(hf + 1) * 64],
                                                            [[-2, NT], [0, 64]], ALU.is_equal, 0.0, -hf, 1, r=[b_c], w=[b_c])
        Mw = k.sb(es, [128, 2304], BF16, 'Mw')
        Vw = k.sb(es, [128, 128], F32, 'Vw')
        Aw = k.sb(es, [128, 128], F32, 'Aw')
        g.memset('pool', Mw[:], 1.0, w=[b_c])
        g.memset('pool', Vw[:], 1.0, w=[b_c])
        g.memset('pool', Aw[:], 0.0, w=[b_c])
        g.affsel(Mw[:], Mw[:], [[1, 2304]], ALU.is_ge, 0.0, -15, -16, r=[b_c], w=[b_c])
        for hf in range(2):
            rows = slice(hf * 64, (hf + 1) * 64)
            g.affsel(Vw[rows, :], Vw[rows, :], [[-1, 128]], ALU.is_ge, 0.0, 64 + hf, 0, r=[b_c], w=[b_c])
            g.affsel(Aw[rows, :], Aw[rows, :], [[-1, 128]], ALU.is_ge, -1.0, 64 + hf, 0, r=[b_c], w=[b_c])
            g.affsel(Aw[rows, :], Aw[rows, :], [[1, 128]], ALU.not_equal, 1e6, -(64 + hf), 0, r=[b_c], w=[b_c])
            g.affsel(Aw[rows, :], Aw[rows, :], [[1, 128]], ALU.not_equal, 1e6, -(63 + hf), 0, r=[b_c], w=[b_c])
        kcT = [[k.sb(es, [64, 160], BF16, 'kcT') for _ in range(G)] for _ in range(2)]
        b_kc = Buf()
        for i in range(2):
            for gg in range(G):
                g.memset('pool', kcT[i][gg][:], 0.0, w=[b_kc])
        ksT = [k.sb(es, [64, S], BF16, 'ksT') for _ in range(G)]
        kwT = [k.sb(es, [64, 5 * 128], BF16, 'kwT') for _ in range(G)]
        b_kw = [Buf() for _ in range(5)]
        b_ks = [Buf() for _ in range(NT)]
        vs = k.sb(es, [128, NT, G, 65], BF16, 'vs')
        vw = k.sb(es, [128, 5, G, 65], BF16, 'vw')
        b_vs = [Buf() for _ in range(NT)]
        b_v1 = Buf()
        g.memset('pool', vs[:], 1.0, w=[b_v1])
        g.memset('pool', vw[:], 1.0, w=[b_v1])
        hidT = [[k.sb(es, [128, 256], BF16, 'hidT') for _ in range(G)] for _ in range(2)]
        b_hid = Buf()
        for i in range(2):
            for gg in range(G):
                g.memset('pool', hidT[i][gg][:], 0.0, w=[b_hid])
        kcmpT = [k.sb(es, [64, 256], BF16, 'kcmpT') for _ in range(G)]
        b_kcmp = Buf()
        for gg in range(G):
            g.memset('pool', kcmpT[gg][:], 0.0, w=[b_kcmp])
        vcmp = k.sb(es, [128, 2, G, 65], BF16, 'vcmp')
        b_vcmp = Buf()
        g.memset('pool', vcmp[:], 1.0, w=[b_vcmp])
        c1 = k.sb(es, [128, 2], F32, 'c1')
        peT = k.sb(es, [64, 2, 32], BF16, 'peT')
        b_c1 = Buf()
        for i in range(2):
            pt, pb = pp.next()
            g.tr(pt[0:64, 0:32], pe[i][:], k.ident_f[0:32, 0:32], r=[b_w1, k.b_ident], w=[pb])
            g.cp('dve', peT[:, i, :], pt[0:64, 0:32], r=[pb], w=[b_c1])
        for i in range(2):
            pt, pb = pp.next()
            for l in range(32):
                g.mm(pt[:, 0:1], w1[i][:, l, :], peT[:, i, l:l + 1], l == 0, l == 31, r=[b_w1, b_c1], w=[pb])
            g.cp('dve', c1[:, i:i + 1], pt[:, 0:1], r=[pb], w=[b_c1])
        hT = k.sb(es, [128, 8, 128], BF16, 'hT')
        b_hT = Buf()
        qT = [k.sb(es, [64, 8, 128], BF16, 'qT') for _ in range(G)]
        b_qT = Buf()
        gts = k.sb(es, [128, 48], F32, 'gts')
        b_gts = Buf()
        hsm = [k.sb(es, [128, 8], F32, 'hsm') for _ in range(5)]
        b_hsm = Buf()
        mask4 = [k.sb(es, [128, 512], BF16, 'mask4') for _ in range(2)]
        b_m4 = Buf()
        e16 = [k.sb(es, [128, 512], BF16, 'e16') for _ in range(2)]
        b_e16 = [Buf() for _ in range(2)]
        e32 = [k.sb(es, [128, 512], F32, 'e32') for _ in range(2)]
        b_e32 = [Buf() for _ in range(2)]
        pc = [k.sb(es, [128, 8, 128], BF16, 'pc') for _ in range(2)]
        b_pc = Buf()
        pT = [k.sb(es, [128, 512], BF16, 'pT') for _ in range(3)]
        b_pT = [Buf() for _ in range(3)]
        imp = k.sb(es, [128, 64], F32, 'imp')
        imp2 = k.sb(es, [128, 64], F32, 'imp2')
        m8 = k.sb(es, [128, 16], F32, 'm8')
        selm = k.sb(es, [128, 64], F32, 'selm')
        b_imp = Buf()
        selT4 = k.sb(es, [64, 4, 128], BF16, 'selT4')
        b_selT = Buf()
        rec = k.sb(es, [128, 3, 8], F32, 'rec')
        b_rec = Buf()
        og = k.sb(es, [128, 1024], F32, 'og')
        b_og = Buf()
        ogb = k.sb(es, [128, 1024], BF16, 'ogb')
        b_ogb = Buf()
        oT = k.sb(es, [128, 8, 128], BF16, 'oT')
        b_oT = Buf()
        acc = [k.ps(es, [128, 512], F32, 'acc') for _ in range(2)]
        b_acc = [Buf(True) for _ in range(2)]
        npt = [0]

        def zero_bank(t_, b_):
            g.mm(t_[:], zrow[0:1, 0:128], zrow[0:1, :], True, False, r=[b_c], w=[b_], skip=True)

        def gelu_small(src_ps, bias_ap, dst, n, rlist, wbuf):
            xs, us, ss_ = hsm[0], hsm[1], hsm[2]
            g.act(xs[:, 0:n], src_ps, AF.Identity, r=rlist + [b_c1], w=[b_hsm], bias=bias_ap)
            g.tt('dve', us[:, 0:n], xs[:, 0:n], xs[:, 0:n], ALU.mult, r=[b_hsm], w=[b_hsm])
            g.ts('dve', us[:, 0:n], us[:, 0:n], 0.044715, 1.0, ALU.mult, ALU.add, r=[b_hsm], w=[b_hsm])
            g.tt('dve', us[:, 0:n], us[:, 0:n], xs[:, 0:n], ALU.mult, r=[b_hsm], w=[b_hsm])
            g.act(ss_[:, 0:n], us[:, 0:n], AF.Sigmoid, r=[b_hsm], w=[b_hsm], scale=1.5957691216057308)
            g.tt('dve', dst, ss_[:, 0:n], xs[:, 0:n], ALU.mult, r=[b_hsm], w=[wbuf])

        def qk_exp(T, kt, gg, half, k_ap, b_kcache):
            pt, pb = pp.next()
            hs0 = gg * 8 + half * 4
            g.mm(pt[:], k_ap, qT[gg][:, half * 4:(half + 1) * 4, :].rearrange('p a b -> p (a b)'),
                 True, False, r=[b_kcache, b_qT], w=[pb])
            d = T - kt
            g.mm(pt[:], kA[:, d * 128:(d + 1) * 128], qA[:, hs0 * 128:(hs0 + 4) * 128], False, True, r=[b_tab], w=[pb])
            return pt, pb

        mixer_front(k, c, 0, xd, b_xd, hT[:], b_hT)
        for T in range(NT):
            g.mark('nsa tile %d' % T)
            for gg in range(G):
                for half in range(2):
                    pt, pb = pp.next()
                    for hh in range(4):
                        h = gg * 8 + half * 4 + hh
                        for ch in range(8):
                            g.mm(pt[0:64, hh * 128:(hh + 1) * 128], win[:, ch, h * 64:(h + 1) * 64], hT[:, ch, :], ch == 0,
                                 ch == 7, r=bw_all + [b_hT], w=[pb])
                    g.cp('act' if half else 'dve', qT[gg][:, half * 4:(half + 1) * 4, :].rearrange('p a b -> p (a b)'),
                         pt[0:64, :], r=[pb], w=[b_qT])
            for idx, (col0, dst, off) in enumerate([(1024, kcT[0], 16), (1152, kcT[1], 16), (1280, ksT, 0), (1536, kwT, 0)]):
                pt, pb = pp.next()
                for gg in range(G):
                    for ch in range(8):
                        g.mm(pt[0:64, gg * 128:(gg + 1) * 128], win[:, ch, col0 + gg * 64:col0 + (gg + 1) * 64], hT[:, ch, :],
                             ch == 0, ch == 7, r=bw_all + [b_hT], w=[pb])
                for gg in range(G):
                    if idx < 2:
                        wb, c0 = b_kc, 16
                    elif idx == 2:
                        wb, c0 = b_ks[T], T * 128
                    else:
                        wb, c0 = b_kw[T % 5], (T % 5) * 128
                    g.cp('act' if gg else 'dve', dst[gg][:, c0:c0 + 128],
                         pt[0:64, gg * 128:(gg + 1) * 128], r=[pb], w=[wb])
            pt, pb = pp.next()
            for ch in range(8):
                g.mm(pt[:, 0:128], hT[:, ch, :], win[:, ch, 1408:1536], ch == 0, ch == 7, r=bw_all + [b_hT], w=[pb])
            for ch in range(8):
                g.mm(pt[:, 128:256], hT[:, ch, :], win[:, ch, 1664:1792], ch == 0, ch == 7, r=bw_all + [b_hT], w=[pb])
            for ch in range(8):
                g.mm(pt[:, 256:304], hT[:, ch, :], win[:, ch, 1792:1840], ch == 0, ch == 7, r=bw_all + [b_hT], w=[pb])
            for gg in range(G):
                g.cp('dve', vs[:, T, gg, 0:64], pt[:, gg * 64:(gg + 1) * 64], r=[pb, b_v1], w=[b_vs[T]])
                g.cp('act', vw[:, T % 5, gg, 0:64], pt[:, 128 + gg * 64:128 + (gg + 1) * 64], r=[pb, b_v1], w=[b_kw[T % 5]])
            g.act(gts[:], pt[:, 256:304], AF.Sigmoid, r=[pb], w=[b_gts])
            if T + 1 < NT:
                mixer_front(k, c, T + 1, xd, b_xd, hT[:], b_hT)
            for i in range(2):
                for gg in range(G):
                    pt, pb = pp.next()
                    for l in range(32):
                        rhs = kcT[i][gg][:, l:l + 128].rearrange('p (m s) -> p m s', s=16)[:, :, 0]
                        g.mm(pt[:, 0:8], w1[i][:, l, :], rhs, l == 0, l == 31, r=[b_w1, b_kc], w=[pb])
                    gelu_small(pt[:, 0:8], c1[:, i:i + 1], hidT[i][gg][:, 8 * T:8 * T + 8], 8, [pb], b_hid)
            for i in range(2):
                for gg in range(G):
                    g.cp('dve', kcT[i][gg][:, 0:16], kcT[i][gg][:, 128:144], r=[], w=[b_kc])
            for gg in range(G):
                pt, pb = pp.next()
                g.mm(pt[0:64, 0:8], w2[0][:], hidT[0][gg][:, 8 * T:8 * T + 8], True, True, r=[b_w1, b_hid], w=[pb])
                g.cp('dve', kcmpT[gg][:, 8 * T:8 * T + 8], pt[0:64, 0:8], r=[pb], w=[b_kcmp])
            ntl = [0] if 8 * T + 7 < 128 else [0, 1]
            pt, pb = pp.next()
            for nt_ in ntl:
                for gg in range(G):
                    sl = pt[:, (nt_ * 2 + gg) * 64:(nt_ * 2 + gg + 1) * 64]
                    g.mm(sl, hidT[1][gg][:, nt_ * 128:(nt_ + 1) * 128], w2[1][:], True, True, r=[b_w1, b_hid], w=[pb])
            for nt_ in ntl:
                for gg in range(G):
                    sl = pt[:, (nt_ * 2 + gg) * 64:(nt_ * 2 + gg + 1) * 64]
                    g.cp('dve', vcmp[:, nt_, gg, 0:64], sl, r=[pb], w=[b_vcmp])
            for gg in range(G):
                g.mark('nsa T%d g%d' % (T, gg))
                for nt_ in ntl:
                    off = min(128 * T - 2048 * nt_, 2176)
                    for rr_ in range(4):
                        g.cp('pool' if rr_ % 2 else 'act', mask4[nt_][:, rr_ * 128:(rr_ + 1) * 128], Mw[:, off:off + 128],
                             r=[b_c], w=[b_m4])
                    if nt_ == 0:
                        g.memset('pool', mask4[0][0:1, :], 0.0, w=[b_m4])
                    for half in range(2):
                        pt, pb = pp.next()
                        hs0 = gg * 8 + half * 4
                        g.mm(pt[:], kcmpT[gg][:, nt_ * 128:(nt_ + 1) * 128],
                             qT[gg][:, half * 4:(half + 1) * 4, :].rearrange('p a b -> p (a b)'), True, False,
                             r=[b_kcmp, b_qT], w=[pb])
                        g.mm(pt[:], kAc[:, T * 256 + nt_ * 128:T * 256 + (nt_ + 1) * 128],
                             qA[:, hs0 * 128:(hs0 + 4) * 128], False, True, r=[b_tab], w=[pb])
                        ei = half
                        g.act(e16[ei][:], pt[:], AF.Exp, r=[pb], w=[b_e16[ei]], scale=0.125, bias=-30.0)
                        g.tt('dve', pc[nt_][:, half * 4:(half + 1) * 4, :].rearrange('p a b -> p (a b)'), e16[ei][:],
                             mask4[nt_][:], ALU.mult, r=[b_e16[ei], b_m4], w=[b_pc])
                pu, pbu = pp.next()
                for hf in range(2):
                    zero_bank(acc[hf], b_acc[hf])
                for hp in range(8):
                    for ii, nt_ in enumerate(ntl):
                        last = ii == len(ntl) - 1
                        g.mm(acc[hp // 4][:, (hp % 4) * 65:(hp % 4) * 65 + 65], pc[nt_][:, hp, :], vcmp[:, nt_, gg, :],
                             False, False, r=[b_pc, b_vcmp], w=[b_acc[hp // 4]], skip=True)
                    for ii, nt_ in enumerate(ntl):
                        g.mm(pu[:, hp * 64:(hp + 1) * 64], pc[nt_][:, hp, :], ovl[:, nt_, :], ii == 0,
                             ii == len(ntl) - 1, r=[b_pc, b_tab], w=[pbu])
                for hf in range(2):
                    den = acc[hf][:, 0:260].rearrange('p (h c) -> p h c', c=65)[:, :, 64]
                    g.ts('dve', rec[:, 0, hf * 4:(hf + 1) * 4], den, 1e-30, None, ALU.max, r=[b_acc[hf]], w=[b_rec])
                g.op('dve', lambda hh: hh.reciprocal(rec[:, 0, :], rec[:, 0, :]), r=[b_rec], w=[b_rec])
                for hp in range(8):
                    if hp == 0:
                        g.ts('dve', imp[:], pu[:, 0:64], rec[:, 0, 0:1], None, ALU.mult, r=[pbu, b_rec], w=[b_imp])
                    else:
                        g.stt('dve', imp[:], pu[:, hp * 64:(hp + 1) * 64], rec[:, 0, hp:hp + 1], imp[:], ALU.mult, ALU.add,
                              r=[pbu, b_rec], w=[b_imp])
                for hp in range(8):
                    h = gg * 8 + hp
                    g.tt('dve', hsm[3][:, hp:hp + 1], rec[:, 0, hp:hp + 1], gts[:, h:h + 1], ALU.mult,
                         r=[b_rec, b_gts], w=[b_hsm])
                    g.ts('dve', og[:, h * 64:(h + 1) * 64],
                         acc[hp // 4][:, (hp % 4) * 65:(hp % 4) * 65 + 64], hsm[3][:, hp:hp + 1], None, ALU.mult,
                         r=[b_acc[hp // 4], b_hsm], w=[b_og])
                g.tt('dve', imp[:], imp[:], Vw[:, 64 - 2 * T:128 - 2 * T], ALU.mult, r=[b_c], w=[b_imp])
                g.tt('dve', imp[:], imp[:], Aw[:, 64 - 2 * T:128 - 2 * T], ALU.add, r=[b_c], w=[b_imp])
                g.memset('pool', imp[:, 0:1], 1e6, w=[b_imp])
                g.op('dve', lambda hh: hh.max(m8[:, 0:8], imp[:]), r=[], w=[b_imp])
                g.op('dve', lambda hh: hh.match_replace(imp2[:], m8[:, 0:8], imp[:], -3.0e38), r=[], w=[b_imp])
                g.op('dve', lambda hh: hh.max(m8[:, 8:16], imp2[:]), r=[], w=[b_imp])
                g.ts('dve', selm[:], imp[:], m8[:, 15:16], None, ALU.is_ge, r=[], w=[b_imp])
                pt, pb = pp.next()
                g.tr(pt[0:64, 0:128], selm[:], k.ident_f[:], r=[b_imp, k.b_ident], w=[pb])
                for rr_ in range(4):
                    g.cp('dve' if rr_ % 2 else 'act', selT4[:, rr_, :], pt[0:64, 0:128], r=[pb], w=[b_selT])
                for br, kts in enumerate([list(range(0, T + 1)), list(range(max(0, T - 4), T + 1))]):
                    g.mark('nsa T%d g%d br%d' % (T, gg, br))
                    for hf in range(2):
                        zero_bank(acc[hf], b_acc[hf])
                    for ki, kt in enumerate(kts):
                        d = T - kt
                        lastk = ki == len(kts) - 1
                        if br == 0 and d >= 1:
                            pm, pbm = pp.next()
                            g.mm(pm[:], Ex[:, kt, :], selT4[:].rearrange('p a b -> p (a b)'), True, True,
                                 r=[b_c, b_selT], w=[pbm])
                        for half in range(2):
                            if br == 0:
                                k_ap, bk, v_ap, bv = ksT[gg][:, kt * 128:(kt + 1) * 128], b_ks[kt], vs[:, kt, gg, :], b_vs[kt]
                            else:
                                sl5 = kt % 5
                                k_ap, bk, v_ap, bv = kwT[gg][:, sl5 * 128:(sl5 + 1) * 128], b_kw[sl5], vw[:, sl5, gg, :], b_kw[sl5]
                            pt, pb = qk_exp(T, kt, gg, half, k_ap, bk)
                            pi = npt[0] % 3
                            npt[0] += 1
                            if br == 1 and 1 <= d <= 3:
                                g.act(pT[pi][:], pt[:], AF.Exp, r=[pb], w=[b_pT[pi]], scale=0.125, bias=-30.0)
                            else:
                                ei = half
                                g.act(e32[ei][:], pt[:], AF.Exp, r=[pb], w=[b_e32[ei]], scale=0.125, bias=-30.0)
                                if d == 0:
                                    g.tt('dve', pT[pi][:], e32[ei][:], tri4[:], ALU.mult, r=[b_e32[ei], b_c], w=[b_pT[pi]])
                                elif br == 1:
                                    g.tt('dve', pT[pi][:], e32[ei][:], triu4[:], ALU.mult, r=[b_e32[ei], b_c],
                                         w=[b_pT[pi]])
                                else:
                                    g.tt('dve', pT[pi][:], e32[ei][:], pm[:], ALU.mult, r=[b_e32[ei], pbm], w=[b_pT[pi]])
                            for hh in range(4):
                                g.mm(acc[half][:, hh * 65:hh * 65 + 65], pT[pi][:, hh * 128:(hh + 1) * 128],
                                     v_ap, False, False, r=[b_pT[pi], bv], w=[b_acc[half]], skip=True)
                    bi = br + 1
                    for hf in range(2):
                        den = acc[hf][:, 0:260].rearrange('p (h c) -> p h c', c=65)[:, :, 64]
                        g.ts('dve', rec[:, bi, hf * 4:(hf + 1) * 4], den, 1e-30, None, ALU.max, r=[b_acc[hf]], w=[b_rec])
                    g.op('dve', lambda hh, bi=bi: hh.reciprocal(rec[:, bi, :], rec[:, bi, :]), r=[b_rec], w=[b_rec])
                    for hp in range(8):
                        h = gg * 8 + hp
                        g.tt('dve', hsm[4][:, hp:hp + 1], rec[:, bi, hp:hp + 1], gts[:, bi * 16 + h:bi * 16 + h + 1],
                             ALU.mult, r=[b_rec, b_gts], w=[b_hsm])
                        g.stt('dve', og[:, h * 64:(h + 1) * 64], acc[hp // 4][:, (hp % 4) * 65:(hp % 4) * 65 + 64],
                              hsm[4][:, hp:hp + 1], og[:, h * 64:(h + 1) * 64], ALU.mult, ALU.add,
                              r=[b_acc[hp // 4], b_hsm], w=[b_og])
            g.cp('act', ogb[:], og[:], r=[b_og], w=[b_ogb])
            pt, pb = pp.next()
            ptb = pt[:].bitcast(BF16)
            for cc in range(8):
                g.tr(ptb[:, cc * 128:(cc + 1) * 128], ogb[:, cc * 128:(cc + 1) * 128], k.ident_b[:],
                     r=[b_ogb, k.b_ident], w=[pb])
            g.cp('dve', oT[:].rearrange('p a b -> p (a b)'), ptb, r=[pb], w=[b_oT])
            for hf in range(2):
                for ec in range(8):
                    g.mm(c['ps_out'][:, hf * 512:(hf + 1) * 512], oT[:, ec, :], wout[:, ec, hf * 512:(hf + 1) * 512],
                         ec == 0, ec == 7, r=[b_oT, b_wout[hf]], w=[c['b_ps_out']])
            mixer_back(k, c, T, xd, b_xd)
        g.barrier()


PARAM_SHAPES = {
    'norm_mix_pre': (DEPTH, D), 'norm_mix_post': (DEPTH, D), 'norm_mlp_pre': (DEPTH, D), 'norm_mlp_post': (DEPTH, D),
    'mlp_w_up': (DEPTH, D, DFF), 'mlp_w_down': (DEPTH, DFF, D),
    'ret_w_in': (1, D, 6144), 'ret_gn': (1, 2048), 'ret_w_out': (1, 2048, D),
    'gla_w_in': (1, D, 3088), 'gla_w_gate_up': (1, 16, 512), 'gla_b_gate': (1, 512), 'gla_gn': (1, 1024),
    'gla_w_out': (1, 1024, D),
    'lru_w_in': (1, D, 2048), 'lru_conv_w': (1, 4, 1024), 'lru_conv_b': (1, 1024), 'lru_w_a': (1, 8, 128, 128),
    'lru_b_a': (1, 1024), 'lru_w_x': (1, 8, 128, 128), 'lru_b_x': (1, 1024), 'lru_lambda': (1, 1024),
    'lru_w_out': (1, 1024, D),
    'nsa_w_in': (1, D, 1840), 'nsa_pe_k': (1, 32, 64), 'nsa_w1_k': (1, 2048, 128), 'nsa_w2_k': (1, 128, 64),
    'nsa_pe_v': (1, 32, 64), 'nsa_w1_v': (1, 2048, 128), 'nsa_w2_v': (1, 128, 64), 'nsa_w_out': (1, 1024, D),
}


def build(S, phases):
    nc = bass.Bass('TRN2', target_bir_lowering=False)
    x_in = nc.dram_tensor('x', [S, D], F32, kind='ExternalInput').ap()
    W = {}
    for name, shp in PARAM_SHAPES.items():
        W[name] = nc.dram_tensor(name, list(shp), F32, kind='ExternalInput').ap()
    if any(kind == 'mix' and li % 4 == 3 for kind, li in phases):
        for name, arr in nsa_tables(S // 128).items():
            W[name] = nc.dram_tensor(name, list(arr.shape), F32, kind='ExternalInput').ap()
    y = nc.dram_tensor('y', [S, D], F32, kind='ExternalOutput').ap()
    with contextlib.ExitStack() as es:
        g = G(nc, es)
        k = K(nc, g, es, S)
        setup_consts(k)
        NT = S // 128
        b_xd = [Buf() for _ in range(NT)]
        for t in range(NT):
            g.dma('sp', y[t * 128:(t + 1) * 128, :], x_in[t * 128:(t + 1) * 128, :], w=[b_xd[t]])
        for kind, li in phases:
            if kind == 'mlp':
                phase_mlp(k, li, y, b_xd, W)
            else:
                MIXERS[li % 4](k, li, y, b_xd, W)
        g.barrier()
        g.emit()
    return nc


MIXERS = {0: phase_ret, 1: phase_gla, 2: phase_lru, 3: phase_nsa}

ALL_PHASES = [(kind, li) for li in range(DEPTH) for kind in ('mix', 'mlp')]


def run(inputs, S, phases):
    nc = build(S, phases)
    B = inputs['x'].shape[0]
    in_maps = []
    tabs = nsa_tables(S // 128) if any(kind == 'mix' and li % 4 == 3 for kind, li in phases) else {}
    for b in range(B):
        m = {'x': np.ascontiguousarray(inputs['x'][b], dtype=np.float32)}
        for name in PARAM_SHAPES:
            m[name] = np.ascontiguousarray(inputs[name], dtype=np.float32)
        m.update(tabs)
        in_maps.append(m)
    res = run_bass_kernel_spmd(nc, in_maps, core_ids=list(range(B)))
    return np.stack([np.asarray(r['y']) for r in res.results], axis=0)


def kernel(**inputs):
    out = run(inputs, 4096, ALL_PHASES)
    return out.astype(np.float32)
```

```python
import contextlib
import numpy as np
import concourse.bass as bass
import concourse.mybir as mybir
from concourse.bass_utils import run_bass_kernel_spmd

F32 = mybir.dt.float32
BF16 = mybir.dt.bfloat16
AF = mybir.ActivationFunctionType
ALU = mybir.AluOpType
AX = mybir.AxisListType

D = 1024
DFF = 4096
DEPTH = 4
EPS = 1e-6
ENG = ['pe', 'act', 'dve', 'pool', 'sp']
NDSEM = 16


class Buf:
    __slots__ = ('w', 'r', 'x')

    def __init__(self, x=False):
        self.w = None
        self.r = {}
        self.x = x


class Op:
    __slots__ = ('eng', 'fn', 'waits', 'idx', 'inc', 'semval', 'dma', 'dsem', 'dval')


class G:
    def __init__(self, nc, es):
        self.nc = nc
        self.q = {e: [] for e in ENG}
        self.waited = {e: {} for e in ENG}
        self.sem = {e: es.enter_context(nc.semaphore('s_' + e)) for e in ENG}
        self.dq = {}
        for e in ('sp', 'pool', 'act'):
            self.dq[e] = [[es.enter_context(nc.semaphore('d_%s%d' % (e, i))) for i in range(NDSEM)], 0]
        self.uid = 0
        self.fill_regs = {}
        import os
        self.max_ops = int(os.environ.get('KMAX', '100000000'))

    def _rawwait(self, o, sem, val):
        key = id(sem)
        if val <= self.waited[o.eng].get(key, 0):
            return
        self.waited[o.eng][key] = val
        o.waits.append(('raw', sem, val))

    def _wait(self, o, d):
        if d.dma:
            self._rawwait(o, d.dsem, d.dval)
            return
        if d.eng == o.eng and o.eng == 'pe':
            return
        if d.idx <= self.waited[o.eng].get(d.eng, -1):
            return
        self.waited[o.eng][d.eng] = d.idx
        d.inc = True
        o.waits.append(('op', d))

    def op(self, eng, fn, r=(), w=(), dma=False):
        if self.uid >= self.max_ops:
            return None
        o = Op()
        o.eng = eng
        o.fn = fn
        o.waits = []
        o.idx = len(self.q[eng])
        o.inc = False
        o.dma = dma
        o.semval = 0
        if any(b.x for b in r):
            w = list(w) + [b for b in r if b.x and b not in w]
            r = [b for b in r if not b.x]
        for b in r:
            if b.w is not None:
                self._wait(o, b.w)
        for b in w:
            if b.w is not None:
                self._wait(o, b.w)
            for d in b.r.values():
                self._wait(o, d)
        if dma:
            sems, cnt = self.dq[eng]
            k = cnt % len(sems)
            o.dsem = sems[k]
            o.dval = 16 * (cnt // len(sems) + 1)
            self.dq[eng][1] = cnt + 1
            if o.dval > 16:
                self._rawwait(o, o.dsem, o.dval - 16)
        self.q[eng].append(o)
        self.uid += 1
        key = ('d', self.uid) if dma else eng
        for b in r:
            b.r[key] = o
        for b in w:
            b.w = o
            b.r = {}
        return o

    def barrier(self):
        last = {}
        for e in ENG:
            last[e] = None
            for o in reversed(self.q[e]):
                if o.fn is not None:
                    last[e] = o
                    break
        dlast = []
        for e in self.dq:
            sems, cnt = self.dq[e]
            for k in range(len(sems)):
                n = (cnt - k + len(sems) - 1) // len(sems) if cnt > k else 0
                if n > 0:
                    dlast.append((sems[k], 16 * n))
        for e in ENG:
            o = Op()
            o.eng = e
            o.fn = None
            o.waits = []
            o.idx = len(self.q[e])
            o.inc = False
            o.dma = False
            o.semval = 0
            for f in ENG:
                d = last[f]
                if d is None or f == e:
                    continue
                if d.dma:
                    continue
                if d.idx <= self.waited[e].get(f, -1):
                    continue
                self.waited[e][f] = d.idx
                d.inc = True
                o.waits.append(('op', d))
            for sem, val in dlast:
                self._rawwait(o, sem, val)
            self.q[e].append(o)

    def emit(self):
        nc = self.nc
        for e in ENG:
            c = 0
            for o in self.q[e]:
                if o.inc:
                    c += 1
                    o.semval = c
        sem = self.sem

        def run(e, h):
            for o in self.q[e]:
                for wt in o.waits:
                    if wt[0] == 'op':
                        h.wait_ge(sem[wt[1].eng], wt[1].semval)
                    else:
                        h.wait_ge(wt[1], wt[2])
                if o.fn is None:
                    continue
                ins = o.fn(h)
                if o.dma:
                    ins.then_inc(o.dsem, 16)
                elif o.inc:
                    ins.then_inc(sem[e], 1)

        with nc.Block() as block:
            @block.tensor
            def _(h):
                run('pe', h)

            @block.scalar
            def _(h):
                run('act', h)

            @block.vector
            def _(h):
                run('dve', h)

            @block.gpsimd
            def _(h):
                run('pool', h)

            @block.sync
            def _(h):
                run('sp', h)

    def mark(self, name):
        import os
        if os.environ.get('KDBG'):
            print('MARK', name, self.uid, flush=True)

    def mm(self, out, lhsT, rhs, start, stop, r=(), w=(), skip=False):
        if skip:
            return self.op('pe', lambda h: h.matmul(out, lhsT, rhs, start=start, stop=stop, skip_group_check=True), r, w)
        return self.op('pe', lambda h: h.matmul(out, lhsT, rhs, start=start, stop=stop), r, w)

    def tr(self, out, in_, ident, r=(), w=()):
        return self.op('pe', lambda h: h.transpose(out, in_, ident), r, w)

    def act(self, out, in_, func, r=(), w=(), **kw):
        return self.op('act', lambda h: h.activation(out, in_, func, **kw), r, w)

    def tt(self, eng, out, in0, in1, op, r=(), w=()):
        return self.op(eng, lambda h: h.tensor_tensor(out, in0, in1, op), r, w)

    def ts(self, eng, out, in0, s1, s2, op0, op1=None, r=(), w=(), **kw):
        if op1 is None:
            return self.op(eng, lambda h: h.tensor_scalar(out, in0, s1, None, op0, **kw), r, w)
        return self.op(eng, lambda h: h.tensor_scalar(out, in0, s1, s2, op0, op1, **kw), r, w)

    def stt(self, eng, out, in0, sc, in1, op0, op1, r=(), w=()):
        return self.op(eng, lambda h: h.scalar_tensor_tensor(out, in0, sc, in1, op0, op1), r, w)

    def cp(self, eng, out, in_, r=(), w=()):
        if eng == 'act':
            return self.op('act', lambda h: h.copy(out, in_), r, w)
        return self.op(eng, lambda h: h.tensor_copy(out, in_), r, w)

    def affsel(self, out, in_, pattern, op, fill, base, cm, r=(), w=()):
        return self.op('pool', lambda h: h.affine_select(out, in_, pattern, op, float(fill), base=base,
                                                         channel_multiplier=cm), r, w)

    def memset(self, eng, ap, val, r=(), w=()):
        return self.op(eng, lambda h: h.memset(ap, val), r, w)

    def dma(self, eng, out, in_, r=(), w=(), **kw):
        return self.op(eng, lambda h: h.dma_start(out, in_, **kw), r, w, dma=True)


class K:
    def __init__(self, nc, g, es, S):
        self.nc = nc
        self.g = g
        self.es = es
        self.S = S
        self.NT = S // 128
        self.n = 0

    def sb(self, es, shape, dt, name=None):
        self.n += 1
        return es.enter_context(self.nc.sbuf_tensor('%s_%d' % (name or 't', self.n), list(shape), dt))

    def ps(self, es, shape, dt=F32, name=None):
        self.n += 1
        return es.enter_context(self.nc.psum_tensor('%s_%d' % (name or 'p', self.n), list(shape), dt))


def setup_consts(k):
    g, es = k.g, k.es
    k.ident_f = k.sb(es, [128, 128], F32, 'identf')
    k.ident_b = k.sb(es, [128, 128], BF16, 'identb')
    k.b_ident = Buf()
    ones = k.sb(es, [128, 128], F32, 'ones')
    bo = Buf()
    g.memset('pool', ones[:], 1.0, w=[bo])
    g.affsel(k.ident_f[:], ones[:], [[-1, 128]], ALU.is_equal, 0.0, 0, 1, r=[bo], w=[k.b_ident])
    g.cp('pool', k.ident_b[:], k.ident_f[:], r=[k.b_ident], w=[k.b_ident])
    k.ones_f = ones
    k.b_ones = bo
    k.stage = k.sb(es, [16, D], F32, 'stage')
    k.b_stage = Buf()


def load_cols(k, es, rows_ap, R, ps_bank, b_ps, name):
    g = k.g
    stage = k.stage[0:R, :]
    cols = k.sb(es, [128, 8, R], F32, name)
    bs, bc = k.b_stage, Buf()
    if isinstance(rows_ap, list):
        r0 = 0
        for ra in rows_ap:
            n = ra.shape[0]
            g.dma('sp', k.stage[r0:r0 + n, :], ra, w=[bs])
            r0 += n
        assert r0 == R
    else:
        g.dma('sp', stage, rows_ap, w=[bs])
    for c in range(8):
        g.tr(ps_bank[:, c * R:(c + 1) * R], stage[:, c * 128:(c + 1) * 128], k.ident_f[0:R, 0:R],
             r=[bs, k.b_ident], w=[b_ps])
    g.cp('dve', cols[:].rearrange('p c r -> p (c r)'), ps_bank[:, 0:8 * R], r=[b_ps], w=[bc])
    return cols, bc


def emit_front(k, xin_tile_ap, b_xd, xt, b_xt, gcol_ap, b_gcol, hT_ap, b_hT, scr, ps_tr, b_ps_tr, st, b_st,
               hb, b_hb):
    g = k.g
    g.dma('sp', xt[:], xin_tile_ap, r=[b_xd], w=[b_xt])
    g.act(scr[:], xt[:], AF.Square, r=[b_xt], w=[b_st, k.b_scr], accum_out=st[:, 0:1])
    g.ts('dve', st[:, 1:2], st[:, 0:1], 1.0 / D, EPS, ALU.mult, ALU.add, r=[b_st], w=[b_st])
    g.act(st[:, 3:4], st[:, 1:2], AF.Sqrt, r=[b_st], w=[b_st])
    g.op('dve', lambda h, o=st[:, 2:3], i=st[:, 3:4]: h.reciprocal(o, i), r=[b_st], w=[b_st])
    g.ts('dve', hb[:], xt[:], st[:, 2:3], None, ALU.mult, r=[b_xt, b_st], w=[b_hb])
    for c in range(8):
        g.tr(ps_tr[:, c * 128:(c + 1) * 128], hb[:, c * 128:(c + 1) * 128], k.ident_b[:],
             r=[b_hb, k.b_ident], w=[b_ps_tr])
    g.tt('dve', hT_ap, ps_tr[:].rearrange('p (c t) -> p c t', c=8),
         gcol_ap.unsqueeze(2).to_broadcast([128, 8, 128]), ALU.mult, r=[b_ps_tr, b_gcol], w=[b_hT])


def emit_post(k, ps_ap, b_ps, xres, b_xres, gpost, b_gpost, xout_tile_ap, b_xd, scr, st, b_st,
              tmp, b_tmp):
    g = k.g
    g.act(scr[:], ps_ap, AF.Square, r=[b_ps], w=[b_st, k.b_scr], accum_out=st[:, 0:1])
    g.ts('dve', st[:, 1:2], st[:, 0:1], 1.0 / D, EPS, ALU.mult, ALU.add, r=[b_st], w=[b_st])
    g.act(st[:, 3:4], st[:, 1:2], AF.Sqrt, r=[b_st], w=[b_st])
    g.op('dve', lambda h, o=st[:, 2:3], i=st[:, 3:4]: h.reciprocal(o, i), r=[b_st], w=[b_st])
    g.stt('dve', tmp[:], ps_ap, st[:, 2:3], gpost[:], ALU.mult, ALU.mult, r=[b_ps, b_st, b_gpost], w=[b_tmp])
    g.tt('pool', xres[:], tmp[:], xres[:], ALU.add, r=[b_tmp], w=[b_xres])
    g.dma('sp', xout_tile_ap, xres[:], r=[b_xres], w=[b_xd])


def phase_mlp(k, li, xd, b_xd, W):
    g, nc = k.g, k.nc
    NT = k.NT
    GT = 4 if NT % 4 == 0 else 1
    NG = NT // GT
    TG = GT * 128
    with contextlib.ExitStack() as es:
        wup = k.sb(es, [128, 8, DFF], BF16, 'wup')
        wdn = k.sb(es, [128, 32, D], BF16, 'wdn')
        b_wup = [Buf() for _ in range(8)]
        b_wdn = [Buf() for _ in range(8)]
        wup_d = W['mlp_w_up'][li].rearrange('(c p) f -> p c f', p=128)
        wdn_d = W['mlp_w_down'][li].rearrange('(c p) d -> p c d', p=128)
        for j in range(8):
            g.dma('pool', wup[:, :, j * 512:(j + 1) * 512], wup_d[:, :, j * 512:(j + 1) * 512], w=[b_wup[j]])
        for j in range(8):
            g.dma('pool', wdn[:, j * 4:(j + 1) * 4, :], wdn_d[:, j * 4:(j + 1) * 4, :], w=[b_wdn[j]])
        ps_tr = k.ps(es, [128, 1024], BF16, 'pstr')
        b_ps_tr = Buf(True)
        ps_up = [k.ps(es, [128, 512], F32, 'psup') for _ in range(3)]
        b_ps_up = [Buf(True) for _ in range(3)]
        ps_dn = [k.ps(es, [128, 1024], F32, 'psdn') for _ in range(2)]
        b_ps_dn = [Buf(True) for _ in range(2)]
        gcol, b_gcol = load_cols(k, es, W['norm_mlp_pre'][li:li + 1, :], 1, ps_up[0], b_ps_up[0], 'gcol')
        gpost = k.sb(es, [128, D], F32, 'gpost')
        b_gpost = Buf()
        g.dma('sp', gpost[:], W['norm_mlp_post'][li:li + 1, :].partition_broadcast(128), w=[b_gpost])
        NX = 3
        xt = [k.sb(es, [128, D], F32, 'xt') for _ in range(NX)]
        b_xt = [Buf() for _ in range(NX)]
        hb = [k.sb(es, [128, D], BF16, 'hb') for _ in range(2)]
        b_hb = [Buf() for _ in range(2)]
        scr = k.sb(es, [128, D], BF16, 'scr')
        k.b_scr = Buf()
        st = [k.sb(es, [128, 4], F32, 'st') for _ in range(4)]
        b_st = [Buf() for _ in range(4)]
        hT = k.sb(es, [128, 8, TG], BF16, 'hT')
        b_hT = [Buf() for _ in range(GT)]
        aT = k.sb(es, [128, 32, TG], BF16, 'aT')
        b_aT = [Buf() for _ in range(32)]
        rr = [k.sb(es, [128, TG], F32, 'rr') for _ in range(2)]
        b_rr = [Buf() for _ in range(2)]
        tmp = [k.sb(es, [128, D], F32, 'tmp') for _ in range(2)]
        b_tmp = [Buf() for _ in range(2)]
        cnt = 0

        def fronts(gi):
            nonlocal cnt
            for tl in range(GT):
                t = gi * GT + tl
                i = cnt % NX
                emit_front(k, xd[t * 128:(t + 1) * 128, :], b_xd[t], xt[i], b_xt[i], gcol[:, :, 0], b_gcol,
                           hT[:, :, tl * 128:(tl + 1) * 128], b_hT[tl], scr, ps_tr, b_ps_tr, st[cnt % 2],
                           b_st[cnt % 2], hb[cnt % 2], b_hb[cnt % 2])
                cnt += 1

        fronts(0)
        for gi in range(NG):
            for f in range(32):
                pi = f % 3
                for c in range(8):
                    g.mm(ps_up[pi][:, 0:TG], wup[:, c, f * 128:(f + 1) * 128], hT[:, c, :], c == 0, c == 7,
                         r=[b_wup[f // 4]] + b_hT, w=[b_ps_up[pi]])
                ri = f % 2
                g.act(rr[ri][:], ps_up[pi][:, 0:TG], AF.Relu, r=[b_ps_up[pi]], w=[b_rr[ri]])
                g.tt('pool' if f % 2 else 'dve', aT[:, f, :], rr[ri][:], rr[ri][:], ALU.mult, r=[b_rr[ri]],
                     w=[b_aT[f]])
            if gi + 1 < NG:
                fronts(gi + 1)
            for tl in range(GT):
                t = gi * GT + tl
                pd = t % 2
                for hf in range(2):
                    for f in range(32):
                        g.mm(ps_dn[pd][:, hf * 512:(hf + 1) * 512], aT[:, f, tl * 128:(tl + 1) * 128],
                             wdn[:, f, hf * 512:(hf + 1) * 512], f == 0, f == 31,
                             r=[b_aT[f], b_wdn[f // 4]], w=[b_ps_dn[pd]])
                i = cnt % NX
                cnt += 1
                g.dma('sp', xt[i][:], xd[t * 128:(t + 1) * 128, :], r=[b_xd[t]], w=[b_xt[i]])
                emit_post(k, ps_dn[pd][:], b_ps_dn[pd], xt[i], b_xt[i], gpost, b_gpost,
                          xd[t * 128:(t + 1) * 128, :], b_xd[t], scr, st[2 + pd], b_st[2 + pd], tmp[pd], b_tmp[pd])
        g.barrier()


class PsPool:
    def __init__(self, k, es, n):
        self.t = [k.ps(es, [128, 512], F32, 'pp') for _ in range(n)]
        self.b = [Buf(True) for _ in range(n)]
        self.i = 0

    def next(self):
        j = self.i % len(self.t)
        self.i += 1
        return self.t[j], self.b[j]


def load_w(k, wt, w_dram_ap, ncols, piece, nchunk=None):
    g = k.g
    src = w_dram_ap.rearrange('(c p) f -> p c f', p=128)
    bufs = []
    for j in range(0, ncols, piece):
        b = Buf()
        e = min(ncols, j + piece)
        g.dma('pool', wt[:, :, j:e], src[:, :, j:e], w=[b])
        bufs.append(b)
    return bufs


def mixer_common(k, es, li, W, nxt=2, npool=6):
    g = k.g
    c = {}
    c['pp'] = PsPool(k, es, npool)
    c['ps_out'] = k.ps(es, [128, 1024], F32, 'psout')
    c['b_ps_out'] = Buf(True)
    pt, pb = c['pp'].next()
    c['gcol'], c['b_gcol'] = load_cols(k, es, W['norm_mix_pre'][li:li + 1, :], 1, pt, pb, 'gcol')
    c['gpost'] = k.sb(es, [128, D], F32, 'gpost')
    c['b_gpost'] = Buf()
    g.dma('sp', c['gpost'][:], W['norm_mix_post'][li:li + 1, :].partition_broadcast(128), w=[c['b_gpost']])
    c['xt'] = [k.sb(es, [128, D], F32, 'xt') for _ in range(nxt)]
    c['b_xt'] = [Buf() for _ in range(nxt)]
    c['hb'] = k.sb(es, [128, D], BF16, 'hb')
    c['b_hb'] = Buf()
    c['scr'] = c['hb']
    k.b_scr = c['b_hb']
    c['st'] = [k.sb(es, [128, 4], F32, 'st') for _ in range(4)]
    c['b_st'] = [Buf() for _ in range(4)]
    c['tmp'] = k.sb(es, [128, D], F32, 'tmp')
    c['b_tmp'] = Buf()
    return c


def mixer_front(k, c, t, xd, b_xd, hT_ap, b_hT):
    pt, pb = c['pp'].next()
    i = t % 2
    ix = t % len(c['xt'])
    src = k.xsrc if getattr(k, 'xsrc', None) is not None else xd
    emit_front(k, src[t * 128:(t + 1) * 128, :], b_xd[t], c['xt'][ix], c['b_xt'][ix], c['gcol'][:, :, 0], c['b_gcol'],
               hT_ap, b_hT, c['scr'], pt[:].bitcast(BF16), pb, c['st'][i], c['b_st'][i], c['hb'], c['b_hb'])


def mixer_back(k, c, t, xd, b_xd):
    i = t % 2
    ix = t % len(c['xt'])
    emit_post(k, c['ps_out'][:], c['b_ps_out'], c['xt'][ix], c['b_xt'][ix], c['gpost'], c['b_gpost'],
              xd[t * 128:(t + 1) * 128, :], b_xd[t], c['scr'], c['st'][2 + i], c['b_st'][2 + i], c['tmp'], c['b_tmp'])


def head_norm_stats(k, src_ap, mv_ap, b_src, b_mv, scratch6, b_s6):
    g = k.g
    g.op('dve', lambda h: h.bn_stats(scratch6, src_ap), r=[b_src], w=[b_s6])
    g.op('dve', lambda h: h.bn_aggr(mv_ap, scratch6), r=[b_s6], w=[b_mv])


def phase_ret(k, li, xd, b_xd, W):
    import math
    g, nc = k.g, k.nc
    NT = k.NT
    j = li // 4
    H, DK, DV = 4, 256, 512
    lg = [math.log1p(-2.0 ** (-5.0 - h)) for h in range(H)]
    with contextlib.ExitStack() as es:
        win = k.sb(es, [128, 8, 6144], BF16, 'win')
        wout = k.sb(es, [128, 16, D], BF16, 'wout')
        b_win = load_w(k, win, W['ret_w_in'][j], 6144, 512)
        b_wout = load_w(k, wout, W['ret_w_out'][j], D, 512)
        c = mixer_common(k, es, li, W)
        pp = c['pp']
        pt, pb = pp.next()
        gncol, b_gncol = load_cols(k, es, W['ret_gn'][j:j + 1, :].rearrange('o (r f) -> (o r) f', r=2), 2, pt, pb,
                                   'gncol')
        decq = k.sb(es, [128, 8, 128], BF16, 'decq')
        dintra = k.sb(es, [128, H, 128], F32, 'dintra')
        dk = k.sb(es, [128, H], F32, 'dk')
        iot = k.sb(es, [128, 128], F32, 'iot')
        iot2 = k.sb(es, [128, 128], F32, 'iot2')
        iot3 = k.sb(es, [128, 1], F32, 'iot3')
        b_c = Buf()
        g.op('pool', lambda h: h.iota(iot[:], [[1, 128]], base=1, channel_multiplier=0,
                                      allow_small_or_imprecise_dtypes=True), w=[b_c])
        g.op('pool', lambda h: h.iota(iot2[:], [[1, 128]], base=0, channel_multiplier=-1,
                                      allow_small_or_imprecise_dtypes=True), w=[b_c])
        g.op('pool', lambda h: h.iota(iot3[:], [[0, 1]], base=127, channel_multiplier=-1,
                                      allow_small_or_imprecise_dtypes=True), w=[b_c])
        g.ts('pool', iot2[:], iot2[:], 0.0, None, ALU.max, r=[b_c], w=[b_c])
        for h in range(H):
            g.act(decq[:, 2 * h, :], iot[:], AF.Exp, r=[b_c], w=[b_c], scale=lg[h])
            g.act(decq[:, 2 * h + 1, :], iot[:], AF.Exp, r=[b_c], w=[b_c], scale=lg[h])
            g.act(dintra[:, h, :], iot2[:], AF.Exp, r=[b_c], w=[b_c], scale=lg[h])
            g.act(dk[:, h:h + 1], iot3[:], AF.Exp, r=[b_c], w=[b_c], scale=lg[h])
            g.affsel(dintra[:, h, :], dintra[:, h, :], [[1, 128]], ALU.is_ge, 0.0, 0, -1, r=[b_c], w=[b_c])
        g.ts('dve', dk[:], dk[:], 1.0 / 16.0, None, ALU.mult, r=[b_c], w=[b_c])
        hT = [k.sb(es, [128, 8, 128], BF16, 'hT')] * 2
        b_hT = [Buf()] * 2
        qT = k.sb(es, [128, 8, 128], BF16, 'qT')
        qdT = k.sb(es, [128, 8, 128], BF16, 'qdT')
        kT = k.sb(es, [128, 8, 128], BF16, 'kT')
        b_qT, b_qdT, b_kT = Buf(), Buf(), Buf()
        kdec = k.sb(es, [128, 1024], BF16, 'kdec')
        b_kdec = Buf()
        v = k.sb(es, [128, 2048], BF16, 'v')
        b_v = Buf()
        sg = k.sb(es, [128, 2048], BF16, 'sg')
        b_sg = Buf()
        stf = k.sb(es, [128, 8, 512], F32, 'stf')
        stb = k.sb(es, [128, 8, 512], BF16, 'stb')
        b_stf = [Buf() for _ in range(8)]
        b_stb = [Buf() for _ in range(8)]
        for i in range(8):
            g.memset('pool', stf[:, i, :], 0.0, w=[b_stf[i]])
            g.memset('pool', stb[:, i, :], 0.0, w=[b_stb[i]])
        sT = k.sb(es, [128, H, 128], BF16, 'sT')
        b_sT = Buf()
        onorm = k.sb(es, [128, 2048], BF16, 'onorm')
        b_on = Buf()
        oT = k.sb(es, [128, 16, 128], BF16, 'oT')
        b_oT = Buf()
        s6 = k.sb(es, [128, 6], F32, 's6')
        b_s6 = Buf()
        mv = k.sb(es, [128, H, 2], F32, 'mv')
        b_mv = Buf()
        hs = k.sb(es, [128, 3, H], F32, 'hs')
        b_hs = Buf()
        ontmp = c['tmp'][:, 0:256].bitcast(BF16)
        b_ontmp = c['b_tmp']

        g.mark('ret setup done')
        mixer_front(k, c, 0, xd, b_xd, hT[0][:], b_hT[0])
        for t in range(NT):
            hTt, bh = hT[t % 2], b_hT[t % 2]
            g.mark('ret tile %d' % t)
            for which in range(2):
                for half in range(2):
                    pt, pb = pp.next()
                    for cc in range(4):
                        col = which * 1024 + (half * 4 + cc) * 128
                        for ch in range(8):
                            g.mm(pt[:, cc * 128:(cc + 1) * 128], win[:, ch, col:col + 128], hTt[:, ch, :], ch == 0,
                                 ch == 7, r=[b_win[col // 512], bh], w=[pb])
                    dst = slice(half * 4, half * 4 + 4)
                    if which == 0:
                        g.cp('act', qT[:, dst, :].rearrange('p a b -> p (a b)'), pt[:], r=[pb], w=[b_qT])
                        g.tt('dve', qdT[:, dst, :].rearrange('p a b -> p (a b)'),
                             qT[:, dst, :].rearrange('p a b -> p (a b)'),
                             decq[:, dst, :].rearrange('p a b -> p (a b)'), ALU.mult,
                             r=[b_qT, b_c], w=[b_qdT])
                    else:
                        g.act(kT[:, dst, :].rearrange('p a b -> p (a b)'), pt[:], AF.Identity, r=[pb], w=[b_kT],
                              scale=1.0 / 16.0)
            g.mark('ret tokmajor')
            for blk in range(10):
                col = 1024 + blk * 512
                pt, pb = pp.next()
                for ch in range(8):
                    g.mm(pt[:], hTt[:, ch, :], win[:, ch, col:col + 512], ch == 0, ch == 7,
                         r=[b_win[col // 512], bh], w=[pb])
                if blk < 2:
                    for hh in range(2):
                        h = blk * 2 + hh
                        g.ts('dve', kdec[:, h * 256:(h + 1) * 256], pt[:, hh * 256:(hh + 1) * 256], dk[:, h:h + 1],
                             None, ALU.mult, r=[pb, b_c], w=[b_kdec])
                elif blk < 6:
                    o0 = (blk - 2) * 512
                    if blk % 2:
                        g.cp('act', v[:, o0:o0 + 512], pt[:], r=[pb], w=[b_v])
                    else:
                        g.cp('dve', v[:, o0:o0 + 512], pt[:], r=[pb], w=[b_v])
                else:
                    o0 = (blk - 6) * 512
                    g.act(sg[:, o0:o0 + 512], pt[:], AF.Silu, r=[pb], w=[b_sg])
            if t + 1 < NT:
                mixer_front(k, c, t + 1, xd, b_xd, hT[(t + 1) % 2][:], b_hT[(t + 1) % 2])
            g.mark('ret scores')
            pt, pb = pp.next()
            for h in range(H):
                for dc in range(2):
                    g.mm(pt[:, h * 128:(h + 1) * 128], kT[:, 2 * h + dc, :], qT[:, 2 * h + dc, :], dc == 0, dc == 1,
                         r=[b_kT, b_qT], w=[pb])
            g.tt('dve', sT[:].rearrange('p h t -> p (h t)'), pt[:], dintra[:].rearrange('p h t -> p (h t)'), ALU.mult,
                 r=[pb, b_c], w=[b_sT])
            g.mark('ret heads')
            o_ps = []
            for h in range(H):
                pt, pb = pp.next()
                g.mm(pt[:], sT[:, h, :], v[:, h * 512:(h + 1) * 512], True, False, r=[b_sT, b_v], w=[pb])
                for dc in range(2):
                    g.mm(pt[:], qdT[:, 2 * h + dc, :], stb[:, 2 * h + dc, :], False, dc == 1,
                         r=[b_qdT, b_stb[2 * h + dc]], w=[pb])
                g.op('dve', lambda hh, pt=pt: hh.bn_stats(s6[:], pt[:]), r=[pb], w=[b_s6])
                g.op('dve', lambda hh, h=h: hh.bn_aggr(mv[:, h, :], s6[:]), r=[b_s6], w=[b_mv])
                g.ts('dve', hs[:, 0, h:h + 1], mv[:, h, 1:2], EPS, None, ALU.add, r=[b_mv], w=[b_hs])
                g.act(hs[:, 1, h:h + 1], hs[:, 0, h:h + 1], AF.Sqrt, r=[b_hs], w=[b_hs])
                g.op('dve', lambda hh, h=h: hh.reciprocal(hs[:, 0, h:h + 1], hs[:, 1, h:h + 1]), r=[b_hs], w=[b_hs])
                g.stt('dve', hs[:, 2, h:h + 1], mv[:, h, 0:1], -1.0, hs[:, 0, h:h + 1], ALU.mult, ALU.mult,
                      r=[b_mv, b_hs], w=[b_hs])
                g.act(ontmp, pt[:], AF.Identity, r=[pb, b_hs], w=[b_ontmp], scale=hs[:, 0, h:h + 1],
                      bias=hs[:, 2, h:h + 1])
                g.tt('pool', onorm[:, h * 512:(h + 1) * 512], ontmp, sg[:, h * 512:(h + 1) * 512], ALU.mult,
                     r=[b_ontmp, b_sg], w=[b_on])
                for dc in range(2):
                    i = 2 * h + dc
                    pt2, pb2 = pp.next()
                    g.mm(pt2[:], kdec[:, h * 256 + dc * 128:h * 256 + (dc + 1) * 128], v[:, h * 512:(h + 1) * 512],
                         True, True, r=[b_kdec, b_v], w=[pb2])
                    g.stt('dve', stf[:, i, :], stf[:, i, :], math.exp(lg[h] * 128.0), pt2[:], ALU.mult, ALU.add,
                          r=[pb2], w=[b_stf[i]])
                    g.cp('act', stb[:, i, :], stf[:, i, :], r=[b_stf[i]], w=[b_stb[i]])
            g.mark('ret oT')
            for half in range(2):
                pt, pb = pp.next()
                ptb = pt[:].bitcast(BF16)
                for cc in range(8):
                    ec = half * 8 + cc
                    g.tr(ptb[:, cc * 128:(cc + 1) * 128], onorm[:, ec * 128:(ec + 1) * 128], k.ident_b[:],
                         r=[b_on, k.b_ident], w=[pb])
                g.tt('dve', oT[:, half * 8:(half + 1) * 8, :], ptb.rearrange('p (c t) -> p c t', c=8),
                     gncol[:, :, half].unsqueeze(2).to_broadcast([128, 8, 128]), ALU.mult, r=[pb, b_gncol], w=[b_oT])
            g.mark('ret outproj')
            for hf in range(2):
                for ec in range(16):
                    g.mm(c['ps_out'][:, hf * 512:(hf + 1) * 512], oT[:, ec, :], wout[:, ec, hf * 512:(hf + 1) * 512],
                         ec == 0, ec == 15, r=[b_oT, b_wout[hf]], w=[c['b_ps_out']])
            mixer_back(k, c, t, xd, b_xd)
        g.barrier()


def phase_lru(k, li, xd, b_xd, W):
    g, nc = k.g, k.nc
    NT = k.NT
    j = li // 4
    GT = 4 if NT % 4 == 0 else 1
    NG = NT // GT
    TG = GT * 128
    with contextlib.ExitStack() as es:
        win = k.sb(es, [128, 8, 2048], BF16, 'win')
        wout = k.sb(es, [128, 8, D], BF16, 'wout')
        b_win = load_w(k, win, W['lru_w_in'][j], 2048, 512)
        b_wout = load_w(k, wout, W['lru_w_out'][j], D, 512)
        wa = k.sb(es, [128, 8, 128], BF16, 'wa')
        wx = k.sb(es, [128, 8, 128], BF16, 'wx')
        b_wa, b_wx = Buf(), Buf()
        g.dma('pool', wa[:], W['lru_w_a'][j].rearrange('n c d -> c n d'), w=[b_wa])
        g.dma('pool', wx[:], W['lru_w_x'][j].rearrange('n c d -> c n d'), w=[b_wx])
        c = mixer_common(k, es, li, W, nxt=2 * GT)
        pp = c['pp']
        pt, pb = pp.next()
        cols, b_cols = load_cols(k, es, [W['lru_conv_w'][j], W['lru_conv_b'][j:j + 1, :], W['lru_b_a'][j:j + 1, :],
                                         W['lru_b_x'][j:j + 1, :], W['lru_lambda'][j:j + 1, :]], 8, pt, pb, 'lcols')
        c8 = k.sb(es, [128, 8], F32, 'c8')
        c8t = k.sb(es, [128, 8], F32, 'c8t')
        b_c8 = Buf()
        g.act(c8t[:], cols[:, :, 7], AF.Exp, r=[b_cols], w=[b_c8], scale=-1.0)
        g.ts('dve', c8t[:], c8t[:], 1.0, None, ALU.add, r=[b_c8], w=[b_c8])
        g.act(c8[:], c8t[:], AF.Ln, r=[b_c8], w=[b_c8])
        g.ts('dve', c8[:], c8[:], -8.0, None, ALU.mult, r=[b_c8], w=[b_c8])
        hT = k.sb(es, [128, 8, TG], BF16, 'hT')
        b_hT = [Buf() for _ in range(GT)]
        xbuf = k.sb(es, [128, 8, TG + 3], F32, 'xbuf')
        b_xbuf = [Buf() for _ in range(8)]
        hlast = k.sb(es, [128, 8], F32, 'hlast')
        b_hl = [Buf() for _ in range(8)]
        for n in range(8):
            g.memset('pool', xbuf[:, n, 0:3], 0.0, w=[b_xbuf[n]])
            g.memset('pool', hlast[:, n:n + 1], 0.0, w=[b_hl[n]])
        hyT = k.sb(es, [128, 8, TG], BF16, 'hyT')
        b_hy = [Buf() for _ in range(8)]

        def T(name, dt=F32):
            return [k.sb(es, [128, TG], dt, name) for _ in range(2)], [Buf() for _ in range(2)]
        xc, b_xc = T('xc')
        xcb, b_xcb = T('xcb', BF16)
        ysb, b_ysb = T('ysb')
        u, b_u = T('u')
        sgm, b_sgm = T('sgm')
        y, b_y = T('y')
        gr, b_gr = T('gr')
        gi, b_gi = T('gi')
        a, b_a = T('a')
        a2, b_a2 = T('a2')
        uu, b_uu = T('uu')
        hs, b_hs = T('hs')
        for gi_ in range(NG):
            for tl in range(GT):
                t = gi_ * GT + tl
                mixer_front(k, c, t, xd, b_xd, hT[:, :, tl * 128:(tl + 1) * 128], b_hT[tl])
            for n in range(8):
                s2 = n % 2
                pt, pb = pp.next()
                for ch in range(8):
                    g.mm(pt[:, 0:TG], win[:, ch, n * 128:(n + 1) * 128], hT[:, ch, :], ch == 0, ch == 7,
                         r=[b_win[(n * 128) // 512]] + b_hT, w=[pb])
                g.cp('act', xbuf[:, n, 3:3 + TG], pt[:, 0:TG], r=[pb], w=[b_xbuf[n]])
                g.ts('dve', xc[s2][:], xbuf[:, n, 0:TG], cols[:, n, 0:1], cols[:, n, 4:5], ALU.mult, ALU.add,
                     r=[b_xbuf[n], b_cols], w=[b_xc[s2]])
                for tap in range(1, 4):
                    g.stt('dve', xc[s2][:], xbuf[:, n, tap:tap + TG], cols[:, n, tap:tap + 1],
                          xc[s2][:], ALU.mult, ALU.add, r=[b_xbuf[n], b_cols], w=[b_xc[s2]])
                g.cp('pool', xbuf[:, n, 0:3], xbuf[:, n, TG:TG + 3], r=[], w=[b_xbuf[n]])
                g.cp('act', xcb[s2][:], xc[s2][:], r=[b_xc[s2]], w=[b_xcb[s2]])
                pt2, pb2 = pp.next()
                for ch in range(8):
                    g.mm(pt2[:, 0:TG], win[:, ch, 1024 + n * 128:1024 + (n + 1) * 128], hT[:, ch, :], ch == 0, ch == 7,
                         r=[b_win[(1024 + n * 128) // 512]] + b_hT, w=[pb2])
                g.cp('act', ysb[s2][:], pt2[:, 0:TG], r=[pb2], w=[b_ysb[s2]])
                g.act(u[s2][:], pt2[:, 0:TG], AF.Square, r=[pb2], w=[b_u[s2]])
                g.ts('dve', u[s2][:], u[s2][:], 0.044715, 1.0, ALU.mult, ALU.add, r=[], w=[b_u[s2]])
                g.tt('dve', u[s2][:], u[s2][:], ysb[s2][:], ALU.mult, r=[b_ysb[s2]], w=[b_u[s2]])
                g.act(sgm[s2][:], u[s2][:], AF.Sigmoid, r=[b_u[s2]], w=[b_sgm[s2]], scale=1.5957691216057308)
                g.tt('pool', y[s2][:], sgm[s2][:], ysb[s2][:], ALU.mult, r=[b_sgm[s2], b_ysb[s2]], w=[b_y[s2]])
                pt3, pb3 = pp.next()
                g.mm(pt3[:, 0:TG], wa[:, n, :], xcb[s2][:], True, True, r=[b_wa, b_xcb[s2]], w=[pb3])
                g.act(gr[s2][:], pt3[:, 0:TG], AF.Sigmoid, r=[pb3, b_cols], w=[b_gr[s2]], bias=cols[:, n, 5:6])
                pt4, pb4 = pp.next()
                g.mm(pt4[:, 0:TG], wx[:, n, :], xcb[s2][:], True, True, r=[b_wx, b_xcb[s2]], w=[pb4])
                g.act(gi[s2][:], pt4[:, 0:TG], AF.Sigmoid, r=[pb4, b_cols], w=[b_gi[s2]], bias=cols[:, n, 6:7])
                g.act(a[s2][:], gr[s2][:], AF.Exp, r=[b_gr[s2], b_c8], w=[b_a[s2]], scale=c8[:, n:n + 1])
                g.tt('pool', a2[s2][:], a[s2][:], a[s2][:], ALU.mult, r=[b_a[s2]], w=[b_a2[s2]])
                g.ts('dve', a2[s2][:], a2[s2][:], -1.0, 1.0, ALU.mult, ALU.add, r=[], w=[b_a2[s2]])
                g.act(a2[s2][:], a2[s2][:], AF.Sqrt, r=[], w=[b_a2[s2]])
                g.tt('dve', uu[s2][:], a2[s2][:], gi[s2][:], ALU.mult, r=[b_a2[s2], b_gi[s2]], w=[b_uu[s2]])
                g.tt('pool', uu[s2][:], uu[s2][:], xc[s2][:], ALU.mult, r=[b_xc[s2]], w=[b_uu[s2]])
                g.op('dve', lambda h, o=hs[s2][:], d0=a[s2][:], d1=uu[s2][:], ini=hlast[:, n:n + 1]:
                     h.tensor_tensor_scan(o, d0, d1, ini, ALU.mult, ALU.add),
                     r=[b_a[s2], b_uu[s2], b_hl[n]], w=[b_hs[s2]])
                g.cp('pool', hlast[:, n:n + 1], hs[s2][:, TG - 1:TG], r=[b_hs[s2]], w=[b_hl[n]])
                g.tt('dve' if n % 2 else 'pool', hyT[:, n, :], hs[s2][:], y[s2][:], ALU.mult,
                     r=[b_hs[s2], b_y[s2]], w=[b_hy[n]])
            for tl in range(GT):
                t = gi_ * GT + tl
                for hf in range(2):
                    for n in range(8):
                        g.mm(c['ps_out'][:, hf * 512:(hf + 1) * 512], hyT[:, n, tl * 128:(tl + 1) * 128],
                             wout[:, n, hf * 512:(hf + 1) * 512], n == 0, n == 7, r=[b_hy[n], b_wout[hf]],
                             w=[c['b_ps_out']])
                mixer_back(k, c, t, xd, b_xd)
        g.barrier()


def phase_gla(k, li, xd, b_xd, W):
    g, nc = k.g, k.nc
    NT = k.NT
    j = li // 4
    H, DK, DV = 4, 128, 256
    with contextlib.ExitStack() as es:
        win = k.sb(es, [128, 8, 3088], BF16, 'win')
        wout = k.sb(es, [128, 8, D], BF16, 'wout')
        b_win = load_w(k, win, W['gla_w_in'][j], 3088, 512)
        b_wout = load_w(k, wout, W['gla_w_out'][j], D, 512)
        wup = k.sb(es, [16, 512], F32, 'wup')
        bgate = k.sb(es, [1, 512], F32, 'bgate')
        b_wup = Buf()
        g.dma('sp', wup[:], W['gla_w_gate_up'][j], w=[b_wup])
        g.dma('sp', bgate[:], W['gla_b_gate'][j:j + 1, :], w=[b_wup])
        c = mixer_common(k, es, li, W)
        pp = c['pp']
        pt, pb = pp.next()
        gncol, b_gncol = load_cols(k, es, W['gla_gn'][j:j + 1, :], 1, pt, pb, 'gncol')
        UT = k.sb(es, [128, 128], F32, 'UT')
        VT = k.sb(es, [128, 128], F32, 'VT')
        mk = k.sb(es, [128, H, 128], F32, 'mk')
        b_c = Buf()
        g.memset('pool', UT[:], -1.0 / 16.0, w=[b_c])
        g.memset('pool', VT[:], -1.0 / 16.0, w=[b_c])
        g.memset('pool', mk[:], 1.0, w=[b_c])
        g.affsel(UT[:], UT[:], [[1, 128]], ALU.is_ge, 0.0, 0, -1, r=[b_c], w=[b_c])
        g.affsel(VT[:], VT[:], [[-1, 128]], ALU.is_gt, 0.0, 0, 1, r=[b_c], w=[b_c])
        for h in range(H):
            g.affsel(mk[:, h, :], mk[:, h, :], [[1, 128]], ALU.is_ge, 0.0, 0, -1, r=[b_c], w=[b_c])
        hT = k.sb(es, [128, 8, 128], BF16, 'hT')
        b_hT = Buf()
        glT = k.sb(es, [16, 128], F32, 'glT')
        b_glT = Buf()
        ez = k.sb(es, [128, 512], F32, 'ez')
        lsp = k.sb(es, [128, 512], F32, 'lsp')
        b_ez, b_lsp = Buf(), Buf()
        E1 = k.sb(es, [128, 512], F32, 'E1')
        E2 = k.sb(es, [128, 512], F32, 'E2')
        E3 = k.sb(es, [128, 512], F32, 'E3')
        b_E1, b_E2, b_E3 = Buf(), Buf(), Buf()
        qtT = k.sb(es, [128, H, 128], BF16, 'qtT')
        ktT = k.sb(es, [128, H, 128], BF16, 'ktT')
        khat = k.sb(es, [128, 512], BF16, 'khat')
        b_qtT, b_ktT, b_khat = Buf(), Buf(), Buf()
        v = k.sb(es, [128, 1024], BF16, 'v')
        sr = k.sb(es, [128, 1024], BF16, 'sr')
        b_v, b_sr = Buf(), Buf()
        AT = k.sb(es, [128, H, 128], BF16, 'AT')
        b_AT = Buf()
        stf = k.sb(es, [128, H, DV], F32, 'stf')
        stb = k.sb(es, [128, H, DV], BF16, 'stb')
        b_stf = [Buf() for _ in range(H)]
        b_stb = [Buf() for _ in range(H)]
        for h in range(H):
            g.memset('pool', stf[:, h, :], 0.0, w=[b_stf[h]])
            g.memset('pool', stb[:, h, :], 0.0, w=[b_stb[h]])
        ss = k.sb(es, [128, 3, H], F32, 'ss')
        b_ss = Buf()
        ontmp = c['tmp'][:, 0:128].bitcast(BF16)
        b_ontmp = c['b_tmp']
        onorm = k.sb(es, [128, 1024], BF16, 'onorm')
        b_on = Buf()
        oT = k.sb(es, [128, 8, 128], BF16, 'oT')
        b_oT = Buf()
        sq_scr = k.sb(es, [128, 256], BF16, 'sqscr')

        mixer_front(k, c, 0, xd, b_xd, hT[:], b_hT)
        for t in range(NT):
            pt_gl, pb_gl = pp.next()
            for ch in range(8):
                g.mm(pt_gl[0:16, 0:128], win[:, ch, 3072:3088], hT[:, ch, :], ch == 0, ch == 7, r=[b_win[6], b_hT],
                     w=[pb_gl])
            g.cp('act', glT[:], pt_gl[0:16, 0:128], r=[pb_gl], w=[b_glT])
            pt_z, pb_z = pp.next()
            g.mm(pt_z[:], glT[:], wup[:], True, False, r=[b_glT, b_wup], w=[pb_z])
            g.mm(pt_z[:], k.ones_f[0:1, :], bgate[:], False, True, r=[k.b_ones, b_wup], w=[pb_z])
            g.act(ez[:], pt_z[:], AF.Exp, r=[pb_z], w=[b_ez], scale=-1.0)
            g.act(lsp[:], ez[:], AF.Ln, r=[b_ez], w=[b_lsp], bias=1.0)
            pt_c, pb_c = pp.next()
            g.mm(pt_c[:], VT[:], lsp[:], True, True, r=[b_c, b_lsp], w=[pb_c])
            g.act(E3[:], pt_c[:], AF.Exp, r=[pb_c], w=[b_E3])
            pt_b, pb_b = pp.next()
            for h in range(H):
                g.mm(pt_b[:, h * 128:(h + 1) * 128], lsp[:, h * 128:(h + 1) * 128], UT[:], True, True,
                     r=[b_c, b_lsp], w=[pb_b])
            g.act(E1[:], pt_b[:], AF.Exp, r=[pb_b], w=[b_E1])
            g.act(E2[:], pt_b[:], AF.Exp, r=[pb_b], w=[b_E2], scale=-1.0)
            for which in range(2):
                pt, pb = pp.next()
                for cc in range(4):
                    col = which * 512 + cc * 128
                    for ch in range(8):
                        g.mm(pt[:, cc * 128:(cc + 1) * 128], win[:, ch, col:col + 128], hT[:, ch, :], ch == 0, ch == 7,
                             r=[b_win[col // 512], b_hT], w=[pb])
                if which == 0:
                    g.stt('dve', qtT[:].rearrange('p h t -> p (h t)'), pt[:], DK ** -0.5, E1[:], ALU.mult, ALU.mult,
                          r=[pb, b_E1], w=[b_qtT])
                else:
                    g.tt('dve', ktT[:].rearrange('p h t -> p (h t)'), pt[:], E2[:], ALU.mult, r=[pb, b_E2],
                         w=[b_ktT])
            pt, pb = pp.next()
            for ch in range(8):
                g.mm(pt[:], hT[:, ch, :], win[:, ch, 512:1024], ch == 0, ch == 7, r=[b_win[1], b_hT], w=[pb])
            g.tt('dve', khat[:], pt[:], E3[:], ALU.mult, r=[pb, b_E3], w=[b_khat])
            for blk in range(4):
                col = 1024 + blk * 512
                pt, pb = pp.next()
                for ch in range(8):
                    g.mm(pt[:], hT[:, ch, :], win[:, ch, col:col + 512], ch == 0, ch == 7,
                         r=[b_win[col // 512], b_hT], w=[pb])
                if blk < 2:
                    g.cp('act' if blk else 'dve', v[:, blk * 512:(blk + 1) * 512], pt[:], r=[pb], w=[b_v])
                else:
                    g.act(sr[:, (blk - 2) * 512:(blk - 1) * 512], pt[:], AF.Silu, r=[pb], w=[b_sr])
            if t + 1 < NT:
                mixer_front(k, c, t + 1, xd, b_xd, hT[:], b_hT)
            pt, pb = pp.next()
            for h in range(H):
                g.mm(pt[:, h * 128:(h + 1) * 128], ktT[:, h, :], qtT[:, h, :], True, True, r=[b_ktT, b_qtT], w=[pb])
            g.tt('dve', AT[:].rearrange('p h t -> p (h t)'), pt[:], mk[:].rearrange('p h t -> p (h t)'), ALU.mult,
                 r=[pb, b_c], w=[b_AT])
            o_banks = [pp.next(), pp.next()]
            for h in range(H):
                pt, pb = o_banks[h // 2]
                osl = pt[:, (h % 2) * 256:(h % 2 + 1) * 256]
                g.mm(osl, AT[:, h, :], v[:, h * 256:(h + 1) * 256], True, False, r=[b_AT, b_v], w=[pb])
                g.mm(osl, qtT[:, h, :], stb[:, h, :], False, True, r=[b_qtT, b_stb[h]], w=[pb])
                g.act(sq_scr[:], osl, AF.Square, r=[pb], w=[b_ss], accum_out=ss[:, 0, h:h + 1])
            g.ts('dve', ss[:, 1, :], ss[:, 0, :], 1.0 / DV, EPS, ALU.mult, ALU.add, r=[b_ss], w=[b_ss])
            g.act(ss[:, 2, :], ss[:, 1, :], AF.Sqrt, r=[b_ss], w=[b_ss])
            g.op('dve', lambda hh: hh.reciprocal(ss[:, 1, :], ss[:, 2, :]), r=[b_ss], w=[b_ss])
            for h in range(H):
                pt, pb = o_banks[h // 2]
                osl = pt[:, (h % 2) * 256:(h % 2 + 1) * 256]
                g.act(ontmp, osl, AF.Identity, r=[pb, b_ss], w=[b_ontmp], scale=ss[:, 1, h:h + 1])
                g.tt('pool', onorm[:, h * 256:(h + 1) * 256], ontmp, sr[:, h * 256:(h + 1) * 256], ALU.mult,
                     r=[b_ontmp, b_sr], w=[b_on])
                pt2, pb2 = pp.next()
                g.mm(pt2[:, 0:256], khat[:, h * 128:(h + 1) * 128], v[:, h * 256:(h + 1) * 256], True, True,
                     r=[b_khat, b_v], w=[pb2])
                g.stt('dve', stf[:, h, :], stf[:, h, :], E1[:, h * 128 + 127:h * 128 + 128], pt2[:, 0:256], ALU.mult,
                      ALU.add, r=[pb2, b_E1], w=[b_stf[h]])
                g.cp('act', stb[:, h, :], stf[:, h, :], r=[b_stf[h]], w=[b_stb[h]])
            pt, pb = pp.next()
            ptb = pt[:].bitcast(BF16)
            for cc in range(8):
                g.tr(ptb[:, cc * 128:(cc + 1) * 128], onorm[:, cc * 128:(cc + 1) * 128], k.ident_b[:],
                     r=[b_on, k.b_ident], w=[pb])
            g.tt('dve', oT[:], ptb.rearrange('p (c t) -> p c t', c=8),
                 gncol[:, :, 0].unsqueeze(2).to_broadcast([128, 8, 128]), ALU.mult, r=[pb, b_gncol], w=[b_oT])
            for hf in range(2):
                for ec in range(8):
                    g.mm(c['ps_out'][:, hf * 512:(hf + 1) * 512], oT[:, ec, :], wout[:, ec, hf * 512:(hf + 1) * 512],
                         ec == 0, ec == 7, r=[b_oT, b_wout[hf]], w=[c['b_ps_out']])
            mixer_back(k, c, t, xd, b_xd)
        g.barrier()


def bf16_round(x):
    u = np.ascontiguousarray(x, dtype=np.float32).view(np.uint32).astype(np.uint64)
    r = ((u + 0x7FFF + ((u >> 16) & 1)) & 0xFFFF0000).astype(np.uint32)
    return r.view(np.float32)


def hilo(x):
    hi = bf16_round(x)
    lo = bf16_round(np.asarray(x, dtype=np.float32) - hi)
    return hi, lo


def nsa_tables(NT):
    H = 16
    slopes = np.exp2(-8.0 * (np.arange(H, dtype=np.float64) + 1.0) / H).astype(np.float32)
    ip = np.arange(128, dtype=np.float32)
    qA = np.zeros((6, H, 128), np.float32)
    for h in range(H):
        a, b = hilo(np.full(128, -8.0 * 128.0 * slopes[h], np.float32))
        qA[0, h], qA[1, h] = a, b
        a, b = hilo(-8.0 * slopes[h] * ip)
        qA[2, h], qA[3, h] = a, b
        a, b = hilo(np.full(128, 8.0 * slopes[h], np.float32))
        qA[4, h], qA[5, h] = a, b
    kA = np.zeros((6, 33, 128), np.float32)
    for d in range(33):
        kA[0, d] = kA[1, d] = d
        kA[2, d] = kA[3, d] = 1.0
        kA[4, d] = kA[5, d] = ip
    s_ = np.arange(256)
    ce = 16 * s_ + 15
    b_, a_ = ce // 128, ce % 128
    kAc = np.zeros((6, NT, 256), np.float32)
    for T in range(NT):
        kAc[0, T] = kAc[1, T] = np.where(b_ > T, 64, T - b_)
        kAc[2, T] = kAc[3, T] = 1.0
        kAc[4, T] = kAc[5, T] = a_
    n_ = s_ - 1
    j_ = np.arange(64)
    ovl = ((16 * n_[:, None] < 64 * j_[None, :] + 64) & (16 * n_[:, None] + 32 > 64 * j_[None, :]) &
           (n_[:, None] >= 0)).astype(np.float32)
    return {'c_qA': qA, 'c_kA': kA, 'c_kAc': kAc, 'c_ovl': ovl}


def phase_nsa(k, li, xd, b_xd, W):
    g, nc = k.g, k.nc
    NT = k.NT
    S = k.S
    j = li // 4
    G, HPG, DK = 2, 8, 64
    with contextlib.ExitStack() as es:
        win = k.sb(es, [128, 8, 1840], BF16, 'win')
        wout = k.sb(es, [128, 8, D], BF16, 'wout')
        b_win = load_w(k, win, W['nsa_w_in'][j], 1840, 460)
        b_wout = load_w(k, wout, W['nsa_w_out'][j], D, 512)
        bw_all = b_win
        w1 = [k.sb(es, [64, 32, 128], BF16, 'w1') for _ in range(2)]
        w2 = [k.sb(es, [128, 64], BF16, 'w2') for _ in range(2)]
        pe = [k.sb(es, [32, 64], F32, 'pe') for _ in range(2)]
        b_w1 = Buf()
        for i, nm in enumerate(['k', 'v']):
            g.dma('pool', w1[i][:], W['nsa_w1_' + nm][j].rearrange('(l d) f -> d l f', d=64), w=[b_w1])
            g.dma('pool', w2[i][:], W['nsa_w2_' + nm][j], w=[b_w1])
            g.dma('sp', pe[i][:], W['nsa_pe_' + nm][j], w=[b_w1])
        qA = k.sb(es, [6, 16 * 128], BF16, 'qA')
        kA = k.sb(es, [6, 33 * 128], BF16, 'kA')
        kAc = k.sb(es, [6, NT * 256], BF16, 'kAc')
        ovl = k.sb(es, [128, 2, 64], BF16, 'ovl')
        b_tab = Buf()
        g.dma('pool', qA[:], W['c_qA'].rearrange('r h i -> r (h i)'), w=[b_tab])
        g.dma('pool', kA[:], W['c_kA'].rearrange('r d i -> r (d i)'), w=[b_tab])
        g.dma('pool', kAc[:], W['c_kAc'].rearrange('r t s -> r (t s)'), w=[b_tab])
        g.dma('pool', ovl[:], W['c_ovl'].rearrange('(n p) j -> p n j', p=128), w=[b_tab])
        c = mixer_common(k, es, li, W, npool=4)
        pp = c['pp']
        tri4 = k.sb(es, [128, 512], F32, 'tri4')
        triu4 = k.sb(es, [128, 512], F32, 'triu4')
        Ex = k.sb(es, [64, NT, 128], BF16, 'Ex')
        zrow = k.sb(es, [1, 512], BF16, 'zrow')
        b_c = Buf()
        g.memset('pool', tri4[:], 1.0, w=[b_c])
        g.memset('pool', triu4[:], 1.0, w=[b_c])
        g.memset('pool', Ex[:], 1.0, w=[b_c])
        g.memset('pool', zrow[:], 0.0, w=[b_c])
        g.affsel(tri4[:], tri4[:], [[0, 4], [1, 128]], ALU.is_ge, 0.0, 0, -1, r=[b_c], w=[b_c])
        g.affsel(triu4[:], triu4[:], [[0, 4], [-1, 128]], ALU.is_gt, 0.0, 0, 1, r=[b_c], w=[b_c])
        for hf in range(2):
            g.affsel(Ex[:, :, hf * 64:(hf + 1) * 64], Ex[:, :, hf * 64:(hf + 1) * 64],
                                                            [[-2, NT], [0, 64]], ALU.is_equal, 0.0, -hf, 1, r=[b_c], w=[b_c])
        Mw = k.sb(es, [128, 2304], BF16, 'Mw')
        Vw = k.sb(es, [128, 128], F32, 'Vw')
        Aw = k.sb(es, [128, 128], F32, 'Aw')
        g.memset('pool', Mw[:], 1.0, w=[b_c])
        g.memset('pool', Vw[:], 1.0, w=[b_c])
        g.memset('pool', Aw[:], 0.0, w=[b_c])
        g.affsel(Mw[:], Mw[:], [[1, 2304]], ALU.is_ge, 0.0, -15, -16, r=[b_c], w=[b_c])
        for hf in range(2):
            rows = slice(hf * 64, (hf + 1) * 64)
            g.affsel(Vw[rows, :], Vw[rows, :], [[-1, 128]], ALU.is_ge, 0.0, 64 + hf, 0, r=[b_c], w=[b_c])
            g.affsel(Aw[rows, :], Aw[rows, :], [[-1, 128]], ALU.is_ge, -1.0, 64 + hf, 0, r=[b_c], w=[b_c])
            g.affsel(Aw[rows, :], Aw[rows, :], [[1, 128]], ALU.not_equal, 1e6, -(64 + hf), 0, r=[b_c], w=[b_c])
            g.affsel(Aw[rows, :], Aw[rows, :], [[1, 128]], ALU.not_equal, 1e6, -(63 + hf), 0, r=[b_c], w=[b_c])
        kcT = [[k.sb(es, [64, 160], BF16, 'kcT') for _ in range(G)] for _ in range(2)]
        b_kc = Buf()
        for i in range(2):
            for gg in range(G):
                g.memset('pool', kcT[i][gg][:], 0.0, w=[b_kc])
        ksT = [k.sb(es, [64, S], BF16, 'ksT') for _ in range(G)]
        kwT = [k.sb(es, [64, 5 * 128], BF16, 'kwT') for _ in range(G)]
        b_kw = [Buf() for _ in range(5)]
        b_ks = [Buf() for _ in range(NT)]
        vs = k.sb(es, [128, NT, G, 65], BF16, 'vs')
        vw = k.sb(es, [128, 5, G, 65], BF16, 'vw')
        b_vs = [Buf() for _ in range(NT)]
        b_v1 = Buf()
        g.memset('pool', vs[:], 1.0, w=[b_v1])
        g.memset('pool', vw[:], 1.0, w=[b_v1])
        hidT = [[k.sb(es, [128, 256], BF16, 'hidT') for _ in range(G)] for _ in range(2)]
        b_hid = Buf()
        for i in range(2):
            for gg in range(G):
                g.memset('pool', hidT[i][gg][:], 0.0, w=[b_hid])
        kcmpT = [k.sb(es, [64, 256], BF16, 'kcmpT') for _ in range(G)]
        b_kcmp = Buf()
        for gg in range(G):
            g.memset('pool', kcmpT[gg][:], 0.0, w=[b_kcmp])
        vcmp = k.sb(es, [128, 2, G, 65], BF16, 'vcmp')
        b_vcmp = Buf()
        g.memset('pool', vcmp[:], 1.0, w=[b_vcmp])
        c1 = k.sb(es, [128, 2], F32, 'c1')
        peT = k.sb(es, [64, 2, 32], BF16, 'peT')
        b_c1 = Buf()
        for i in range(2):
            pt, pb = pp.next()
            g.tr(pt[0:64, 0:32], pe[i][:], k.ident_f[0:32, 0:32], r=[b_w1, k.b_ident], w=[pb])
            g.cp('dve', peT[:, i, :], pt[0:64, 0:32], r=[pb], w=[b_c1])
        for i in range(2):
            pt, pb = pp.next()
            for l in range(32):
                g.mm(pt[:, 0:1], w1[i][:, l, :], peT[:, i, l:l + 1], l == 0, l == 31, r=[b_w1, b_c1], w=[pb])
            g.cp('dve', c1[:, i:i + 1], pt[:, 0:1], r=[pb], w=[b_c1])
        hT = k.sb(es, [128, 8, 128], BF16, 'hT')
        b_hT = Buf()
        qT = [k.sb(es, [64, 8, 128], BF16, 'qT') for _ in range(G)]
        b_qT = Buf()
        gts = k.sb(es, [128, 48], F32, 'gts')
        b_gts = Buf()
        hsm = [k.sb(es, [128, 8], F32, 'hsm') for _ in range(5)]
        b_hsm = Buf()
        mask4 = [k.sb(es, [128, 512], BF16, 'mask4') for _ in range(2)]
        b_m4 = Buf()
        e16 = [k.sb(es, [128, 512], BF16, 'e16') for _ in range(2)]
        b_e16 = [Buf() for _ in range(2)]
        e32 = [k.sb(es, [128, 512], F32, 'e32') for _ in range(2)]
        b_e32 = [Buf() for _ in range(2)]
        pc = [k.sb(es, [128, 8, 128], BF16, 'pc') for _ in range(2)]
        b_pc = Buf()
        pT = [k.sb(es, [128, 512], BF16, 'pT') for _ in range(3)]
        b_pT = [Buf() for _ in range(3)]
        imp = k.sb(es, [128, 64], F32, 'imp')
        imp2 = k.sb(es, [128, 64], F32, 'imp2')
        m8 = k.sb(es, [128, 16], F32, 'm8')
        selm = k.sb(es, [128, 64], F32, 'selm')
        b_imp = Buf()
        selT4 = k.sb(es, [64, 4, 128], BF16, 'selT4')
        b_selT = Buf()
        rec = k.sb(es, [128, 3, 8], F32, 'rec')
        b_rec = Buf()
        og = k.sb(es, [128, 1024], F32, 'og')
        b_og = Buf()
        ogb = k.sb(es, [128, 1024], BF16, 'ogb')
        b_ogb = Buf()
        oT = k.sb(es, [128, 8, 128], BF16, 'oT')
        b_oT = Buf()
        acc = [k.ps(es, [128, 512], F32, 'acc') for _ in range(2)]
        b_acc = [Buf(True) for _ in range(2)]
        npt = [0]

        def zero_bank(t_, b_):
            g.mm(t_[:], zrow[0:1, 0:128], zrow[0:1, :], True, False, r=[b_c], w=[b_], skip=True)

        def gelu_small(src_ps, bias_ap, dst, n, rlist, wbuf):
            xs, us, ss_ = hsm[0], hsm[1], hsm[2]
            g.act(xs[:, 0:n], src_ps, AF.Identity, r=rlist + [b_c1], w=[b_hsm], bias=bias_ap)
            g.tt('dve', us[:, 0:n], xs[:, 0:n], xs[:, 0:n], ALU.mult, r=[b_hsm], w=[b_hsm])
            g.ts('dve', us[:, 0:n], us[:, 0:n], 0.044715, 1.0, ALU.mult, ALU.add, r=[b_hsm], w=[b_hsm])
            g.tt('dve', us[:, 0:n], us[:, 0:n], xs[:, 0:n], ALU.mult, r=[b_hsm], w=[b_hsm])
            g.act(ss_[:, 0:n], us[:, 0:n], AF.Sigmoid, r=[b_hsm], w=[b_hsm], scale=1.5957691216057308)
            g.tt('dve', dst, ss_[:, 0:n], xs[:, 0:n], ALU.mult, r=[b_hsm], w=[wbuf])

        def qk_exp(T, kt, gg, half, k_ap, b_kcache):
            pt, pb = pp.next()
            hs0 = gg * 8 + half * 4
            g.mm(pt[:], k_ap, qT[gg][:, half * 4:(half + 1) * 4, :].rearrange('p a b -> p (a b)'),
                 True, False, r=[b_kcache, b_qT], w=[pb])
            d = T - kt
            g.mm(pt[:], kA[:, d * 128:(d + 1) * 128], qA[:, hs0 * 128:(hs0 + 4) * 128], False, True, r=[b_tab], w=[pb])
            return pt, pb

        mixer_front(k, c, 0, xd, b_xd, hT[:], b_hT)
        for T in range(NT):
            g.mark('nsa tile %d' % T)
            for gg in range(G):
                for half in range(2):
                    pt, pb = pp.next()
                    for hh in range(4):
                        h = gg * 8 + half * 4 + hh
                        for ch in range(8):
                            g.mm(pt[0:64, hh * 128:(hh + 1) * 128], win[:, ch, h * 64:(h + 1) * 64], hT[:, ch, :], ch == 0,
                                 ch == 7, r=bw_all + [b_hT], w=[pb])
                    g.cp('act' if half else 'dve', qT[gg][:, half * 4:(half + 1) * 4, :].rearrange('p a b -> p (a b)'),
                         pt[0:64, :], r=[pb], w=[b_qT])
            for idx, (col0, dst, off) in enumerate([(1024, kcT[0], 16), (1152, kcT[1], 16), (1280, ksT, 0), (1536, kwT, 0)]):
                pt, pb = pp.next()
                for gg in range(G):
                    for ch in range(8):
                        g.mm(pt[0:64, gg * 128:(gg + 1) * 128], win[:, ch, col0 + gg * 64:col0 + (gg + 1) * 64], hT[:, ch, :],
                             ch == 0, ch == 7, r=bw_all + [b_hT], w=[pb])
                for gg in range(G):
                    if idx < 2:
                        wb, c0 = b_kc, 16
                    elif idx == 2:
                        wb, c0 = b_ks[T], T * 128
                    else:
                        wb, c0 = b_kw[T % 5], (T % 5) * 128
                    g.cp('act' if gg else 'dve', dst[gg][:, c0:c0 + 128],
                         pt[0:64, gg * 128:(gg + 1) * 128], r=[pb], w=[wb])
            pt, pb = pp.next()
            for ch in range(8):
                g.mm(pt[:, 0:128], hT[:, ch, :], win[:, ch, 1408:1536], ch == 0, ch == 7, r=bw_all + [b_hT], w=[pb])
            for ch in range(8):
                g.mm(pt[:, 128:256], hT[:, ch, :], win[:, ch, 1664:1792], ch == 0, ch == 7, r=bw_all + [b_hT], w=[pb])
            for ch in range(8):
                g.mm(pt[:, 256:304], hT[:, ch, :], win[:, ch, 1792:1840], ch == 0, ch == 7, r=bw_all + [b_hT], w=[pb])
            for gg in range(G):
                g.cp('dve', vs[:, T, gg, 0:64], pt[:, gg * 64:(gg + 1) * 64], r=[pb, b_v1], w=[b_vs[T]])
                g.cp('act', vw[:, T % 5, gg, 0:64], pt[:, 128 + gg * 64:128 + (gg + 1) * 64], r=[pb, b_v1], w=[b_kw[T % 5]])
            g.act(gts[:], pt[:, 256:304], AF.Sigmoid, r=[pb], w=[b_gts])
            if T + 1 < NT:
                mixer_front(k, c, T + 1, xd, b_xd, hT[:], b_hT)
            for i in range(2):
                for gg in range(G):
                    pt, pb = pp.next()
                    for l in range(32):
                        rhs = kcT[i][gg][:, l:l + 128].rearrange('p (m s) -> p m s', s=16)[:, :, 0]
                        g.mm(pt[:, 0:8], w1[i][:, l, :], rhs, l == 0, l == 31, r=[b_w1, b_kc], w=[pb])
                    gelu_small(pt[:, 0:8], c1[:, i:i + 1], hidT[i][gg][:, 8 * T:8 * T + 8], 8, [pb], b_hid)
            for i in range(2):
                for gg in range(G):
                    g.cp('dve', kcT[i][gg][:, 0:16], kcT[i][gg][:, 128:144], r=[], w=[b_kc])
            for gg in range(G):
                pt, pb = pp.next()
                g.mm(pt[0:64, 0:8], w2[0][:], hidT[0][gg][:, 8 * T:8 * T + 8], True, True, r=[b_w1, b_hid], w=[pb])
                g.cp('dve', kcmpT[gg][:, 8 * T:8 * T + 8], pt[0:64, 0:8], r=[pb], w=[b_kcmp])
            ntl = [0] if 8 * T + 7 < 128 else [0, 1]
            pt, pb = pp.next()
            for nt_ in ntl:
                for gg in range(G):
                    sl = pt[:, (nt_ * 2 + gg) * 64:(nt_ * 2 + gg + 1) * 64]
                    g.mm(sl, hidT[1][gg][:, nt_ * 128:(nt_ + 1) * 128], w2[1][:], True, True, r=[b_w1, b_hid], w=[pb])
            for nt_ in ntl:
                for gg in range(G):
                    sl = pt[:, (nt_ * 2 + gg) * 64:(nt_ * 2 + gg + 1) * 64]
                    g.cp('dve', vcmp[:, nt_, gg, 0:64], sl, r=[pb], w=[b_vcmp])
            for gg in range(G):
                g.mark('nsa T%d g%d' % (T, gg))
                for nt_ in ntl:
                    off = min(128 * T - 2048 * nt_, 2176)
                    for rr_ in range(4):
                        g.cp('pool' if rr_ % 2 else 'act', mask4[nt_][:, rr_ * 128:(rr_ + 1) * 128], Mw[:, off:off + 128],
                             r=[b_c], w=[b_m4])
                    if nt_ == 0:
                        g.memset('pool', mask4[0][0:1, :], 0.0, w=[b_m4])
                    for half in range(2):
                        pt, pb = pp.next()
                        hs0 = gg * 8 + half * 4
                        g.mm(pt[:], kcmpT[gg][:, nt_ * 128:(nt_ + 1) * 128],
                             qT[gg][:, half * 4:(half + 1) * 4, :].rearrange('p a b -> p (a b)'), True, False,
                             r=[b_kcmp, b_qT], w=[pb])
                        g.mm(pt[:], kAc[:, T * 256 + nt_ * 128:T * 256 + (nt_ + 1) * 128],
                             qA[:, hs0 * 128:(hs0 + 4) * 128], False, True, r=[b_tab], w=[pb])
                        ei = half
                        g.act(e16[ei][:], pt[:], AF.Exp, r=[pb], w=[b_e16[ei]], scale=0.125, bias=-30.0)
                        g.tt('dve', pc[nt_][:, half * 4:(half + 1) * 4, :].rearrange('p a b -> p (a b)'), e16[ei][:],
                             mask4[nt_][:], ALU.mult, r=[b_e16[ei], b_m4], w=[b_pc])
                pu, pbu = pp.next()
                for hf in range(2):
                    zero_bank(acc[hf], b_acc[hf])
                for hp in range(8):
                    for ii, nt_ in enumerate(ntl):
                        last = ii == len(ntl) - 1
                        g.mm(acc[hp // 4][:, (hp % 4) * 65:(hp % 4) * 65 + 65], pc[nt_][:, hp, :], vcmp[:, nt_, gg, :],
                             False, False, r=[b_pc, b_vcmp], w=[b_acc[hp // 4]], skip=True)
                    for ii, nt_ in enumerate(ntl):
                        g.mm(pu[:, hp * 64:(hp + 1) * 64], pc[nt_][:, hp, :], ovl[:, nt_, :], ii == 0,
                             ii == len(ntl) - 1, r=[b_pc, b_tab], w=[pbu])
                for hf in range(2):
                    den = acc[hf][:, 0:260].rearrange('p (h c) -> p h c', c=65)[:, :, 64]
                    g.ts('dve', rec[:, 0, hf * 4:(hf + 1) * 4], den, 1e-30, None, ALU.max, r=[b_acc[hf]], w=[b_rec])
                g.op('dve', lambda hh: hh.reciprocal(rec[:, 0, :], rec[:, 0, :]), r=[b_rec], w=[b_rec])
                for hp in range(8):
                    if hp == 0:
                        g.ts('dve', imp[:], pu[:, 0:64], rec[:, 0, 0:1], None, ALU.mult, r=[pbu, b_rec], w=[b_imp])
                    else:
                        g.stt('dve', imp[:], pu[:, hp * 64:(hp + 1) * 64], rec[:, 0, hp:hp + 1], imp[:], ALU.mult, ALU.add,
                              r=[pbu, b_rec], w=[b_imp])
                for hp in range(8):
                    h = gg * 8 + hp
                    g.tt('dve', hsm[3][:, hp:hp + 1], rec[:, 0, hp:hp + 1], gts[:, h:h + 1], ALU.mult,
                         r=[b_rec, b_gts], w=[b_hsm])
                    g.ts('dve', og[:, h * 64:(h + 1) * 64],
                         acc[hp // 4][:, (hp % 4) * 65:(hp % 4) * 65 + 64], hsm[3][:, hp:hp + 1], None, ALU.mult,
                         r=[b_acc[hp // 4], b_hsm], w=[b_og])
                g.tt('dve', imp[:], imp[:], Vw[:, 64 - 2 * T:128 - 2 * T], ALU.mult, r=[b_c], w=[b_imp])
                g.tt('dve', imp[:], imp[:], Aw[:, 64 - 2 * T:128 - 2 * T], ALU.add, r=[b_c], w=[b_imp])
                g.memset('pool', imp[:, 0:1], 1e6, w=[b_imp])
                g.op('dve', lambda hh: hh.max(m8[:, 0:8], imp[:]), r=[], w=[b_imp])
                g.op('dve', lambda hh: hh.match_replace(imp2[:], m8[:, 0:8], imp[:], -3.0e38), r=[], w=[b_imp])
                g.op('dve', lambda hh: hh.max(m8[:, 8:16], imp2[:]), r=[], w=[b_imp])
                g.ts('dve', selm[:], imp[:], m8[:, 15:16], None, ALU.is_ge, r=[], w=[b_imp])
                pt, pb = pp.next()
                g.tr(pt[0:64, 0:128], selm[:], k.ident_f[:], r=[b_imp, k.b_ident], w=[pb])
                for rr_ in range(4):
                    g.cp('dve' if rr_ % 2 else 'act', selT4[:, rr_, :], pt[0:64, 0:128], r=[pb], w=[b_selT])
                for br, kts in enumerate([list(range(0, T + 1)), list(range(max(0, T - 4), T + 1))]):
                    g.mark('nsa T%d g%d br%d' % (T, gg, br))
                    for hf in range(2):
                        zero_bank(acc[hf], b_acc[hf])
                    for ki, kt in enumerate(kts):
                        d = T - kt
                        lastk = ki == len(kts) - 1
                        if br == 0 and d >= 1:
                            pm, pbm = pp.next()
                            g.mm(pm[:], Ex[:, kt, :], selT4[:].rearrange('p a b -> p (a b)'), True, True,
                                 r=[b_c, b_selT], w=[pbm])
                        for half in range(2):
                            if br == 0:
                                k_ap, bk, v_ap, bv = ksT[gg][:, kt * 128:(kt + 1) * 128], b_ks[kt], vs[:, kt, gg, :], b_vs[kt]
                            else:
                                sl5 = kt % 5
                                k_ap, bk, v_ap, bv = kwT[gg][:, sl5 * 128:(sl5 + 1) * 128], b_kw[sl5], vw[:, sl5, gg, :], b_kw[sl5]
                            pt, pb = qk_exp(T, kt, gg, half, k_ap, bk)
                            pi = npt[0] % 3
                            npt[0] += 1
                            if br == 1 and 1 <= d <= 3:
                                g.act(pT[pi][:], pt[:], AF.Exp, r=[pb], w=[b_pT[pi]], scale=0.125, bias=-30.0)
                            else:
                                ei = half
                                g.act(e32[ei][:], pt[:], AF.Exp, r=[pb], w=[b_e32[ei]], scale=0.125, bias=-30.0)
                                if d == 0:
                                    g.tt('dve', pT[pi][:], e32[ei][:], tri4[:], ALU.mult, r=[b_e32[ei], b_c], w=[b_pT[pi]])
                                elif br == 1:
                                    g.tt('dve', pT[pi][:], e32[ei][:], triu4[:], ALU.mult, r=[b_e32[ei], b_c],
                                         w=[b_pT[pi]])
                                else:
                                    g.tt('dve', pT[pi][:], e32[ei][:], pm[:], ALU.mult, r=[b_e32[ei], pbm], w=[b_pT[pi]])
                            for hh in range(4):
                                g.mm(acc[half][:, hh * 65:hh * 65 + 65], pT[pi][:, hh * 128:(hh + 1) * 128],
                                     v_ap, False, False, r=[b_pT[pi], bv], w=[b_acc[half]], skip=True)
                    bi = br + 1
                    for hf in range(2):
                        den = acc[hf][:, 0:260].rearrange('p (h c) -> p h c', c=65)[:, :, 64]
                        g.ts('dve', rec[:, bi, hf * 4:(hf + 1) * 4], den, 1e-30, None, ALU.max, r=[b_acc[hf]], w=[b_rec])
                    g.op('dve', lambda hh, bi=bi: hh.reciprocal(rec[:, bi, :], rec[:, bi, :]), r=[b_rec], w=[b_rec])
                    for hp in range(8):
                        h = gg * 8 + hp
                        g.tt('dve', hsm[4][:, hp:hp + 1], rec[:, bi, hp:hp + 1], gts[:, bi * 16 + h:bi * 16 + h + 1],
                             ALU.mult, r=[b_rec, b_gts], w=[b_hsm])
                        g.stt('dve', og[:, h * 64:(h + 1) * 64], acc[hp // 4][:, (hp % 4) * 65:(hp % 4) * 65 + 64],
                              hsm[4][:, hp:hp + 1], og[:, h * 64:(h + 1) * 64], ALU.mult, ALU.add,
                              r=[b_acc[hp // 4], b_hsm], w=[b_og])
            g.cp('act', ogb[:], og[:], r=[b_og], w=[b_ogb])
            pt, pb = pp.next()
            ptb = pt[:].bitcast(BF16)
            for cc in range(8):
                g.tr(ptb[:, cc * 128:(cc + 1) * 128], ogb[:, cc * 128:(cc + 1) * 128], k.ident_b[:],
                     r=[b_ogb, k.b_ident], w=[pb])
            g.cp('dve', oT[:].rearrange('p a b -> p (a b)'), ptb, r=[pb], w=[b_oT])
            for hf in range(2):
                for ec in range(8):
                    g.mm(c['ps_out'][:, hf * 512:(hf + 1) * 512], oT[:, ec, :], wout[:, ec, hf * 512:(hf + 1) * 512],
                         ec == 0, ec == 7, r=[b_oT, b_wout[hf]], w=[c['b_ps_out']])
            mixer_back(k, c, T, xd, b_xd)
        g.barrier()


PARAM_SHAPES = {
    'norm_mix_pre': (DEPTH, D), 'norm_mix_post': (DEPTH, D), 'norm_mlp_pre': (DEPTH, D), 'norm_mlp_post': (DEPTH, D),
    'mlp_w_up': (DEPTH, D, DFF), 'mlp_w_down': (DEPTH, DFF, D),
    'ret_w_in': (1, D, 6144), 'ret_gn': (1, 2048), 'ret_w_out': (1, 2048, D),
    'gla_w_in': (1, D, 3088), 'gla_w_gate_up': (1, 16, 512), 'gla_b_gate': (1, 512), 'gla_gn': (1, 1024),
    'gla_w_out': (1, 1024, D),
    'lru_w_in': (1, D, 2048), 'lru_conv_w': (1, 4, 1024), 'lru_conv_b': (1, 1024), 'lru_w_a': (1, 8, 128, 128),
    'lru_b_a': (1, 1024), 'lru_w_x': (1, 8, 128, 128), 'lru_b_x': (1, 1024), 'lru_lambda': (1, 1024),
    'lru_w_out': (1, 1024, D),
    'nsa_w_in': (1, D, 1840), 'nsa_pe_k': (1, 32, 64), 'nsa_w1_k': (1, 2048, 128), 'nsa_w2_k': (1, 128, 64),
    'nsa_pe_v': (1, 32, 64), 'nsa_w1_v': (1, 2048, 128), 'nsa_w2_v': (1, 128, 64), 'nsa_w_out': (1, 1024, D),
}


def build(S, phases):
    nc = bass.Bass('TRN2', target_bir_lowering=False)
    x_in = nc.dram_tensor('x', [S, D], F32, kind='ExternalInput').ap()
    W = {}
    for name, shp in PARAM_SHAPES.items():
        W[name] = nc.dram_tensor(name, list(shp), F32, kind='ExternalInput').ap()
    if any(kind == 'mix' and li % 4 == 3 for kind, li in phases):
        for name, arr in nsa_tables(S // 128).items():
            W[name] = nc.dram_tensor(name, list(arr.shape), F32, kind='ExternalInput').ap()
    y = nc.dram_tensor('y', [S, D], F32, kind='ExternalOutput').ap()
    with contextlib.ExitStack() as es:
        g = G(nc, es)
        k = K(nc, g, es, S)
        setup_consts(k)
        NT = S // 128
        b_xd = [Buf() for _ in range(NT)]
        direct = bool(phases) and phases[0][0] == 'mix'
        if not direct:
            for t in range(NT):
                g.dma('sp', y[t * 128:(t + 1) * 128, :], x_in[t * 128:(t + 1) * 128, :], w=[b_xd[t]])
        for pi_, (kind, li) in enumerate(phases):
            k.xsrc = x_in if (direct and pi_ == 0) else None
            if kind == 'mlp':
                phase_mlp(k, li, y, b_xd, W)
            else:
                MIXERS[li % 4](k, li, y, b_xd, W)
        k.xsrc = None
        g.barrier()
        g.emit()
    return nc


MIXERS = {0: phase_ret, 1: phase_gla, 2: phase_lru, 3: phase_nsa}

ALL_PHASES = [(kind, li) for li in range(DEPTH) for kind in ('mix', 'mlp')]


def run(inputs, S, phases):
    nc = build(S, phases)
    B = inputs['x'].shape[0]
    in_maps = []
    tabs = nsa_tables(S // 128) if any(kind == 'mix' and li % 4 == 3 for kind, li in phases) else {}
    for b in range(B):
        m = {'x': np.ascontiguousarray(inputs['x'][b], dtype=np.float32)}
        for name in PARAM_SHAPES:
            m[name] = np.ascontiguousarray(inputs[name], dtype=np.float32)
        m.update(tabs)
        in_maps.append(m)
    res = run_bass_kernel_spmd(nc, in_maps, core_ids=list(range(B)))
    return np.stack([np.asarray(r['y']) for r in res.results], axis=0)


def kernel(**inputs):
    out = run(inputs, 4096, ALL_PHASES)
    return out.astype(np.float32)
```
